# Optimizing a Trainium2 kernel written in Bass

```python
import jax, jax.numpy as jnp
from jax import lax
import numpy as np

D_MODEL = 1024
BATCH = 32
SEQ = 2048
DEPTH = 4

N_EVEN = (DEPTH + 1) // 2
N_ODD = DEPTH // 2
CHUNK = 64
EPS = 1e-6
RET_HEADS = 4
RET_DV = D_MODEL // 2 // RET_HEADS
RET_DK = RET_DV
ROPE_BASE = 10000.0
GLA_HEADS = 4
GLA_DV = D_MODEL // 2 // GLA_HEADS
GLA_DK = GLA_DV // 2
GLA_RANK = 16
GLA_TAU = 16.0
AB_SIZES = (RET_HEADS * RET_DK, RET_HEADS * RET_DK, RET_HEADS * RET_DV, RET_HEADS * RET_DV,
            GLA_HEADS * GLA_DK, GLA_HEADS * GLA_DK, GLA_HEADS * GLA_DV, GLA_HEADS * GLA_DV,
            GLA_RANK)
AB_IN = sum(AB_SIZES)
HGRN_DK = 128
HGRN_HEADS = D_MODEL // HGRN_DK
HGRN_DV = D_MODEL // HGRN_HEADS
C_IN = 4 * D_MODEL
D_FF = ((8 * D_MODEL // 3 + 255) // 256) * 256
N_EXPERTS = 8
TOP_K = 2
EXPERT_FF = 7 * D_MODEL // 2
OUT_SCALE = (2 * DEPTH) ** -0.5

kernel_name = "hybrid_retnet_gla_hgrn2_moe_trunk"


def rms_norm(x, g):
    xf = x.astype(jnp.float32)
    y = xf * lax.rsqrt(jnp.mean(xf * xf, axis=-1, keepdims=True) + EPS)
    return (y * g.astype(jnp.float32)).astype(x.dtype)


def rotary(x, positions):
    d = x.shape[-1]
    inv_freq = ROPE_BASE ** (-jnp.arange(0, d, 2, dtype=jnp.float32) / d)
    ang = positions.astype(jnp.float32)[..., None, None] * inv_freq
    cos, sin = jnp.cos(ang), jnp.sin(ang)
    xf = x.astype(jnp.float32)
    x1, x2 = xf[..., : d // 2], xf[..., d // 2:]
    return jnp.concatenate([x1 * cos - x2 * sin, x2 * cos + x1 * sin], axis=-1)


def _to_chunks(a):
    b, t, h, d = a.shape
    return a.reshape(b, t // CHUNK, CHUNK, h, d).transpose(1, 0, 3, 2, 4)


def _from_chunks(a):
    nc, b, h, c, d = a.shape
    return a.transpose(1, 0, 3, 2, 4).reshape(b, nc * c, h, d)


def _causal_mask():
    t = jnp.arange(CHUNK)
    return t[:, None] >= t[None, :]


def retention_chunkwise(q, k, v, log_gamma):
    b, _, h, dk = q.shape
    dv = v.shape[-1]
    t = jnp.arange(CHUNK, dtype=jnp.float32)
    lg = log_gamma.astype(jnp.float32)
    rel = t[:, None] - t[None, :]
    intra_decay = jnp.exp(jnp.where(_causal_mask()[None], rel[None] * lg[:, None, None], -jnp.inf))
    q_decay = jnp.exp((t[None, :] + 1.0) * lg[:, None])[..., None]
    k_decay = jnp.exp((CHUNK - 1.0 - t[None, :]) * lg[:, None])[..., None]
    chunk_decay = jnp.exp(CHUNK * lg)[:, None, None]

    def step(state, inp):
        qc, kc, vc = inp
        scores = jnp.einsum('bhid,bhjd->bhij', qc, kc) * intra_decay
        o = (jnp.einsum('bhij,bhjv->bhiv', scores, vc)
             + jnp.einsum('bhid,bhdv->bhiv', qc * q_decay, state))
        state = chunk_decay * state + jnp.einsum('bhjd,bhjv->bhdv', kc * k_decay, vc)
        return state, o

    s0 = jnp.zeros((b, h, dk, dv), jnp.float32)
    _, o = lax.scan(step, s0, (_to_chunks(q), _to_chunks(k), _to_chunks(v)))
    return _from_chunks(o)


def gated_chunkwise(q, k, v, log_f):
    b, _, h, dk = q.shape
    dv = v.shape[-1]
    mask = _causal_mask()[:, :, None]
    g_cum = jnp.cumsum(_to_chunks(log_f), axis=-2)

    def step(state, inp):
        qc, kc, vc, gc = inp
        diff = gc[:, :, :, None, :] - gc[:, :, None, :, :]
        decay = jnp.exp(jnp.where(mask, diff, -jnp.inf))
        scores = jnp.einsum('bhid,bhjd,bhijd->bhij', qc, kc, decay)
        g_last = gc[:, :, -1:, :]
        o = (jnp.einsum('bhij,bhjv->bhiv', scores, vc)
             + jnp.einsum('bhid,bhdv->bhiv', qc * jnp.exp(gc), state))
        state = (jnp.exp(g_last[:, :, 0, :])[..., None] * state
                 + jnp.einsum('bhjd,bhjv->bhdv', kc * jnp.exp(g_last - gc), vc))
        return state, o

    s0 = jnp.zeros((b, h, dk, dv), jnp.float32)
    _, o = lax.scan(step, s0, (_to_chunks(q), _to_chunks(k), _to_chunks(v), g_cum))
    return _from_chunks(o)


def retnet_gla_mixer(h, positions, w_in, w_a2, b_a2, ret_g, gla_g, w_out):
    b, t, _ = h.shape
    z = h @ w_in
    aq, ak, av, ag, bq, bk, bv, bg, ba = jnp.split(z, np.cumsum(AB_SIZES)[:-1], axis=-1)
    f32 = jnp.float32

    def heads(a, n):
        return a.reshape(b, t, n, -1).astype(f32)

    qa = rotary(heads(aq, RET_HEADS), positions)
    ka = rotary(heads(ak, RET_HEADS), positions) * (RET_DK ** -0.5)
    log_gamma = jnp.log1p(-jnp.exp2(-5.0 - jnp.arange(RET_HEADS, dtype=f32)))
    oa = retention_chunkwise(qa, ka, heads(av, RET_HEADS), log_gamma)
    oa = rms_norm(oa, ret_g) * jax.nn.silu(heads(ag, RET_HEADS))
    log_alpha = jax.nn.log_sigmoid((ba @ w_a2 + b_a2).astype(f32)) / GLA_TAU
    ob = gated_chunkwise(heads(bq, GLA_HEADS) * (GLA_DK ** -0.5), heads(bk, GLA_HEADS),
                         heads(bv, GLA_HEADS), heads(log_alpha, GLA_HEADS))
    ob = rms_norm(ob, gla_g) * jax.nn.silu(heads(bg, GLA_HEADS))
    o = jnp.concatenate([oa.reshape(b, t, -1), ob.reshape(b, t, -1)], axis=-1)
    return o.astype(h.dtype) @ w_out


def hgrn2_mixer(h, lb, w_in, norm_g, w_out):
    b, t, d = h.shape
    f32 = jnp.float32
    q, f, i, g = jnp.split(h @ w_in, 4, axis=-1)

    def heads(a):
        return a.reshape(b, t, HGRN_HEADS, -1).astype(f32)

    f = heads(f)
    lb = lb.astype(f32)
    log_f = jnp.logaddexp(jnp.log(lb), jnp.log1p(-lb) + jax.nn.log_sigmoid(f))
    k = (1.0 - lb) * jax.nn.sigmoid(-f)
    qh = jax.nn.silu(heads(q)) * (HGRN_DK ** -0.5)
    o = gated_chunkwise(qh, k, heads(i), log_f)
    o = rms_norm(o, norm_g) * jax.nn.silu(heads(g))
    return o.reshape(b, t, d).astype(h.dtype) @ w_out


def swiglu(h, w_gate, w_up, w_down):
    return (jax.nn.silu(h @ w_gate) * (h @ w_up)) @ w_down


def moe_swiglu(h, w_router, w_gate, w_up, w_down):
    b, t, d = h.shape
    xt = h.reshape(b * t, d)
    logits = (xt @ w_router).astype(jnp.float32)
    top_logits, top_idx = lax.top_k(logits, TOP_K)
    gates = jax.nn.softmax(top_logits, axis=-1)
    flat_e = top_idx.reshape(-1)
    order = jnp.argsort(flat_e)
    group_sizes = jnp.bincount(flat_e, length=N_EXPERTS).astype(jnp.int32)
    xs = xt[order // TOP_K]
    hid = (jax.nn.silu(lax.ragged_dot(xs, w_gate, group_sizes))
           * lax.ragged_dot(xs, w_up, group_sizes))
    ys = lax.ragged_dot(hid, w_down, group_sizes)
    y = jnp.zeros_like(ys).at[order].set(ys).reshape(b * t, TOP_K, d)
    out = jnp.einsum('nk,nkd->nd', gates.astype(y.dtype), y)
    return out.reshape(b, t, d)


def setup_inputs(seed: int = 0) -> dict:
    key = jax.random.key(seed)
    ks = jax.random.split(key, 24)
    f32 = jnp.float32

    def nrm(k, shape, scale):
        return jax.random.normal(k, shape, f32) * scale

    def gain(k, shape):
        return 1.0 + 0.02 * jax.random.normal(k, shape, f32)

    ds = D_MODEL ** -0.5
    return {
        "x": nrm(ks[0], (BATCH, SEQ, D_MODEL), 1.0),
        "positions": jnp.broadcast_to(jnp.arange(SEQ, dtype=jnp.int32), (BATCH, SEQ)),
        "norm_mix_g": gain(ks[1], (DEPTH, D_MODEL)),
        "norm_ffn_g": gain(ks[2], (DEPTH, D_MODEL)),
        "final_norm_g": gain(ks[3], (D_MODEL,)),
        "ab_w_in": nrm(ks[4], (N_EVEN, D_MODEL, AB_IN), ds),
        "gla_w_a2": nrm(ks[5], (N_EVEN, GLA_RANK, GLA_HEADS * GLA_DK), GLA_RANK ** -0.5),
        "gla_b_a2": nrm(ks[6], (N_EVEN, GLA_HEADS * GLA_DK), 0.1),
        "ret_norm_g": gain(ks[7], (N_EVEN, RET_DV)),
        "gla_norm_g": gain(ks[8], (N_EVEN, GLA_DV)),
        "ab_w_out": nrm(ks[9], (N_EVEN, D_MODEL, D_MODEL), ds * OUT_SCALE),
        "ffn_w_gate": nrm(ks[10], (N_EVEN, D_MODEL, D_FF), ds),
        "ffn_w_up": nrm(ks[11], (N_EVEN, D_MODEL, D_FF), ds),
        "ffn_w_down": nrm(ks[12], (N_EVEN, D_FF, D_MODEL), D_FF ** -0.5 * OUT_SCALE),
        "hgrn_lb_logits": nrm(ks[13], (DEPTH, HGRN_HEADS * HGRN_DK), 0.1),
        "c_w_in": nrm(ks[14], (N_ODD, D_MODEL, C_IN), ds),
        "hgrn_norm_g": gain(ks[15], (N_ODD, HGRN_DV)),
        "c_w_out": nrm(ks[16], (N_ODD, D_MODEL, D_MODEL), ds * OUT_SCALE),
        "moe_router": nrm(ks[17], (N_ODD, D_MODEL, N_EXPERTS), ds),
        "moe_w_gate": nrm(ks[18], (N_ODD, N_EXPERTS, D_MODEL, EXPERT_FF), ds),
        "moe_w_up": nrm(ks[19], (N_ODD, N_EXPERTS, D_MODEL, EXPERT_FF), ds),
        "moe_w_down": nrm(ks[20], (N_ODD, N_EXPERTS, EXPERT_FF, D_MODEL), EXPERT_FF ** -0.5 * OUT_SCALE),
    }


def reference(x, positions, norm_mix_g, norm_ffn_g, final_norm_g, ab_w_in, gla_w_a2, gla_b_a2,
              ret_norm_g, gla_norm_g, ab_w_out, ffn_w_gate, ffn_w_up, ffn_w_down,
              hgrn_lb_logits, c_w_in, hgrn_norm_g, c_w_out, moe_router, moe_w_gate,
              moe_w_up, moe_w_down):
    lb_cum = jnp.cumsum(jax.nn.softmax(hgrn_lb_logits.astype(jnp.float32), axis=0), axis=0)
    lower_bounds = lb_cum - lb_cum[0:1]
    h = x
    for l in range(DEPTH):
        e = l // 2
        hn = rms_norm(h, norm_mix_g[l])
        if l % 2 == 0:
            h = h + retnet_gla_mixer(hn, positions, ab_w_in[e], gla_w_a2[e], gla_b_a2[e],
                                     ret_norm_g[e], gla_norm_g[e], ab_w_out[e])
            h = h + swiglu(rms_norm(h, norm_ffn_g[l]), ffn_w_gate[e], ffn_w_up[e], ffn_w_down[e])
        else:
            lb = lower_bounds[l].reshape(HGRN_HEADS, HGRN_DK)
            h = h + hgrn2_mixer(hn, lb, c_w_in[e], hgrn_norm_g[e], c_w_out[e])
            h = h + moe_swiglu(rms_norm(h, norm_ffn_g[l]), moe_router[e], moe_w_gate[e],
                               moe_w_up[e], moe_w_down[e])
    return rms_norm(h, final_norm_g)
```

```python
import numpy as np
from contextlib import ExitStack
import concourse.bass as bass
import concourse.mybir as mybir
from concourse.bass_utils import run_bass_kernel_spmd

F32 = mybir.dt.float32
BF16 = mybir.dt.bfloat16
I32 = mybir.dt.int32
AF = mybir.ActivationFunctionType
ALU = mybir.AluOpType
AX = mybir.AxisListType

D = 1024
DBG_FLIP = False
DBG = False
T = 2048
NT = 16
EPS = 1e-6


class Tok:
    __slots__ = ("sem", "val", "eng")

    def __init__(self, sem, val, eng):
        self.sem = sem
        self.val = val
        self.eng = eng


class Buf:
    __slots__ = ("name", "w", "r", "dsem", "dcnt")

    def __init__(self, name):
        self.name = name
        self.w = None
        self.r = {}
        self.dsem = None
        self.dcnt = 0


class Prog:
    ENGS = ("pe", "act", "dve", "pool", "sp")

    def __init__(self, nc, stack):
        self.nc = nc
        self.stack = stack
        self.q = {e: [] for e in self.ENGS}
        self.cnt = {e: 0 for e in self.ENGS}
        self.sem = {e: stack.enter_context(nc.semaphore("s_" + e)) for e in ("pe", "act", "dve", "pool")}
        self.waited = {e: {} for e in self.ENGS}
        self.nsem = 4
        self.last_dma = None

    def _deps(self, eng, reads, writes):
        need = {}

        def add(t):
            if t is None:
                return
            if t.eng == "pe" and eng == "pe":
                return
            k = id(t.sem)
            if k not in need or need[k][1] < t.val:
                need[k] = (t.sem, t.val)

        for b in reads:
            add(b.w)
        for b in writes:
            add(b.w)
            for t in b.r.values():
                add(t)
        wd = self.waited[eng]
        for k, (sem, val) in need.items():
            if wd.get(k, -1) >= val:
                continue
            wd[k] = val
            self.q[eng].append(("wait", sem, val))

    def _mark(self, tok, reads, writes):
        for b in reads:
            b.r[id(tok.sem)] = tok
        for b in writes:
            b.w = tok
            b.r = {}

    def op(self, eng, fn, reads=(), writes=()):
        self._deps(eng, reads, writes)
        self.cnt[eng] += 1
        tok = Tok(self.sem[eng], self.cnt[eng], eng)
        self.q[eng].append(("op", fn, self.sem[eng], 1))
        self._mark(tok, reads, writes)
        return tok

    def dma(self, qeng, fn, reads, writes, primary):
        self._deps(qeng, reads, writes)
        if primary.dsem is None:
            primary.dsem = self.stack.enter_context(self.nc.semaphore("d_" + primary.name))
            self.nsem += 1
        primary.dcnt += 16
        tok = Tok(primary.dsem, primary.dcnt, None)
        self.q[qeng].append(("op", fn, primary.dsem, 16))
        self._mark(tok, reads, writes)
        return tok

    def emit(self, final_toks):
        nc = self.nc
        for t in final_toks:
            self.q["sp"].append(("wait", t.sem, t.val))

        def run(e, items):
            for it in items:
                if it[0] == "wait":
                    e.wait_ge(it[1], it[2])
                else:
                    it[1](e).then_inc(it[2], it[3])

        with nc.Block() as block:
            @block.tensor
            def _(e):
                run(e, self.q["pe"])

            @block.scalar
            def _(e):
                run(e, self.q["act"])

            @block.vector
            def _(e):
                run(e, self.q["dve"])

            @block.gpsimd
            def _(e):
                run(e, self.q["pool"])

            @block.sync
            def _(e):
                run(e, self.q["sp"])


C_ID = 0
C_ONES = 128
C_MASK = 256
C_RESET = 320
C_INVF = 832
C_SIGN = 833
C_EPS = 834
C_ONE = 835
C_NEGPI = 836
C_DQ = 840
C_DK = 840 + 256
NCONST = 840 + 512


def make_consts():
    c = np.zeros((128, NCONST), np.float32)
    c[:, C_ID:C_ID + 128] = np.eye(128, dtype=np.float32)
    c[:, C_ONES:C_ONES + 128] = 1.0
    j = np.arange(64)[:, None]
    i = np.arange(64)[None, :]
    c[0:64, C_MASK:C_MASK + 64] = (j <= i).astype(np.float32)
    r = np.ones(512, np.float32)
    r[::64] = 0.0
    c[:, C_RESET:C_RESET + 512] = r[None, :]
    p = np.arange(128)
    c[:, C_INVF] = (10000.0 ** (-(2.0 * (p % 64)) / 128.0)).astype(np.float32)
    c[:, C_SIGN] = np.where(p < 64, -1.0, 1.0)
    c[:, C_EPS] = EPS
    c[:, C_ONE] = 1.0
    c[:, C_NEGPI] = -np.pi
    for hh in range(4):
        lg = np.log1p(-np.exp2(-5.0 - hh))
        t = np.arange(64, dtype=np.float64)
        c[:, C_DQ + hh * 64:C_DQ + (hh + 1) * 64] = np.exp((t + 1.0) * lg)[None, :]
        c[:, C_DK + hh * 64:C_DK + (hh + 1) * 64] = (np.exp(-(t + 1.0) * lg) * 128.0 ** -0.5)[None, :]
    return c


RET_A = [float(np.exp(64.0 * np.log1p(-np.exp2(-5.0 - hh)))) for hh in range(4)]

WNAMES = [("norm_mix_g", [4, 1024]), ("norm_ffn_g", [4, 1024]), ("final_norm_g", [1024]),
          ("ab_w_in", [2, 1024, 3600]), ("gla_w_a2", [2, 16, 256]), ("gla_b_a2", [2, 256]),
          ("ret_norm_g", [2, 128]), ("gla_norm_g", [2, 128]), ("ab_w_out", [2, 1024, 1024]),
          ("ffn_w_gate", [2, 1024, 2816]), ("ffn_w_up", [2, 1024, 2816]), ("ffn_w_down", [2, 2816, 1024]),
          ("hgrn_lb_logits", [4, 1024]), ("c_w_in", [2, 1024, 4096]), ("hgrn_norm_g", [2, 128]),
          ("c_w_out", [2, 1024, 1024]), ("moe_router", [2, 1024, 8]), ("moe_w_gate", [2, 8, 1024, 3584]),
          ("moe_w_up", [2, 8, 1024, 3584]), ("moe_w_down", [2, 8, 3584, 1024])]


def build(nseq, nsub=8, final=True):
    nc = bass.Bass("TRN2", target_bir_lowering=False)
    x_d = nc.dram_tensor("x", [nseq, T, D], F32, kind="ExternalInput").ap()
    pos_d = nc.dram_tensor("positions", [nseq, T], I32, kind="ExternalInput").ap()
    W = {n: nc.dram_tensor(n, s, F32, kind="ExternalInput").ap() for n, s in WNAMES}
    cst_d = nc.dram_tensor("consts", [128, NCONST], F32, kind="ExternalInput").ap()
    out_d = nc.dram_tensor("out", [nseq, T, D], F32, kind="ExternalOutput").ap()
    dbg_d = nc.dram_tensor("dbg", [128, 8, 512], F32, kind="ExternalOutput").ap() if DBG else None

    with ExitStack() as st:
        P = Prog(nc, st)

        def sb(n, s, d):
            return st.enter_context(nc.sbuf_tensor(n, s, d))

        h = sb("h", [128, NT, D], F32)
        Bh = [Buf("h%d" % t) for t in range(NT)]
        hnT = sb("hnT", [128, 8, T], BF16)
        BhnT = [Buf("hnT%d" % t) for t in range(NT)]
        cst = sb("cst", [128, NCONST], F32)
        Bcst = Buf("cst")
        idb = sb("idb", [128, 128], BF16)
        Bidb = Buf("idb")
        gbc = sb("gbc", [128, D], F32)
        Bgbc = Buf("gbc")
        hntok = [sb("hntok%d" % i, [128, D], BF16) for i in range(2)]
        Bhntok = [Buf("hntok%d" % i) for i in range(2)]
        stat = sb("stat", [128, 64], F32)
        Bstat = Buf("stat")
        junk = hntok[1]
        Bjunk = Bhntok[1]
        lbt = sb("lbt", [128, 4, 8], F32)
        Blbt = Buf("lbt")
        oml = sb("oml", [128, 4, 8], F32)
        noml = sb("noml", [128, 4, 8], F32)
        hng = sb("hng", [128, 8], F32)
        Bhng = Buf("hng")
        ba2 = sb("ba2", [64, 2, 4], F32)
        Bba2 = Buf("ba2")
        wa2f = sb("wa2f", [16, 2, 256], F32)
        wa2 = sb("wa2", [16, 2, 256], BF16)
        Bwa2 = Buf("wa2")
        NW = 6
        wk = [sb("wk%d" % i, [128, 512], F32) for i in range(NW)]
        Bwk = [Buf("wk%d" % i) for i in range(NW)]
        arena = sb("arena", [128, 24576], BF16)
        win = [arena[:, i * 6144:(i + 1) * 6144].rearrange("p (k f) -> p k f", k=8) for i in range(2)]
        Bwin = [[Buf("win%d_%d" % (i, j)) for j in range(8)] for i in range(2)]
        qh = [sb("qh%d" % i, [128, 512], BF16) for i in range(2)]
        Bqh = [Buf("qh%d" % i) for i in range(2)]
        kh = [sb("kh%d" % i, [128, 512], BF16) for i in range(2)]
        Bkh = [Buf("kh%d" % i) for i in range(2)]
        vt = [sb("vt%d" % i, [64, 8, 128], BF16) for i in range(2)]
        Bvt = [Buf("vt%d" % i) for i in range(2)]
        gsl = [sb("gsl%d" % i, [128, 512], BF16) for i in range(2)]
        Bgsl = [Buf("gsl%d" % i) for i in range(2)]
        acol = [sb("acol%d" % i, [128, 8], F32) for i in range(2)]
        Bacol = [Buf("acol%d" % i) for i in range(2)]
        bab = sb("bab", [16, 512], BF16)
        Bbab = Buf("bab")
        Sst = sb("Sst", [128, 8, 128], F32)
        arena2 = sb("arena2", [128, 4096], BF16)
        Sbf = arena2[:, 3072:4096].rearrange("p (k f) -> p k f", k=8)
        BS = [Buf("S%d" % i) for i in range(8)]
        BSb = [Buf("Sb%d" % i) for i in range(8)]
        Sa = sb("Sa", [128, 128], F32)
        BSa = Buf("Sa")
        pTall = [sb("pTall%d" % i, [64, 8, 64], BF16) for i in range(2)]
        BpTall = [Buf("pTall%d" % i) for i in range(2)]
        ktkall = [sb("ktkall%d" % i, [64, 8, 128], BF16) for i in range(2)]
        Bktkall = [Buf("ktkall%d" % i) for i in range(2)]
        Sball = [arena2[:, 3072:4096].rearrange("p (k f) -> p k f", k=8), sb("Sball1", [128, 8, 128], BF16)]
        BSball = [Buf("Sball%d" % i) for i in range(2)]
        og = arena[:, 20480:24576].rearrange("p (k f) -> p k f", k=8)
        Bog = [Buf("og%d" % i) for i in range(8)]
        cs = arena2[:, 0:2048].bitcast(F32).rearrange("p (k f) -> p k f", k=2)
        Bcs = Buf("cs")
        posi = arena2[:, 2048:3072].bitcast(I32)
        Bposi = Buf("posi")
        wo = arena[:, 12288:20480].rearrange("p (k f) -> p k f", k=8)
        Bwo = [Buf("wo0"), Buf("wo1")]
        wg = [arena[:, i * 4096:(i + 1) * 4096].rearrange("p (k f) -> p k f", k=8) for i in range(2)]
        wu = [arena[:, 8192 + i * 4096:8192 + (i + 1) * 4096].rearrange("p (k f) -> p k f", k=8) for i in range(2)]
        wd = [arena[:, 16384 + i * 4096:16384 + (i + 1) * 4096].rearrange("p (k f) -> p k f", k=4) for i in range(2)]
        Bwg = [Buf("wg%d" % i) for i in range(2)]
        Bwu = [Buf("wu%d" % i) for i in range(2)]
        Bwd = [Buf("wd%d" % i) for i in range(2)]
        hid = [arena2[:, i * 2048:(i + 1) * 2048].rearrange("p (k f) -> p k f", k=4) for i in range(2)]
        Bhid = [[Buf("hid%d_%d" % (i, j)) for j in range(4)] for i in range(2)]
        wr = sb("wr", [128, 8, 8], BF16)
        Bwr = Buf("wr")
        lg = sb("lg", [128, NT, 8], F32)
        Blg = Buf("lg")
        gt = sb("gt", [128, NT, 8], F32)
        Bgt = Buf("gt")
        rt = [sb("rt%d" % i, [128, NT, 8], F32) for i in range(3)]
        Brt = [Buf("rt%d" % i) for i in range(3)]

        def alias_sync(src, dst):
            toks = {}
            for b_ in src:
                for t_ in ([b_.w] if b_.w is not None else []) + list(b_.r.values()):
                    k_ = id(t_.sem)
                    if k_ not in toks or toks[k_].val < t_.val:
                        toks[k_] = t_
            for b_ in dst:
                for k_, t_ in toks.items():
                    if k_ not in b_.r or b_.r[k_].val < t_.val:
                        b_.r[k_] = t_

        def mixer_bufs():
            return [x_ for l_ in Bwin for x_ in l_] + Bwo + Bog + [Bcs, Bposi, BSball[0]]

        def ffn_bufs():
            return Bwg + Bwu + Bwd + [x_ for l_ in Bhid for x_ in l_]

        pb = [st.enter_context(nc.psum_tensor("pb%d" % i, [128, 512], F32)) for i in range(8)]
        Bpb = [Buf("pb%d" % i) for i in range(8)]
        gctr = [0]

        def gbank():
            i = gctr[0] % 4
            gctr[0] += 1
            return pb[i], Bpb[i]

        wctr = [0]

        def gwk():
            i = wctr[0] % NW
            wctr[0] += 1
            return wk[i], Bwk[i]

        def C(col, n=1, rows=128):
            return cst[0:rows, col:col + n]

        P.dma("sp", lambda e: e.dma_start(out=cst[:], in_=cst_d), [], [Bcst], Bcst)
        P.op("dve", lambda e: e.tensor_copy(out=idb[:], in_=cst[:, C_ID:C_ID + 128]), [Bcst], [Bidb])
        P.dma("sp", lambda e: e.dma_start(out=lbt[:], in_=W["hgrn_lb_logits"].rearrange("l (hh p) -> p l hh", p=128), allow_slow_non_contiguous=True), [], [Blbt], Blbt)
        P.op("act", lambda e: e.activation(out=lbt[:], in_=lbt[:], func=AF.Exp), [Blbt], [Blbt])
        P.op("dve", lambda e: e.tensor_tensor(out=stat[:, 0:8], in0=lbt[:, 0, :], in1=lbt[:, 1, :], op=ALU.add), [Blbt], [Bstat])
        P.op("dve", lambda e: e.tensor_tensor(out=stat[:, 0:8], in0=stat[:, 0:8], in1=lbt[:, 2, :], op=ALU.add), [Blbt, Bstat], [Bstat])
        P.op("dve", lambda e: e.tensor_tensor(out=stat[:, 0:8], in0=stat[:, 0:8], in1=lbt[:, 3, :], op=ALU.add), [Blbt, Bstat], [Bstat])
        P.op("dve", lambda e: e.reciprocal(out=stat[:, 0:8], in_=stat[:, 0:8]), [Bstat], [Bstat])
        for l in range(4):
            P.op("dve", lambda e, l=l: e.tensor_tensor(out=lbt[:, l, :], in0=lbt[:, l, :], in1=stat[:, 0:8], op=ALU.mult), [Blbt, Bstat], [Blbt])
        P.op("dve", lambda e: e.memset(lbt[:, 0, :], 0.0), [Blbt], [Blbt])
        P.op("dve", lambda e: e.tensor_tensor(out=lbt[:, 2, :], in0=lbt[:, 2, :], in1=lbt[:, 1, :], op=ALU.add), [Blbt], [Blbt])
        P.op("dve", lambda e: e.tensor_tensor(out=lbt[:, 3, :], in0=lbt[:, 3, :], in1=lbt[:, 2, :], op=ALU.add), [Blbt], [Blbt])
        P.op("dve", lambda e: e.tensor_scalar(out=oml[:], in0=lbt[:], scalar1=-1.0, scalar2=1.0, op0=ALU.mult, op1=ALU.add), [Blbt], [Blbt])
        P.op("dve", lambda e: e.tensor_scalar(out=noml[:], in0=oml[:], scalar1=-1.0, scalar2=None, op0=ALU.mult), [Blbt], [Blbt])
        P.dma("sp", lambda e: e.dma_start(out=hng[:, 0:2], in_=W["ret_norm_g"].rearrange("l p -> p l"), allow_slow_non_contiguous=True), [], [Bhng], Bhng)
        P.dma("sp", lambda e: e.dma_start(out=hng[:, 2:4], in_=W["gla_norm_g"].rearrange("l p -> p l"), allow_slow_non_contiguous=True), [], [Bhng], Bhng)
        P.dma("sp", lambda e: e.dma_start(out=hng[:, 4:6], in_=W["hgrn_norm_g"].rearrange("l p -> p l"), allow_slow_non_contiguous=True), [], [Bhng], Bhng)
        P.dma("sp", lambda e: e.dma_start(out=ba2[:], in_=W["gla_b_a2"].rearrange("l (hh p) -> p l hh", p=64), allow_slow_non_contiguous=True), [], [Bba2], Bba2)
        P.op("dve", lambda e: e.tensor_scalar(out=ba2[:], in0=ba2[:], scalar1=-1.0, scalar2=None, op0=ALU.mult), [Bba2], [Bba2])
        P.dma("sp", lambda e: e.dma_start(out=wa2f[:], in_=W["gla_w_a2"].rearrange("l r c -> r l c")), [], [Bwa2], Bwa2)
        P.op("dve", lambda e: e.tensor_copy(out=wa2[:], in_=wa2f[:]), [Bwa2], [Bwa2])

        def rstd_from_sumsq(ap, n, bufs):
            P.op("act", lambda e: e.activation(out=ap, in_=ap, func=AF.Ln, scale=1.0 / n, bias=C(C_EPS, rows=ap.shape[0])), bufs + [Bcst], bufs)
            P.op("act", lambda e: e.activation(out=ap, in_=ap, func=AF.Exp, scale=-0.5), bufs, bufs)

        def norm(g_row_ap, to_out=None):
            P.dma("sp", lambda e: e.dma_start(out=gbc[:], in_=g_row_ap.partition_broadcast(128)), [], [Bgbc], Bgbc)
            for t in range(NT):
                P.op("act", lambda e, t=t: e.activation(out=junk[:], in_=h[:, t, :], func=AF.Square, accum_out=stat[:, 16 + t:17 + t]), [Bh[t]], [Bjunk, Bstat])
            rstd_from_sumsq(stat[:, 16:32], float(D), [Bstat])
            toks = []
            for t in range(NT):
                if to_out is not None:
                    for dh in range(2):
                        ot, Bot = gwk()
                        P.op("dve", lambda e, t=t, dh=dh, ot=ot: e.scalar_tensor_tensor(out=ot[:], in0=h[:, t, dh * 512:(dh + 1) * 512], scalar=stat[:, 16 + t:17 + t], in1=gbc[:, dh * 512:(dh + 1) * 512], op0=ALU.mult, op1=ALU.mult), [Bh[t], Bstat, Bgbc], [Bot])
                        toks.append(P.dma("sp", lambda e, t=t, dh=dh, ot=ot: e.dma_start(out=to_out[t * 128:(t + 1) * 128, dh * 512:(dh + 1) * 512], in_=ot[:]), [Bot], [], Bot))
                    continue
                i = t % 2
                P.op("dve", lambda e, t=t, i=i: e.scalar_tensor_tensor(out=hntok[i][:], in0=h[:, t, :], scalar=stat[:, 16 + t:17 + t], in1=gbc[:], op0=ALU.mult, op1=ALU.mult), [Bh[t], Bstat, Bgbc], [Bhntok[i]])
                bank, Bb = gbank()
                bv = bank[:].bitcast(BF16)
                for c in range(8):
                    P.op("pe", lambda e, c=c, i=i, bv=bv: e.transpose(out=bv[:, c * 128:(c + 1) * 128], in_=hntok[i][:, c * 128:(c + 1) * 128], identity=idb[:]), [Bhntok[i], Bidb], [Bb])
                P.op("act", lambda e, t=t, bv=bv: e.copy(out=hnT[:, :, t * 128:(t + 1) * 128], in_=bv.rearrange("p (c k) -> p c k", c=8)), [Bb], [BhnT[t]])
            return toks

        def proj_fm(bank, Bb, wslot, Bw, c0, m, tok0):
            for kc in range(8):
                P.op("pe", lambda e, kc=kc: e.matmul(bank[0:m, :], lhsT=wslot[:, kc, c0:c0 + m], rhs=hnT[:, kc, tok0:tok0 + 512], start=(kc == 0), stop=(kc == 7)),
                     Bw + BhnT[tok0 // 128:tok0 // 128 + 4], [Bb])

        def load_cols(slot, j, w_ap, c0, n, off):
            P.dma("pool", lambda e: e.dma_start(out=win[slot][:, :, off:off + n], in_=w_ap.rearrange("(kc p) f -> p kc f", p=128)[:, :, c0:c0 + n]), [], [Bwin[slot][j]], Bwin[slot][j])

        vstate = {}

        def vproj_a(s, off, tok0):
            bank, Bb = gbank()
            proj_fm(bank, Bb, win[s], Bwin[s], off, 128, tok0)
            vf, Bvf = gwk()
            vfb = vf[:].bitcast(BF16)
            P.op("act", lambda e: e.copy(out=vfb[:, 0:512], in_=bank[:]), [Bb], [Bvf])
            vstate[s] = (vfb, Bvf)

        def vproj_b(s):
            vfb, Bvf = vstate[s]
            bank2, Bb2 = gbank()
            b2v = bank2[:].bitcast(BF16)
            for c in range(8):
                P.op("pe", lambda e, c=c: e.transpose(out=b2v[0:64, c * 128:(c + 1) * 128], in_=vfb[:, c * 64:(c + 1) * 64], identity=idb[:]), [Bvf, Bidb], [Bb2])
            P.op("act", lambda e: e.copy(out=vt[s][:], in_=b2v[0:64, :].rearrange("p (c v) -> p c v", c=8)), [Bb2], [Bvt[s]])

        def core_a(s, hd, dk, a_float):
            Ps, BPs = pb[4], Bpb[4]
            Pk, BPk = pb[5], Bpb[5]
            pkv = Pk[:].bitcast(BF16)
            for c in range(8):
                cols = slice(c * 64, (c + 1) * 64)
                P.op("pe", lambda e, cols=cols: e.matmul(Ps[0:64, cols], lhsT=kh[s][0:dk, cols], rhs=qh[s][0:dk, cols], start=True, stop=True), [Bkh[s], Bqh[s]], [BPs])
            for c in range(8):
                cols = slice(c * 64, (c + 1) * 64)
                P.op("pe", lambda e, cols=cols, c=c: e.transpose(out=pkv[0:64, c * 128:c * 128 + dk], in_=kh[s][0:dk, cols], identity=idb[0:dk, 0:dk]), [Bkh[s], Bidb], [BPk])
            P.op("dve", lambda e: e.tensor_tensor(out=pTall[s][:], in0=Ps[0:64, :].rearrange("p (c i) -> p c i", c=8), in1=cst[0:64, C_MASK:C_MASK + 64].unsqueeze(1).to_broadcast([64, 8, 64]), op=ALU.mult), [BPs, Bcst], [BpTall[s]])
            P.op("act", lambda e: e.copy(out=ktkall[s][:, :, 0:dk], in_=pkv[0:64, :].rearrange("p (c k) -> p c k", c=8)[:, :, 0:dk]), [BPk], [Bktkall[s]])
            for c in range(8):
                bank, Bb_ = (Ps, BPs) if c < 4 else (Pk, BPk)
                cc = c % 4
                P.op("pe", lambda e, c=c, cc=cc, bank=bank: e.matmul(bank[0:dk, cc * 128:(cc + 1) * 128], lhsT=ktkall[s][:, c, 0:dk], rhs=vt[s][:, c, :], start=True, stop=True), [Bktkall[s], Bvt[s]], [Bb_])
            P.op("act", lambda e: e.copy(out=Sball[s][0:dk, 0, :], in_=Sst[0:dk, hd, :]), [BS[hd]], [BSball[s]])
            for c in range(8):
                bank, Bb_ = (Ps, BPs) if c < 4 else (Pk, BPk)
                cc = c % 4
                U = bank[0:dk, cc * 128:(cc + 1) * 128]
                P.op("dve", lambda e, U=U: e.tensor_tensor(out=Sa[0:dk, :], in0=U, in1=Sst[0:dk, hd, :], op=ALU.add), [Bb_, BS[hd]], [BSa])
                if a_float is not None:
                    P.op("dve", lambda e: e.tensor_scalar(out=Sst[0:dk, hd, :], in0=Sa[0:dk, :], scalar1=a_float, scalar2=None, op0=ALU.mult), [BSa], [BS[hd]])
                else:
                    P.op("dve", lambda e, c=c: e.tensor_scalar(out=Sst[0:dk, hd, :], in0=Sa[0:dk, :], scalar1=acol[s][0:dk, c:c + 1], scalar2=None, op0=ALU.mult), [BSa, Bacol[s]], [BS[hd]])
                if c < 7:
                    P.op("act", lambda e, c=c: e.copy(out=Sball[s][0:dk, c + 1, :], in_=Sst[0:dk, hd, :]), [BS[hd]], [BSball[s]])

        def core_b(s, hd, dk, gcol):
            Po, BPo = pb[6 + (hd % 2)], Bpb[6 + (hd % 2)]
            for c in range(8):
                cols = slice(c * 64, (c + 1) * 64)
                P.op("pe", lambda e, cols=cols, c=c: e.matmul(Po[:, cols], lhsT=vt[s][:, c, :], rhs=pTall[s][:, c, :], start=True, stop=False), [Bvt[s], BpTall[s]], [BPo])
                P.op("pe", lambda e, cols=cols, c=c: e.matmul(Po[:, cols], lhsT=Sball[s][0:dk, c, :], rhs=qh[s][0:dk, cols], start=False, stop=True), [BSball[s], Bqh[s]], [BPo])
            post(Po, BPo, s, hd, gcol)

        def post(Po, BPo, s, hd, gcol):
            sq, Bsq = gwk()
            P.op("act", lambda e: e.activation(out=sq[:], in_=Po[:], func=AF.Square), [BPo], [Bsq])
            bank, Bb = gbank()
            P.op("pe", lambda e: e.matmul(bank[:], lhsT=cst[:, C_ONES:C_ONES + 128], rhs=sq[:], start=True, stop=True), [Bcst, Bsq], [Bb])
            r, Br = gwk()
            P.op("act", lambda e: e.activation(out=r[:], in_=bank[:], func=AF.Ln, scale=1.0 / 128.0, bias=C(C_EPS)), [Bb, Bcst], [Br])
            P.op("act", lambda e: e.activation(out=r[:], in_=r[:], func=AF.Exp, scale=-0.5), [Br], [Br])
            P.op("dve", lambda e: e.scalar_tensor_tensor(out=r[:], in0=Po[:], scalar=hng[:, gcol:gcol + 1], in1=r[:], op0=ALU.mult, op1=ALU.mult), [BPo, Bhng, Br], [Br])
            P.op("dve", lambda e: e.tensor_tensor(out=og[:, hd, :], in0=r[:], in1=gsl[s][:], op=ALU.mult), [Br, Bgsl[s]], [Bog[hd]])

        def silu_to(out_ap, Bout, bank, Bb, m=128):
            P.op("act", lambda e: e.activation(out=out_ap, in_=bank[0:m, :], func=AF.Silu), [Bb], Bout)

        def outproj(w_ap, tile4):
            for sub in range(4):
                t = tile4 * 4 + sub
                for dh in range(2):
                    bank, Bb = gbank()
                    for f in range(8):
                        P.op("pe", lambda e, f=f, sub=sub, dh=dh, bank=bank: e.matmul(bank[:], lhsT=og[:, f, sub * 128:(sub + 1) * 128], rhs=wo[:, f, dh * 512:(dh + 1) * 512], start=(f == 0), stop=(f == 7)), [Bog[f], Bwo[dh]], [Bb])
                    P.op("dve", lambda e, t=t, dh=dh, bank=bank: e.tensor_tensor(out=h[:, t, dh * 512:(dh + 1) * 512], in0=bank[:], in1=h[:, t, dh * 512:(dh + 1) * 512], op=ALU.add), [Bb, Bh[t]], [Bh[t]])

        def load_wo(w_ap):
            for dh in range(2):
                P.dma("pool", lambda e, dh=dh: e.dma_start(out=wo[:, :, dh * 512:(dh + 1) * 512], in_=w_ap.rearrange("(kc p) f -> p kc f", p=128)[:, :, dh * 512:(dh + 1) * 512]), [], [Bwo[dh]], Bwo[dh])

        def zero_state():
            for hd in range(8):
                P.op("dve", lambda e, hd=hd: e.memset(Sst[:, hd, :], 0.0), [], [BS[hd]])

        def rotary_tables(b, tok0):
            P.dma("sp", lambda e: e.dma_start(out=posi[:], in_=pos_d[b:b + 1, tok0:tok0 + 512].partition_broadcast(128)), [], [Bposi], Bposi)
            ang, Bang = gwk()
            tf, Btf = gwk()
            ti = tf[:].bitcast(I32)
            P.op("dve", lambda e: e.tensor_copy(out=ang[:], in_=posi[:]), [Bposi], [Bang])
            P.op("dve", lambda e: e.tensor_scalar(out=ang[:], in0=ang[:], scalar1=C(C_INVF), scalar2=None, op0=ALU.mult), [Bang, Bcst], [Bang])
            for k, shift in ((1, 0.0), (0, float(np.pi / 2))):
                R = cs[:, k, :]
                P.op("dve", lambda e, shift=shift: e.tensor_scalar(out=tf[:], in0=ang[:], scalar1=shift, scalar2=float(1 / (2 * np.pi)), op0=ALU.add, op1=ALU.mult), [Bang], [Btf])
                sc, Bsc = gwk()
                P.op("dve", lambda e, sc=sc: e.tensor_copy(out=sc[:].bitcast(I32), in_=tf[:]), [Btf], [Bsc])
                P.op("dve", lambda e, sc=sc: e.tensor_copy(out=tf[:], in_=sc[:].bitcast(I32)), [Bsc], [Btf])
                P.op("dve", lambda e, R=R: e.scalar_tensor_tensor(out=R, in0=tf[:], scalar=float(-2 * np.pi), in1=ang[:], op0=ALU.mult, op1=ALU.add), [Btf, Bang], [Bcs])
                if shift != 0.0:
                    P.op("dve", lambda e, R=R, shift=shift: e.tensor_scalar_add(out=R, in0=R, scalar1=shift), [Bcs], [Bcs])
                P.op("dve", lambda e, R=R: e.tensor_single_scalar(out=tf[:], in_=R, scalar=float(np.pi), op=ALU.is_gt), [Bcs], [Btf])
                P.op("dve", lambda e, R=R: e.scalar_tensor_tensor(out=R, in0=tf[:], scalar=float(-2 * np.pi), in1=R, op0=ALU.mult, op1=ALU.add), [Btf, Bcs], [Bcs])
                P.op("dve", lambda e, R=R: e.tensor_single_scalar(out=tf[:], in_=R, scalar=float(-np.pi), op=ALU.is_lt), [Bcs], [Btf])
                P.op("dve", lambda e, R=R: e.scalar_tensor_tensor(out=R, in0=tf[:], scalar=float(2 * np.pi), in1=R, op0=ALU.mult, op1=ALU.add), [Btf, Bcs], [Bcs])
                P.op("act", lambda e, R=R: e.activation(out=R, in_=R, func=AF.Sin), [Bcs], [Bcs])
            P.op("dve", lambda e: e.tensor_scalar(out=cs[:, 1, :], in0=cs[:, 1, :], scalar1=C(C_SIGN), scalar2=None, op0=ALU.mult), [Bcs, Bcst], [Bcs])

        hctr = [0]
        dbg_toks = []
        pend = [None]

        def even_mixer(b, l):
            e_ = l // 2
            win_ap = W["ab_w_in"][e_]
            load_wo(W["ab_w_out"][e_])
            zero_state()
            for tile4 in range(4):
                tok0 = tile4 * 512
                rotary_tables(b, tok0)
                for hd in range(8):
                    s = hctr[0] % 2
                    hctr[0] += 1
                    if hd >= 4 and DBG_FLIP:
                        s = 1 - s
                    Bw = Bwin[s]
                    if hd < 4:
                        hh = hd
                        cq, ck, cv, cg = hh * 128, 512 + hh * 128, 1024 + hh * 128, 1536 + hh * 128
                        load_cols(s, 0, win_ap, cq, 128, 0)
                        load_cols(s, 1, win_ap, cq + 64, 64, 128)
                        load_cols(s, 2, win_ap, cq, 64, 192)
                        load_cols(s, 3, win_ap, ck, 128, 256)
                        load_cols(s, 4, win_ap, ck + 64, 64, 384)
                        load_cols(s, 5, win_ap, ck, 64, 448)
                        load_cols(s, 6, win_ap, cv, 128, 512)
                        load_cols(s, 7, win_ap, cg, 128, 640)
                        vproj_a(s, 512, tok0)
                        for (o0, dst, Bdst, dtab) in ((0, qh[s], Bqh[s], C_DQ), (256, kh[s], Bkh[s], C_DK)):
                            b1, Bb1 = gbank()
                            proj_fm(b1, Bb1, win[s], Bw, o0, 128, tok0)
                            b2, Bb2 = gbank()
                            proj_fm(b2, Bb2, win[s], Bw, o0 + 128, 128, tok0)
                            t1, Bt1 = gwk()
                            t2, Bt2 = gwk()
                            P.op("dve", lambda e, b1=b1, t1=t1: e.tensor_tensor(out=t1[:], in0=b1[:], in1=cs[:, 0, :], op=ALU.mult), [Bb1, Bcs], [Bt1])
                            P.op("dve", lambda e, b2=b2, t2=t2: e.tensor_tensor(out=t2[:], in0=b2[:], in1=cs[:, 1, :], op=ALU.mult), [Bb2, Bcs], [Bt2])
                            P.op("pool", lambda e, t1=t1, t2=t2: e.tensor_tensor(out=t1[:], in0=t1[:], in1=t2[:], op=ALU.add), [Bt1, Bt2], [Bt1])
                            P.op("dve", lambda e, t1=t1, dst=dst, dtab=dtab, hh=hh: e.tensor_tensor(out=dst[:].rearrange("p (c i) -> p c i", c=8), in0=t1[:].rearrange("p (c i) -> p c i", c=8), in1=cst[:, dtab + hh * 64:dtab + (hh + 1) * 64].unsqueeze(1).to_broadcast([128, 8, 64]), op=ALU.mult), [Bt1, Bcst], [Bdst])
                        bg, Bbg = gbank()
                        proj_fm(bg, Bbg, win[s], Bw, 640, 128, tok0)
                        silu_to(gsl[s][:], [Bgsl[s]], bg, Bbg)
                        vproj_b(s)
                        if pend[0] is not None:
                            core_b(*pend[0])
                        core_a(s, hd, 128, RET_A[hh])
                        pend[0] = (s, hd, 128, 0 + e_)
                    else:
                        hh = hd - 4
                        cq, ck, cv, cg = 2048 + hh * 64, 2304 + hh * 64, 2560 + hh * 128, 3072 + hh * 128
                        load_cols(s, 0, win_ap, cq, 64, 0)
                        load_cols(s, 1, win_ap, ck, 64, 64)
                        if hh == 0:
                            load_cols(s, 2, win_ap, 3584, 16, 128)
                        load_cols(s, 6, win_ap, cv, 128, 512)
                        load_cols(s, 7, win_ap, cg, 128, 640)
                        vproj_a(s, 512, tok0)
                        if hh == 0:
                            bb, Bbb = gbank()
                            proj_fm(bb, Bbb, win[s], Bw, 128, 16, tok0)
                            P.op("act", lambda e, bb=bb: e.copy(out=bab[:], in_=bb[0:16, :]), [Bbb], [Bbab])
                        bx, Bbx = gbank()
                        P.op("pe", lambda e, bx=bx, hh=hh: e.matmul(bx[0:64, :], lhsT=wa2[:, e_, hh * 64:(hh + 1) * 64], rhs=bab[:], start=True, stop=True), [Bwa2, Bbab], [Bbx])
                        L, BL = gwk()
                        P.op("act", lambda e, bx=bx, L=L, hh=hh: e.activation(out=L[0:64, :], in_=bx[0:64, :], func=AF.Exp, scale=-1.0, bias=ba2[:, e_, hh:hh + 1]), [Bbx, Bba2], [BL])
                        P.op("act", lambda e, L=L: e.activation(out=L[0:64, :], in_=L[0:64, :], func=AF.Ln, scale=1.0, bias=C(C_ONE, rows=64)), [BL, Bcst], [BL])
                        G, BG = gwk()
                        P.op("dve", lambda e, L=L, G=G: e.tensor_tensor_scan(out=G[0:64, :], data0=cst[0:64, C_RESET:C_RESET + 512], data1=L[0:64, :], initial=0.0, op0=ALU.mult, op1=ALU.add), [BL, Bcst], [BG])
                        E1, BE1 = gwk()
                        P.op("act", lambda e, G=G, E1=E1: e.activation(out=E1[0:64, :], in_=G[0:64, :], func=AF.Exp, scale=-1.0 / 16.0), [BG], [BE1])
                        P.op("act", lambda e, G=G, s=s: e.activation(out=acol[s][0:64, :], in_=G[0:64, :].rearrange("p (c i) -> p c i", c=8)[:, :, 63], func=AF.Exp, scale=-1.0 / 16.0), [BG], [Bacol[s]])
                        P.op("act", lambda e, G=G: e.activation(out=G[0:64, :], in_=G[0:64, :], func=AF.Exp, scale=1.0 / 16.0), [BG], [BG])
                        bq, Bbq = gbank()
                        proj_fm(bq, Bbq, win[s], Bw, 0, 64, tok0)
                        P.op("dve", lambda e, bq=bq, E1=E1, s=s: e.scalar_tensor_tensor(out=qh[s][0:64, :], in0=bq[0:64, :], scalar=0.125, in1=E1[0:64, :], op0=ALU.mult, op1=ALU.mult), [Bbq, BE1], [Bqh[s]])
                        bk, Bbk = gbank()
                        proj_fm(bk, Bbk, win[s], Bw, 64, 64, tok0)
                        P.op("dve", lambda e, bk=bk, G=G, s=s: e.tensor_tensor(out=kh[s][0:64, :], in0=bk[0:64, :], in1=G[0:64, :], op=ALU.mult), [Bbk, BG], [Bkh[s]])
                        bg, Bbg = gbank()
                        proj_fm(bg, Bbg, win[s], Bw, 640, 128, tok0)
                        silu_to(gsl[s][:], [Bgsl[s]], bg, Bbg)
                        vproj_b(s)
                        if pend[0] is not None:
                            core_b(*pend[0])
                        core_a(s, hd, 64, None)
                        pend[0] = (s, hd, 64, 2 + e_)
                core_b(*pend[0])
                pend[0] = None
                outproj(W["ab_w_out"][e_], tile4)

        def odd_mixer(b, l):
            e_ = l // 2
            win_ap = W["c_w_in"][e_]
            load_wo(W["c_w_out"][e_])
            zero_state()
            for tile4 in range(4):
                tok0 = tile4 * 512
                for hd in range(8):
                    s = hctr[0] % 2
                    hctr[0] += 1
                    Bw = Bwin[s]
                    load_cols(s, 0, win_ap, hd * 128, 128, 0)
                    load_cols(s, 1, win_ap, 1024 + hd * 128, 128, 128)
                    load_cols(s, 6, win_ap, 2048 + hd * 128, 128, 512)
                    load_cols(s, 7, win_ap, 3072 + hd * 128, 128, 640)
                    vproj_a(s, 512, tok0)
                    bf_, Bbf = gbank()
                    proj_fm(bf_, Bbf, win[s], Bw, 128, 128, tok0)
                    sg, Bsg = gwk()
                    P.op("act", lambda e, bf_=bf_, sg=sg: e.activation(out=sg[:], in_=bf_[:], func=AF.Exp, scale=-1.0), [Bbf], [Bsg])
                    P.op("dve", lambda e, sg=sg: e.tensor_scalar_add(out=sg[:], in0=sg[:], scalar1=1.0), [Bsg], [Bsg])
                    P.op("dve", lambda e, sg=sg: e.reciprocal(out=sg[:], in_=sg[:]), [Bsg], [Bsg])
                    G, BG = gwk()
                    P.op("dve", lambda e, sg=sg, G=G, hd=hd: e.tensor_scalar(out=G[:], in0=sg[:], scalar1=oml[:, l, hd:hd + 1], scalar2=lbt[:, l, hd:hd + 1], op0=ALU.mult, op1=ALU.add), [Bsg, Blbt], [BG])
                    P.op("act", lambda e, G=G: e.activation(out=G[:], in_=G[:], func=AF.Ln), [BG], [BG])
                    G2, BG2 = gwk()
                    P.op("dve", lambda e, G=G, G2=G2: e.tensor_tensor_scan(out=G2[:], data0=cst[:, C_RESET:C_RESET + 512], data1=G[:], initial=0.0, op0=ALU.mult, op1=ALU.add), [BG, Bcst], [BG2])
                    P.op("dve", lambda e, sg=sg, G=G, hd=hd: e.tensor_scalar(out=G[:], in0=sg[:], scalar1=noml[:, l, hd:hd + 1], scalar2=oml[:, l, hd:hd + 1], op0=ALU.mult, op1=ALU.add), [Bsg, Blbt], [BG])
                    P.op("act", lambda e, G2=G2, s=s: e.activation(out=acol[s][:], in_=G2[:].rearrange("p (c i) -> p c i", c=8)[:, :, 63], func=AF.Exp), [BG2], [Bacol[s]])
                    P.op("act", lambda e, G2=G2, sg=sg: e.activation(out=sg[:], in_=G2[:], func=AF.Exp, scale=-1.0), [BG2], [Bsg])
                    P.op("dve", lambda e, sg=sg, G=G, s=s: e.tensor_tensor(out=kh[s][:], in0=G[:], in1=sg[:], op=ALU.mult), [BG, Bsg], [Bkh[s]])
                    P.op("act", lambda e, G2=G2: e.activation(out=G2[:], in_=G2[:], func=AF.Exp), [BG2], [BG2])
                    bq, Bbq = gbank()
                    proj_fm(bq, Bbq, win[s], Bw, 0, 128, tok0)
                    qs, Bqs = gwk()
                    silu_to(qs[:], [Bqs], bq, Bbq)
                    P.op("dve", lambda e, qs=qs, G2=G2, s=s: e.scalar_tensor_tensor(out=qh[s][:], in0=qs[:], scalar=float(128.0 ** -0.5), in1=G2[:], op0=ALU.mult, op1=ALU.mult), [Bqs, BG2], [Bqh[s]])
                    bg, Bbg = gbank()
                    proj_fm(bg, Bbg, win[s], Bw, 640, 128, tok0)
                    silu_to(gsl[s][:], [Bgsl[s]], bg, Bbg)
                    vproj_b(s)
                    if pend[0] is not None:
                        core_b(*pend[0])
                    core_a(s, hd, 128, None)
                    pend[0] = (s, hd, 128, 4 + e_)
                core_b(*pend[0])
                pend[0] = None
                outproj(W["c_w_out"][e_], tile4)

        fctr = [0]
        dctr = [0]

        def swiglu_expert(wg_ap, wu_ap, wd_ap, F, gate_col):
            nchunks = F // 128
            c0 = 0
            while c0 < nchunks:
                ncg = min(4, nchunks - c0)
                s = fctr[0] % 2
                fctr[0] += 1
                f0 = c0 * 128
                nf = ncg * 128
                P.dma("pool", lambda e, s=s, f0=f0, nf=nf: e.dma_start(out=wg[s][:, :, 0:nf], in_=wg_ap.rearrange("(kc p) f -> p kc f", p=128)[:, :, f0:f0 + nf]), [], [Bwg[s]], Bwg[s])
                P.dma("pool", lambda e, s=s, f0=f0, nf=nf: e.dma_start(out=wu[s][:, :, 0:nf], in_=wu_ap.rearrange("(kc p) f -> p kc f", p=128)[:, :, f0:f0 + nf]), [], [Bwu[s]], Bwu[s])
                P.dma("pool", lambda e, s=s, f0=f0, ncg=ncg: e.dma_start(out=wd[s][:, 0:ncg, :], in_=wd_ap[f0:f0 + ncg * 128, :].rearrange("(fc p) d -> p fc d", p=128)), [], [Bwd[s]], Bwd[s])
                for tg in range(4):
                    tok0 = tg * 512
                    hs = (fctr[0] * 4 + tg) % 2
                    for fc in range(ncg):
                        Pg, BPg = pb[0 + (fc % 2)], Bpb[0 + (fc % 2)]
                        Pu, BPu = pb[2 + (fc % 2)], Bpb[2 + (fc % 2)]
                        for kc in range(8):
                            P.op("pe", lambda e, kc=kc, fc=fc, Pg=Pg, s=s, tok0=tok0: e.matmul(Pg[:], lhsT=wg[s][:, kc, fc * 128:(fc + 1) * 128], rhs=hnT[:, kc, tok0:tok0 + 512], start=(kc == 0), stop=(kc == 7)), [Bwg[s]] + BhnT[tg * 4:tg * 4 + 4], [BPg])
                        for kc in range(8):
                            P.op("pe", lambda e, kc=kc, fc=fc, Pu=Pu, s=s, tok0=tok0: e.matmul(Pu[:], lhsT=wu[s][:, kc, fc * 128:(fc + 1) * 128], rhs=hnT[:, kc, tok0:tok0 + 512], start=(kc == 0), stop=(kc == 7)), [Bwu[s]] + BhnT[tg * 4:tg * 4 + 4], [BPu])
                        sg, Bsg = gwk()
                        P.op("act", lambda e, Pg=Pg, sg=sg: e.activation(out=sg[:], in_=Pg[:], func=AF.Silu), [BPg], [Bsg])
                        P.op("dve", lambda e, Pu=Pu, sg=sg, hs=hs, fc=fc: e.tensor_tensor(out=hid[hs][:, fc, :], in0=Pu[:], in1=sg[:], op=ALU.mult), [BPu, Bsg], [Bhid[hs][fc]])
                    for sub in range(4):
                        t = tg * 4 + sub
                        for dh in range(2):
                            k = 4 + (dctr[0] % 4)
                            dctr[0] += 1
                            Pd, BPd = pb[k], Bpb[k]
                            for fc in range(ncg):
                                P.op("pe", lambda e, fc=fc, sub=sub, dh=dh, Pd=Pd, s=s, hs=hs: e.matmul(Pd[:], lhsT=hid[hs][:, fc, sub * 128:(sub + 1) * 128], rhs=wd[s][:, fc, dh * 512:(dh + 1) * 512], start=(fc == 0), stop=(fc == ncg - 1)), [Bhid[hs][fc], Bwd[s]], [BPd])
                            hsl = h[:, t, dh * 512:(dh + 1) * 512]
                            if gate_col is None:
                                P.op("dve", lambda e, Pd=Pd, hsl=hsl: e.tensor_tensor(out=hsl, in0=Pd[:], in1=hsl, op=ALU.add), [BPd, Bh[t]], [Bh[t]])
                            else:
                                P.op("dve", lambda e, Pd=Pd, hsl=hsl, t=t: e.scalar_tensor_tensor(out=hsl, in0=Pd[:], scalar=gt[:, t, gate_col:gate_col + 1], in1=hsl, op0=ALU.mult, op1=ALU.add), [BPd, Bh[t], Bgt], [Bh[t]])
                c0 += ncg

        def router(e_):
            P.dma("pool", lambda e: e.dma_start(out=wr[:], in_=W["moe_router"][e_].rearrange("(kc p) n -> p kc n", p=128)), [], [Bwr], Bwr)
            for t in range(NT):
                bank, Bb = gbank()
                for kc in range(8):
                    P.op("pe", lambda e, kc=kc, t=t, bank=bank: e.matmul(bank[:, 0:8], lhsT=hnT[:, kc, t * 128:(t + 1) * 128], rhs=wr[:, kc, :], start=(kc == 0), stop=(kc == 7)), [BhnT[t], Bwr], [Bb])
                P.op("act", lambda e, t=t, bank=bank: e.copy(out=lg[:, t, :], in_=bank[:, 0:8]), [Bb], [Blg])
            m1 = stat[:, 32:48]
            m2 = stat[:, 48:64]

            def bc(a):
                return a.unsqueeze(2).to_broadcast([128, NT, 8])
            P.op("dve", lambda e: e.tensor_reduce(out=m1, in_=lg[:], axis=AX.X, op=ALU.max), [Blg], [Bstat])
            P.op("dve", lambda e: e.tensor_tensor(out=rt[0][:], in0=lg[:], in1=bc(m1), op=ALU.is_equal), [Blg, Bstat], [Brt[0]])
            P.op("dve", lambda e: e.scalar_tensor_tensor(out=rt[1][:], in0=rt[0][:], scalar=-1e30, in1=lg[:], op0=ALU.mult, op1=ALU.add), [Brt[0], Blg], [Brt[1]])
            P.op("dve", lambda e: e.tensor_reduce(out=m2, in_=rt[1][:], axis=AX.X, op=ALU.max), [Brt[1]], [Bstat])
            P.op("dve", lambda e: e.tensor_tensor(out=rt[0][:], in0=lg[:], in1=bc(m2), op=ALU.is_ge), [Blg, Bstat], [Brt[0]])
            P.op("dve", lambda e: e.tensor_tensor(out=rt[1][:], in0=lg[:], in1=bc(m1), op=ALU.subtract), [Blg, Bstat], [Brt[1]])
            P.op("act", lambda e: e.activation(out=rt[1][:], in_=rt[1][:], func=AF.Exp), [Brt[1]], [Brt[1]])
            P.op("dve", lambda e: e.tensor_tensor(out=rt[1][:], in0=rt[1][:], in1=rt[0][:], op=ALU.mult), [Brt[0], Brt[1]], [Brt[1]])
            P.op("dve", lambda e: e.tensor_reduce(out=m1, in_=rt[1][:], axis=AX.X, op=ALU.add), [Brt[1]], [Bstat])
            P.op("dve", lambda e: e.reciprocal(out=m1, in_=m1), [Bstat], [Bstat])
            P.op("dve", lambda e: e.tensor_tensor(out=gt[:], in0=rt[1][:], in1=bc(m1), op=ALU.mult), [Brt[1], Bstat], [Bgt])

        final_toks = []
        for b in range(nseq):
            for q4 in range(4):
                P.dma("sp", lambda e, b=b, q4=q4: e.dma_start(out=h[:, q4 * 4:(q4 + 1) * 4, :], in_=x_d[b, q4 * 512:(q4 + 1) * 512, :].rearrange("(t p) d -> p t d", p=128)), [], Bh[q4 * 4:q4 * 4 + 4], Bh[q4 * 4])
            sub = 0
            for l in range(4):
                e_ = l // 2
                if sub < nsub:
                    norm(W["norm_mix_g"][l:l + 1, :])
                    alias_sync(ffn_bufs(), mixer_bufs())
                    if l % 2 == 0:
                        even_mixer(b, l)
                    else:
                        odd_mixer(b, l)
                sub += 1
                if sub < nsub:
                    norm(W["norm_ffn_g"][l:l + 1, :])
                    alias_sync(mixer_bufs(), ffn_bufs())
                    if l % 2 == 0:
                        swiglu_expert(W["ffn_w_gate"][e_], W["ffn_w_up"][e_], W["ffn_w_down"][e_], 2816, None)
                    else:
                        router(e_)
                        for ex in range(8):
                            swiglu_expert(W["moe_w_gate"][e_, ex], W["moe_w_up"][e_, ex], W["moe_w_down"][e_, ex], 3584, ex)
                sub += 1
            if final:
                final_toks = norm(W["final_norm_g"].rearrange("(o d) -> o d", o=1), to_out=out_d[b])
            else:
                final_toks = []
                for t in range(NT):
                    final_toks.append(P.dma("sp", lambda e, t=t, b=b: e.dma_start(out=out_d[b, t * 128:(t + 1) * 128, :], in_=h[:, t, :]), [Bh[t]], [], Bh[t]))
        P.emit(final_toks + dbg_toks)
        print("instr counts", {k: len(v) for k, v in P.q.items()}, "sems", P.nsem, flush=True)
    return nc


_NC_CACHE = {}


def run(inputs, nseq_per_core, nsub=8, final=True, ncores=8):
    key = (nseq_per_core, nsub, final)
    if key not in _NC_CACHE:
        _NC_CACHE[key] = build(nseq_per_core, nsub, final)
    nc = _NC_CACHE[key]
    consts = make_consts()
    in_maps = []
    for c in range(ncores):
        m = {n: np.ascontiguousarray(inputs[n], dtype=np.float32) for n, _ in WNAMES}
        m["x"] = np.ascontiguousarray(inputs["x"][c * nseq_per_core:(c + 1) * nseq_per_core], dtype=np.float32)
        m["positions"] = np.ascontiguousarray(inputs["positions"][c * nseq_per_core:(c + 1) * nseq_per_core], dtype=np.int32)
        m["consts"] = consts
        in_maps.append(m)
    res = run_bass_kernel_spmd(nc, in_maps, core_ids=list(range(ncores)))
    if DBG:
        np.save("dbg_out.npy", res.results[0]["dbg"])
    return np.concatenate([r["out"] for r in res.results], axis=0)


def kernel(**inputs):
    return run(inputs, 4).astype(np.float32)
```

```python
import numpy as np
from contextlib import ExitStack
import concourse.bass as bass
import concourse.mybir as mybir
from concourse.bass_utils import run_bass_kernel_spmd

F32 = mybir.dt.float32
BF16 = mybir.dt.bfloat16
I32 = mybir.dt.int32
AF = mybir.ActivationFunctionType
ALU = mybir.AluOpType
AX = mybir.AxisListType

D = 1024
DBG_FLIP = False
DBG = False
ONLY = None
T = 2048
NT = 16
EPS = 1e-6


class Tok:
    __slots__ = ("sem", "val", "eng")

    def __init__(self, sem, val, eng):
        self.sem = sem
        self.val = val
        self.eng = eng


class Buf:
    __slots__ = ("name", "w", "r", "dsem", "dcnt")

    def __init__(self, name):
        self.name = name
        self.w = None
        self.r = {}
        self.dsem = None
        self.dcnt = 0


class Prog:
    ENGS = ("pe", "act", "dve", "pool", "sp")

    def __init__(self, nc, stack):
        self.nc = nc
        self.stack = stack
        self.q = {e: [] for e in self.ENGS}
        self.cnt = {e: 0 for e in self.ENGS}
        self.sem = {e: stack.enter_context(nc.semaphore("s_" + e)) for e in ("pe", "act", "dve", "pool")}
        self.waited = {e: {} for e in self.ENGS}
        self.nsem = 4
        self.last_dma = None

    def _deps(self, eng, reads, writes):
        need = {}

        def add(t):
            if t is None:
                return
            if t.eng == "pe" and eng == "pe":
                return
            k = id(t.sem)
            if k not in need or need[k][1] < t.val:
                need[k] = (t.sem, t.val)

        for b in reads:
            add(b.w)
        for b in writes:
            add(b.w)
            for t in b.r.values():
                add(t)
        wd = self.waited[eng]
        for k, (sem, val) in need.items():
            if wd.get(k, -1) >= val:
                continue
            wd[k] = val
            self.q[eng].append(("wait", sem, val))

    def _mark(self, tok, reads, writes):
        for b in reads:
            b.r[id(tok.sem)] = tok
        for b in writes:
            b.w = tok
            b.r = {}

    def op(self, eng, fn, reads=(), writes=()):
        self._deps(eng, reads, writes)
        self.cnt[eng] += 1
        tok = Tok(self.sem[eng], self.cnt[eng], eng)
        self.q[eng].append(("op", fn, self.sem[eng], 1))
        self._mark(tok, reads, writes)
        return tok

    def dma(self, qeng, fn, reads, writes, primary):
        self._deps(qeng, reads, writes)
        if primary.dsem is None:
            primary.dsem = self.stack.enter_context(self.nc.semaphore("d_" + primary.name))
            self.nsem += 1
        primary.dcnt += 16
        tok = Tok(primary.dsem, primary.dcnt, None)
        self.q[qeng].append(("op", fn, primary.dsem, 16))
        self._mark(tok, reads, writes)
        return tok

    def emit(self, final_toks):
        nc = self.nc
        for t in final_toks:
            self.q["sp"].append(("wait", t.sem, t.val))

        def run(e, items):
            for it in items:
                if it[0] == "wait":
                    e.wait_ge(it[1], it[2])
                else:
                    it[1](e).then_inc(it[2], it[3])

        with nc.Block() as block:
            @block.tensor
            def _(e):
                run(e, self.q["pe"])

            @block.scalar
            def _(e):
                run(e, self.q["act"])

            @block.vector
            def _(e):
                run(e, self.q["dve"])

            @block.gpsimd
            def _(e):
                run(e, self.q["pool"])

            @block.sync
            def _(e):
                run(e, self.q["sp"])


C_ID = 0
C_ONES = 128
C_MASK = 256
C_RESET = 320
C_INVF = 832
C_SIGN = 833
C_EPS = 834
C_ONE = 835
C_NEGPI = 836
C_DQ = 840
C_DK = 840 + 256
NCONST = 840 + 512


def make_consts():
    c = np.zeros((128, NCONST), np.float32)
    c[:, C_ID:C_ID + 128] = np.eye(128, dtype=np.float32)
    c[:, C_ONES:C_ONES + 128] = 1.0
    j = np.arange(64)[:, None]
    i = np.arange(64)[None, :]
    c[0:64, C_MASK:C_MASK + 64] = (j <= i).astype(np.float32)
    r = np.ones(512, np.float32)
    r[::64] = 0.0
    c[:, C_RESET:C_RESET + 512] = r[None, :]
    p = np.arange(128)
    c[:, C_INVF] = (10000.0 ** (-(2.0 * (p % 64)) / 128.0)).astype(np.float32)
    c[:, C_SIGN] = np.where(p < 64, -1.0, 1.0)
    c[:, C_EPS] = EPS
    c[:, C_ONE] = 1.0
    c[:, C_NEGPI] = -np.pi
    for hh in range(4):
        lg = np.log1p(-np.exp2(-5.0 - hh))
        t = np.arange(64, dtype=np.float64)
        c[:, C_DQ + hh * 64:C_DQ + (hh + 1) * 64] = np.exp((t + 1.0) * lg)[None, :]
        c[:, C_DK + hh * 64:C_DK + (hh + 1) * 64] = (np.exp(-(t + 1.0) * lg) * 128.0 ** -0.5)[None, :]
    return c


RET_A = [float(np.exp(64.0 * np.log1p(-np.exp2(-5.0 - hh)))) for hh in range(4)]

WNAMES = [("norm_mix_g", [4, 1024]), ("norm_ffn_g", [4, 1024]), ("final_norm_g", [1024]),
          ("ab_w_in", [2, 1024, 3600]), ("gla_w_a2", [2, 16, 256]), ("gla_b_a2", [2, 256]),
          ("ret_norm_g", [2, 128]), ("gla_norm_g", [2, 128]), ("ab_w_out", [2, 1024, 1024]),
          ("ffn_w_gate", [2, 1024, 2816]), ("ffn_w_up", [2, 1024, 2816]), ("ffn_w_down", [2, 2816, 1024]),
          ("hgrn_lb_logits", [4, 1024]), ("c_w_in", [2, 1024, 4096]), ("hgrn_norm_g", [2, 128]),
          ("c_w_out", [2, 1024, 1024]), ("moe_router", [2, 1024, 8]), ("moe_w_gate", [2, 8, 1024, 3584]),
          ("moe_w_up", [2, 8, 1024, 3584]), ("moe_w_down", [2, 8, 3584, 1024])]


def build(nseq, nsub=8, final=True):
    nc = bass.Bass("TRN2", target_bir_lowering=False)
    x_d = nc.dram_tensor("x", [nseq, T, D], F32, kind="ExternalInput").ap()
    pos_d = nc.dram_tensor("positions", [nseq, T], I32, kind="ExternalInput").ap()
    W = {n: nc.dram_tensor(n, s, F32, kind="ExternalInput").ap() for n, s in WNAMES}
    cst_d = nc.dram_tensor("consts", [128, NCONST], F32, kind="ExternalInput").ap()
    out_d = nc.dram_tensor("out", [nseq, T, D], F32, kind="ExternalOutput").ap()
    dbg_d = nc.dram_tensor("dbg", [128, 8, 512], F32, kind="ExternalOutput").ap() if DBG else None

    with ExitStack() as st:
        P = Prog(nc, st)

        def sb(n, s, d):
            return st.enter_context(nc.sbuf_tensor(n, s, d))

        h = sb("h", [128, NT, D], F32)
        Bh = [Buf("h%d" % t) for t in range(NT)]
        hnT = sb("hnT", [128, 8, T], BF16)
        BhnT = [Buf("hnT%d" % t) for t in range(NT)]
        cst = sb("cst", [128, NCONST], F32)
        Bcst = Buf("cst")
        idb = sb("idb", [128, 128], BF16)
        Bidb = Buf("idb")
        gbc = sb("gbc", [128, D], F32)
        Bgbc = Buf("gbc")
        hntok = [sb("hntok%d" % i, [128, D], BF16) for i in range(2)]
        Bhntok = [Buf("hntok%d" % i) for i in range(2)]
        stat = sb("stat", [128, 64], F32)
        Bstat = Buf("stat")
        junk = hntok[1]
        Bjunk = Bhntok[1]
        lbt = sb("lbt", [128, 4, 8], F32)
        Blbt = Buf("lbt")
        oml = sb("oml", [128, 4, 8], F32)
        noml = sb("noml", [128, 4, 8], F32)
        hng = sb("hng", [128, 8], F32)
        Bhng = Buf("hng")
        ba2 = sb("ba2", [64, 2, 4], F32)
        Bba2 = Buf("ba2")
        wa2f = sb("wa2f", [16, 2, 256], F32)
        wa2 = sb("wa2", [16, 2, 256], BF16)
        Bwa2 = Buf("wa2")
        NW = 6
        wk = [sb("wk%d" % i, [128, 512], F32) for i in range(NW)]
        Bwk = [Buf("wk%d" % i) for i in range(NW)]
        arena = sb("arena", [128, 24576], BF16)
        win = [arena[:, i * 6144:(i + 1) * 6144].rearrange("p (k f) -> p k f", k=8) for i in range(2)]
        Bwin = [[Buf("win%d_%d" % (i, j)) for j in range(8)] for i in range(2)]
        qh = [sb("qh%d" % i, [128, 512], BF16) for i in range(2)]
        Bqh = [Buf("qh%d" % i) for i in range(2)]
        kh = [sb("kh%d" % i, [128, 512], BF16) for i in range(2)]
        Bkh = [Buf("kh%d" % i) for i in range(2)]
        vt = [sb("vt%d" % i, [64, 8, 128], BF16) for i in range(2)]
        Bvt = [Buf("vt%d" % i) for i in range(2)]
        gsl = [sb("gsl%d" % i, [128, 512], BF16) for i in range(2)]
        Bgsl = [Buf("gsl%d" % i) for i in range(2)]
        acol = [sb("acol%d" % i, [128, 8], F32) for i in range(2)]
        Bacol = [Buf("acol%d" % i) for i in range(2)]
        bab = sb("bab", [16, 512], BF16)
        Bbab = Buf("bab")
        Sst = sb("Sst", [128, 8, 128], F32)
        arena2 = sb("arena2", [128, 4096], BF16)
        Sbf = arena2[:, 3072:4096].rearrange("p (k f) -> p k f", k=8)
        BS = [Buf("S%d" % i) for i in range(8)]
        BSb = [Buf("Sb%d" % i) for i in range(8)]
        Sa = sb("Sa", [128, 128], F32)
        BSa = Buf("Sa")
        pTall = [sb("pTall%d" % i, [64, 8, 64], BF16) for i in range(2)]
        BpTall = [Buf("pTall%d" % i) for i in range(2)]
        ktkall = [sb("ktkall%d" % i, [64, 8, 128], BF16) for i in range(2)]
        Bktkall = [Buf("ktkall%d" % i) for i in range(2)]
        Sball = [arena2[:, 3072:4096].rearrange("p (k f) -> p k f", k=8), sb("Sball1", [128, 8, 128], BF16)]
        BSball = [Buf("Sball%d" % i) for i in range(2)]
        og = arena[:, 20480:24576].rearrange("p (k f) -> p k f", k=8)
        Bog = [Buf("og%d" % i) for i in range(8)]
        cs = arena2[:, 0:2048].bitcast(F32).rearrange("p (k f) -> p k f", k=2)
        Bcs = Buf("cs")
        posi = arena2[:, 2048:3072].bitcast(I32)
        Bposi = Buf("posi")
        wo = arena[:, 12288:20480].rearrange("p (k f) -> p k f", k=8)
        Bwo = [Buf("wo0"), Buf("wo1")]
        wg = [arena[:, i * 4096:(i + 1) * 4096].rearrange("p (k f) -> p k f", k=8) for i in range(2)]
        wu = [arena[:, 8192 + i * 4096:8192 + (i + 1) * 4096].rearrange("p (k f) -> p k f", k=8) for i in range(2)]
        wd = [arena[:, 16384 + i * 4096:16384 + (i + 1) * 4096].rearrange("p (k f) -> p k f", k=4) for i in range(2)]
        Bwg = [Buf("wg%d" % i) for i in range(2)]
        Bwu = [Buf("wu%d" % i) for i in range(2)]
        Bwd = [Buf("wd%d" % i) for i in range(2)]
        hid = [arena2[:, i * 2048:(i + 1) * 2048].rearrange("p (k f) -> p k f", k=4) for i in range(2)]
        Bhid = [[Buf("hid%d_%d" % (i, j)) for j in range(4)] for i in range(2)]
        wr = sb("wr", [128, 8, 8], BF16)
        Bwr = Buf("wr")
        lg = sb("lg", [128, NT, 8], F32)
        Blg = Buf("lg")
        gt = sb("gt", [128, NT, 8], F32)
        Bgt = Buf("gt")
        rt = [sb("rt%d" % i, [128, NT, 8], F32) for i in range(3)]
        Brt = [Buf("rt%d" % i) for i in range(3)]

        def alias_sync(src, dst):
            toks = {}
            for b_ in src:
                for t_ in ([b_.w] if b_.w is not None else []) + list(b_.r.values()):
                    k_ = id(t_.sem)
                    if k_ not in toks or toks[k_].val < t_.val:
                        toks[k_] = t_
            for b_ in dst:
                for k_, t_ in toks.items():
                    if k_ not in b_.r or b_.r[k_].val < t_.val:
                        b_.r[k_] = t_

        def mixer_bufs():
            return [x_ for l_ in Bwin for x_ in l_] + Bwo + Bog + [Bcs, Bposi, BSball[0]]

        def ffn_bufs():
            return Bwg + Bwu + Bwd + [x_ for l_ in Bhid for x_ in l_]

        pb = [st.enter_context(nc.psum_tensor("pb%d" % i, [128, 512], F32)) for i in range(8)]
        Bpb = [Buf("pb%d" % i) for i in range(8)]
        gctr = [0]

        def gbank():
            i = gctr[0] % 4
            gctr[0] += 1
            return pb[i], Bpb[i]

        wctr = [0]

        def gwk():
            i = wctr[0] % NW
            wctr[0] += 1
            return wk[i], Bwk[i]

        def C(col, n=1, rows=128):
            return cst[0:rows, col:col + n]

        P.dma("sp", lambda e: e.dma_start(out=cst[:], in_=cst_d), [], [Bcst], Bcst)
        P.op("dve", lambda e: e.tensor_copy(out=idb[:], in_=cst[:, C_ID:C_ID + 128]), [Bcst], [Bidb])
        P.dma("sp", lambda e: e.dma_start(out=lbt[:], in_=W["hgrn_lb_logits"].rearrange("l (hh p) -> p l hh", p=128), allow_slow_non_contiguous=True), [], [Blbt], Blbt)
        P.op("act", lambda e: e.activation(out=lbt[:], in_=lbt[:], func=AF.Exp), [Blbt], [Blbt])
        P.op("dve", lambda e: e.tensor_tensor(out=stat[:, 0:8], in0=lbt[:, 0, :], in1=lbt[:, 1, :], op=ALU.add), [Blbt], [Bstat])
        P.op("dve", lambda e: e.tensor_tensor(out=stat[:, 0:8], in0=stat[:, 0:8], in1=lbt[:, 2, :], op=ALU.add), [Blbt, Bstat], [Bstat])
        P.op("dve", lambda e: e.tensor_tensor(out=stat[:, 0:8], in0=stat[:, 0:8], in1=lbt[:, 3, :], op=ALU.add), [Blbt, Bstat], [Bstat])
        P.op("dve", lambda e: e.reciprocal(out=stat[:, 0:8], in_=stat[:, 0:8]), [Bstat], [Bstat])
        for l in range(4):
            P.op("dve", lambda e, l=l: e.tensor_tensor(out=lbt[:, l, :], in0=lbt[:, l, :], in1=stat[:, 0:8], op=ALU.mult), [Blbt, Bstat], [Blbt])
        P.op("dve", lambda e: e.memset(lbt[:, 0, :], 0.0), [Blbt], [Blbt])
        P.op("dve", lambda e: e.tensor_tensor(out=lbt[:, 2, :], in0=lbt[:, 2, :], in1=lbt[:, 1, :], op=ALU.add), [Blbt], [Blbt])
        P.op("dve", lambda e: e.tensor_tensor(out=lbt[:, 3, :], in0=lbt[:, 3, :], in1=lbt[:, 2, :], op=ALU.add), [Blbt], [Blbt])
        P.op("dve", lambda e: e.tensor_scalar(out=oml[:], in0=lbt[:], scalar1=-1.0, scalar2=1.0, op0=ALU.mult, op1=ALU.add), [Blbt], [Blbt])
        P.op("dve", lambda e: e.tensor_scalar(out=noml[:], in0=oml[:], scalar1=-1.0, scalar2=None, op0=ALU.mult), [Blbt], [Blbt])
        P.dma("sp", lambda e: e.dma_start(out=hng[:, 0:2], in_=W["ret_norm_g"].rearrange("l p -> p l"), allow_slow_non_contiguous=True), [], [Bhng], Bhng)
        P.dma("sp", lambda e: e.dma_start(out=hng[:, 2:4], in_=W["gla_norm_g"].rearrange("l p -> p l"), allow_slow_non_contiguous=True), [], [Bhng], Bhng)
        P.dma("sp", lambda e: e.dma_start(out=hng[:, 4:6], in_=W["hgrn_norm_g"].rearrange("l p -> p l"), allow_slow_non_contiguous=True), [], [Bhng], Bhng)
        P.dma("sp", lambda e: e.dma_start(out=ba2[:], in_=W["gla_b_a2"].rearrange("l (hh p) -> p l hh", p=64), allow_slow_non_contiguous=True), [], [Bba2], Bba2)
        P.op("dve", lambda e: e.tensor_scalar(out=ba2[:], in0=ba2[:], scalar1=-1.0, scalar2=None, op0=ALU.mult), [Bba2], [Bba2])
        P.dma("sp", lambda e: e.dma_start(out=wa2f[:], in_=W["gla_w_a2"].rearrange("l r c -> r l c")), [], [Bwa2], Bwa2)
        P.op("dve", lambda e: e.tensor_copy(out=wa2[:], in_=wa2f[:]), [Bwa2], [Bwa2])

        def rstd_from_sumsq(ap, n, bufs):
            P.op("act", lambda e: e.activation(out=ap, in_=ap, func=AF.Ln, scale=1.0 / n, bias=C(C_EPS, rows=ap.shape[0])), bufs + [Bcst], bufs)
            P.op("act", lambda e: e.activation(out=ap, in_=ap, func=AF.Exp, scale=-0.5), bufs, bufs)

        def norm(g_row_ap, to_out=None):
            P.dma("sp", lambda e: e.dma_start(out=gbc[:], in_=g_row_ap.partition_broadcast(128)), [], [Bgbc], Bgbc)
            for t in range(NT):
                P.op("act", lambda e, t=t: e.activation(out=junk[:], in_=h[:, t, :], func=AF.Square, accum_out=stat[:, 16 + t:17 + t]), [Bh[t]], [Bjunk, Bstat])
            rstd_from_sumsq(stat[:, 16:32], float(D), [Bstat])
            toks = []
            for t in range(NT):
                if to_out is not None:
                    for dh in range(2):
                        ot, Bot = gwk()
                        P.op("dve", lambda e, t=t, dh=dh, ot=ot: e.scalar_tensor_tensor(out=ot[:], in0=h[:, t, dh * 512:(dh + 1) * 512], scalar=stat[:, 16 + t:17 + t], in1=gbc[:, dh * 512:(dh + 1) * 512], op0=ALU.mult, op1=ALU.mult), [Bh[t], Bstat, Bgbc], [Bot])
                        toks.append(P.dma("sp", lambda e, t=t, dh=dh, ot=ot: e.dma_start(out=to_out[t * 128:(t + 1) * 128, dh * 512:(dh + 1) * 512], in_=ot[:]), [Bot], [], Bot))
                    continue
                i = t % 2
                P.op("dve", lambda e, t=t, i=i: e.scalar_tensor_tensor(out=hntok[i][:], in0=h[:, t, :], scalar=stat[:, 16 + t:17 + t], in1=gbc[:], op0=ALU.mult, op1=ALU.mult), [Bh[t], Bstat, Bgbc], [Bhntok[i]])
                bank, Bb = gbank()
                bv = bank[:].bitcast(BF16)
                for c in range(8):
                    P.op("pe", lambda e, c=c, i=i, bv=bv: e.transpose(out=bv[:, c * 128:(c + 1) * 128], in_=hntok[i][:, c * 128:(c + 1) * 128], identity=idb[:]), [Bhntok[i], Bidb], [Bb])
                P.op("act", lambda e, t=t, bv=bv: e.copy(out=hnT[:, :, t * 128:(t + 1) * 128], in_=bv.rearrange("p (c k) -> p c k", c=8)), [Bb], [BhnT[t]])
            return toks

        def proj_fm(bank, Bb, wslot, Bw, c0, m, tok0):
            for kc in range(8):
                P.op("pe", lambda e, kc=kc: e.matmul(bank[0:m, :], lhsT=wslot[:, kc, c0:c0 + m], rhs=hnT[:, kc, tok0:tok0 + 512], start=(kc == 0), stop=(kc == 7)),
                     Bw + BhnT[tok0 // 128:tok0 // 128 + 4], [Bb])

        def load_cols(slot, j, w_ap, c0, n, off):
            P.dma("pool", lambda e: e.dma_start(out=win[slot][:, :, off:off + n], in_=w_ap.rearrange("(kc p) f -> p kc f", p=128)[:, :, c0:c0 + n]), [], [Bwin[slot][j]], Bwin[slot][j])

        vstate = {}

        def vproj_a(s, off, tok0):
            bank, Bb = gbank()
            proj_fm(bank, Bb, win[s], Bwin[s], off, 128, tok0)
            vf, Bvf = gwk()
            vfb = vf[:].bitcast(BF16)
            P.op("act", lambda e: e.copy(out=vfb[:, 0:512], in_=bank[:]), [Bb], [Bvf])
            vstate[s] = (vfb, Bvf)

        def vproj_b(s):
            vfb, Bvf = vstate[s]
            bank2, Bb2 = gbank()
            b2v = bank2[:].bitcast(BF16)
            for c in range(8):
                P.op("pe", lambda e, c=c: e.transpose(out=b2v[0:64, c * 128:(c + 1) * 128], in_=vfb[:, c * 64:(c + 1) * 64], identity=idb[:]), [Bvf, Bidb], [Bb2])
            P.op("act", lambda e: e.copy(out=vt[s][:], in_=b2v[0:64, :].rearrange("p (c v) -> p c v", c=8)), [Bb2], [Bvt[s]])

        def core_a(s, hd, dk, a_float):
            Ps, BPs = pb[4], Bpb[4]
            Pk, BPk = pb[5], Bpb[5]
            pkv = Pk[:].bitcast(BF16)
            for c in range(8):
                cols = slice(c * 64, (c + 1) * 64)
                P.op("pe", lambda e, cols=cols: e.matmul(Ps[0:64, cols], lhsT=kh[s][0:dk, cols], rhs=qh[s][0:dk, cols], start=True, stop=True), [Bkh[s], Bqh[s]], [BPs])
            for c in range(8):
                cols = slice(c * 64, (c + 1) * 64)
                P.op("pe", lambda e, cols=cols, c=c: e.transpose(out=pkv[0:64, c * 128:c * 128 + dk], in_=kh[s][0:dk, cols], identity=idb[0:dk, 0:dk]), [Bkh[s], Bidb], [BPk])
            P.op("dve", lambda e: e.tensor_tensor(out=pTall[s][:], in0=Ps[0:64, :].rearrange("p (c i) -> p c i", c=8), in1=cst[0:64, C_MASK:C_MASK + 64].unsqueeze(1).to_broadcast([64, 8, 64]), op=ALU.mult), [BPs, Bcst], [BpTall[s]])
            P.op("act", lambda e: e.copy(out=ktkall[s][:, :, 0:dk], in_=pkv[0:64, :].rearrange("p (c k) -> p c k", c=8)[:, :, 0:dk]), [BPk], [Bktkall[s]])
            for c in range(8):
                bank, Bb_ = (Ps, BPs) if c < 4 else (Pk, BPk)
                cc = c % 4
                P.op("pe", lambda e, c=c, cc=cc, bank=bank: e.matmul(bank[0:dk, cc * 128:(cc + 1) * 128], lhsT=ktkall[s][:, c, 0:dk], rhs=vt[s][:, c, :], start=True, stop=True), [Bktkall[s], Bvt[s]], [Bb_])
            P.op("act", lambda e: e.copy(out=Sball[s][0:dk, 0, :], in_=Sst[0:dk, hd, :]), [BS[hd]], [BSball[s]])
            for c in range(8):
                bank, Bb_ = (Ps, BPs) if c < 4 else (Pk, BPk)
                cc = c % 4
                U = bank[0:dk, cc * 128:(cc + 1) * 128]
                P.op("dve", lambda e, U=U: e.tensor_tensor(out=Sa[0:dk, :], in0=U, in1=Sst[0:dk, hd, :], op=ALU.add), [Bb_, BS[hd]], [BSa])
                if a_float is not None:
                    P.op("dve", lambda e: e.tensor_scalar(out=Sst[0:dk, hd, :], in0=Sa[0:dk, :], scalar1=a_float, scalar2=None, op0=ALU.mult), [BSa], [BS[hd]])
                else:
                    P.op("dve", lambda e, c=c: e.tensor_scalar(out=Sst[0:dk, hd, :], in0=Sa[0:dk, :], scalar1=acol[s][0:dk, c:c + 1], scalar2=None, op0=ALU.mult), [BSa, Bacol[s]], [BS[hd]])
                if c < 7:
                    P.op("act", lambda e, c=c: e.copy(out=Sball[s][0:dk, c + 1, :], in_=Sst[0:dk, hd, :]), [BS[hd]], [BSball[s]])

        def core_b(s, hd, dk, gcol):
            Po, BPo = pb[6 + (hd % 2)], Bpb[6 + (hd % 2)]
            for c in range(8):
                cols = slice(c * 64, (c + 1) * 64)
                P.op("pe", lambda e, cols=cols, c=c: e.matmul(Po[:, cols], lhsT=vt[s][:, c, :], rhs=pTall[s][:, c, :], start=True, stop=False), [Bvt[s], BpTall[s]], [BPo])
                P.op("pe", lambda e, cols=cols, c=c: e.matmul(Po[:, cols], lhsT=Sball[s][0:dk, c, :], rhs=qh[s][0:dk, cols], start=False, stop=True), [BSball[s], Bqh[s]], [BPo])
            post(Po, BPo, s, hd, gcol)

        def post(Po, BPo, s, hd, gcol):
            sq, Bsq = gwk()
            P.op("act", lambda e: e.activation(out=sq[:], in_=Po[:], func=AF.Square), [BPo], [Bsq])
            bank, Bb = gbank()
            P.op("pe", lambda e: e.matmul(bank[:], lhsT=cst[:, C_ONES:C_ONES + 128], rhs=sq[:], start=True, stop=True), [Bcst, Bsq], [Bb])
            r, Br = gwk()
            P.op("act", lambda e: e.activation(out=r[:], in_=bank[:], func=AF.Ln, scale=1.0 / 128.0, bias=C(C_EPS)), [Bb, Bcst], [Br])
            P.op("act", lambda e: e.activation(out=r[:], in_=r[:], func=AF.Exp, scale=-0.5), [Br], [Br])
            P.op("dve", lambda e: e.scalar_tensor_tensor(out=r[:], in0=Po[:], scalar=hng[:, gcol:gcol + 1], in1=r[:], op0=ALU.mult, op1=ALU.mult), [BPo, Bhng, Br], [Br])
            P.op("dve", lambda e: e.tensor_tensor(out=og[:, hd, :], in0=r[:], in1=gsl[s][:], op=ALU.mult), [Br, Bgsl[s]], [Bog[hd]])

        def silu_to(out_ap, Bout, bank, Bb, m=128):
            P.op("act", lambda e: e.activation(out=out_ap, in_=bank[0:m, :], func=AF.Silu), [Bb], Bout)

        def outproj(w_ap, tile4):
            for sub in range(4):
                t = tile4 * 4 + sub
                for dh in range(2):
                    bank, Bb = gbank()
                    for f in range(8):
                        P.op("pe", lambda e, f=f, sub=sub, dh=dh, bank=bank: e.matmul(bank[:], lhsT=og[:, f, sub * 128:(sub + 1) * 128], rhs=wo[:, f, dh * 512:(dh + 1) * 512], start=(f == 0), stop=(f == 7)), [Bog[f], Bwo[dh]], [Bb])
                    P.op("dve", lambda e, t=t, dh=dh, bank=bank: e.tensor_tensor(out=h[:, t, dh * 512:(dh + 1) * 512], in0=bank[:], in1=h[:, t, dh * 512:(dh + 1) * 512], op=ALU.add), [Bb, Bh[t]], [Bh[t]])

        def load_wo(w_ap):
            for dh in range(2):
                P.dma("pool", lambda e, dh=dh: e.dma_start(out=wo[:, :, dh * 512:(dh + 1) * 512], in_=w_ap.rearrange("(kc p) f -> p kc f", p=128)[:, :, dh * 512:(dh + 1) * 512]), [], [Bwo[dh]], Bwo[dh])

        def zero_state():
            for hd in range(8):
                P.op("dve", lambda e, hd=hd: e.memset(Sst[:, hd, :], 0.0), [], [BS[hd]])

        def rotary_tables(b, tok0):
            P.dma("sp", lambda e: e.dma_start(out=posi[:], in_=pos_d[b:b + 1, tok0:tok0 + 512].partition_broadcast(128)), [], [Bposi], Bposi)
            ang, Bang = gwk()
            tf, Btf = gwk()
            ti = tf[:].bitcast(I32)
            P.op("dve", lambda e: e.tensor_copy(out=ang[:], in_=posi[:]), [Bposi], [Bang])
            P.op("dve", lambda e: e.tensor_scalar(out=ang[:], in0=ang[:], scalar1=C(C_INVF), scalar2=None, op0=ALU.mult), [Bang, Bcst], [Bang])
            for k, shift in ((1, 0.0), (0, float(np.pi / 2))):
                R = cs[:, k, :]
                P.op("dve", lambda e, shift=shift: e.tensor_scalar(out=tf[:], in0=ang[:], scalar1=shift, scalar2=float(1 / (2 * np.pi)), op0=ALU.add, op1=ALU.mult), [Bang], [Btf])
                sc, Bsc = gwk()
                P.op("dve", lambda e, sc=sc: e.tensor_copy(out=sc[:].bitcast(I32), in_=tf[:]), [Btf], [Bsc])
                P.op("dve", lambda e, sc=sc: e.tensor_copy(out=tf[:], in_=sc[:].bitcast(I32)), [Bsc], [Btf])
                P.op("dve", lambda e, R=R: e.scalar_tensor_tensor(out=R, in0=tf[:], scalar=float(-2 * np.pi), in1=ang[:], op0=ALU.mult, op1=ALU.add), [Btf, Bang], [Bcs])
                if shift != 0.0:
                    P.op("dve", lambda e, R=R, shift=shift: e.tensor_scalar_add(out=R, in0=R, scalar1=shift), [Bcs], [Bcs])
                P.op("dve", lambda e, R=R: e.tensor_single_scalar(out=tf[:], in_=R, scalar=float(np.pi), op=ALU.is_gt), [Bcs], [Btf])
                P.op("dve", lambda e, R=R: e.scalar_tensor_tensor(out=R, in0=tf[:], scalar=float(-2 * np.pi), in1=R, op0=ALU.mult, op1=ALU.add), [Btf, Bcs], [Bcs])
                P.op("dve", lambda e, R=R: e.tensor_single_scalar(out=tf[:], in_=R, scalar=float(-np.pi), op=ALU.is_lt), [Bcs], [Btf])
                P.op("dve", lambda e, R=R: e.scalar_tensor_tensor(out=R, in0=tf[:], scalar=float(2 * np.pi), in1=R, op0=ALU.mult, op1=ALU.add), [Btf, Bcs], [Bcs])
                P.op("act", lambda e, R=R: e.activation(out=R, in_=R, func=AF.Sin), [Bcs], [Bcs])
            P.op("dve", lambda e: e.tensor_scalar(out=cs[:, 1, :], in0=cs[:, 1, :], scalar1=C(C_SIGN), scalar2=None, op0=ALU.mult), [Bcs, Bcst], [Bcs])

        hctr = [0]
        dbg_toks = []
        pend = [None]

        def even_mixer(b, l):
            e_ = l // 2
            win_ap = W["ab_w_in"][e_]
            load_wo(W["ab_w_out"][e_])
            zero_state()
            for tile4 in range(4):
                tok0 = tile4 * 512
                rotary_tables(b, tok0)
                for hd in range(8):
                    s = hctr[0] % 2
                    hctr[0] += 1
                    if hd >= 4 and DBG_FLIP:
                        s = 1 - s
                    Bw = Bwin[s]
                    if hd < 4:
                        hh = hd
                        cq, ck, cv, cg = hh * 128, 512 + hh * 128, 1024 + hh * 128, 1536 + hh * 128
                        load_cols(s, 0, win_ap, cq, 128, 0)
                        load_cols(s, 1, win_ap, cq + 64, 64, 128)
                        load_cols(s, 2, win_ap, cq, 64, 192)
                        load_cols(s, 3, win_ap, ck, 128, 256)
                        load_cols(s, 4, win_ap, ck + 64, 64, 384)
                        load_cols(s, 5, win_ap, ck, 64, 448)
                        load_cols(s, 6, win_ap, cv, 128, 512)
                        load_cols(s, 7, win_ap, cg, 128, 640)
                        vproj_a(s, 512, tok0)
                        for (o0, dst, Bdst, dtab) in ((0, qh[s], Bqh[s], C_DQ), (256, kh[s], Bkh[s], C_DK)):
                            b1, Bb1 = gbank()
                            proj_fm(b1, Bb1, win[s], Bw, o0, 128, tok0)
                            b2, Bb2 = gbank()
                            proj_fm(b2, Bb2, win[s], Bw, o0 + 128, 128, tok0)
                            t1, Bt1 = gwk()
                            t2, Bt2 = gwk()
                            P.op("dve", lambda e, b1=b1, t1=t1: e.tensor_tensor(out=t1[:], in0=b1[:], in1=cs[:, 0, :], op=ALU.mult), [Bb1, Bcs], [Bt1])
                            P.op("dve", lambda e, b2=b2, t2=t2: e.tensor_tensor(out=t2[:], in0=b2[:], in1=cs[:, 1, :], op=ALU.mult), [Bb2, Bcs], [Bt2])
                            P.op("pool", lambda e, t1=t1, t2=t2: e.tensor_tensor(out=t1[:], in0=t1[:], in1=t2[:], op=ALU.add), [Bt1, Bt2], [Bt1])
                            P.op("dve", lambda e, t1=t1, dst=dst, dtab=dtab, hh=hh: e.tensor_tensor(out=dst[:].rearrange("p (c i) -> p c i", c=8), in0=t1[:].rearrange("p (c i) -> p c i", c=8), in1=cst[:, dtab + hh * 64:dtab + (hh + 1) * 64].unsqueeze(1).to_broadcast([128, 8, 64]), op=ALU.mult), [Bt1, Bcst], [Bdst])
                        bg, Bbg = gbank()
                        proj_fm(bg, Bbg, win[s], Bw, 640, 128, tok0)
                        silu_to(gsl[s][:], [Bgsl[s]], bg, Bbg)
                        vproj_b(s)
                        if pend[0] is not None:
                            core_b(*pend[0])
                        core_a(s, hd, 128, RET_A[hh])
                        pend[0] = (s, hd, 128, 0 + e_)
                    else:
                        hh = hd - 4
                        cq, ck, cv, cg = 2048 + hh * 64, 2304 + hh * 64, 2560 + hh * 128, 3072 + hh * 128
                        load_cols(s, 0, win_ap, cq, 64, 0)
                        load_cols(s, 1, win_ap, ck, 64, 64)
                        if hh == 0:
                            load_cols(s, 2, win_ap, 3584, 16, 128)
                        load_cols(s, 6, win_ap, cv, 128, 512)
                        load_cols(s, 7, win_ap, cg, 128, 640)
                        vproj_a(s, 512, tok0)
                        if hh == 0:
                            bb, Bbb = gbank()
                            proj_fm(bb, Bbb, win[s], Bw, 128, 16, tok0)
                            P.op("act", lambda e, bb=bb: e.copy(out=bab[:], in_=bb[0:16, :]), [Bbb], [Bbab])
                        bx, Bbx = gbank()
                        P.op("pe", lambda e, bx=bx, hh=hh: e.matmul(bx[0:64, :], lhsT=wa2[:, e_, hh * 64:(hh + 1) * 64], rhs=bab[:], start=True, stop=True), [Bwa2, Bbab], [Bbx])
                        L, BL = gwk()
                        P.op("act", lambda e, bx=bx, L=L, hh=hh: e.activation(out=L[0:64, :], in_=bx[0:64, :], func=AF.Exp, scale=-1.0, bias=ba2[:, e_, hh:hh + 1]), [Bbx, Bba2], [BL])
                        P.op("act", lambda e, L=L: e.activation(out=L[0:64, :], in_=L[0:64, :], func=AF.Ln, scale=1.0, bias=C(C_ONE, rows=64)), [BL, Bcst], [BL])
                        G, BG = gwk()
                        P.op("dve", lambda e, L=L, G=G: e.tensor_tensor_scan(out=G[0:64, :], data0=cst[0:64, C_RESET:C_RESET + 512], data1=L[0:64, :], initial=0.0, op0=ALU.mult, op1=ALU.add), [BL, Bcst], [BG])
                        E1, BE1 = gwk()
                        P.op("act", lambda e, G=G, E1=E1: e.activation(out=E1[0:64, :], in_=G[0:64, :], func=AF.Exp, scale=-1.0 / 16.0), [BG], [BE1])
                        P.op("act", lambda e, G=G, s=s: e.activation(out=acol[s][0:64, :], in_=G[0:64, :].rearrange("p (c i) -> p c i", c=8)[:, :, 63], func=AF.Exp, scale=-1.0 / 16.0), [BG], [Bacol[s]])
                        P.op("act", lambda e, G=G: e.activation(out=G[0:64, :], in_=G[0:64, :], func=AF.Exp, scale=1.0 / 16.0), [BG], [BG])
                        bq, Bbq = gbank()
                        proj_fm(bq, Bbq, win[s], Bw, 0, 64, tok0)
                        P.op("dve", lambda e, bq=bq, E1=E1, s=s: e.scalar_tensor_tensor(out=qh[s][0:64, :], in0=bq[0:64, :], scalar=0.125, in1=E1[0:64, :], op0=ALU.mult, op1=ALU.mult), [Bbq, BE1], [Bqh[s]])
                        bk, Bbk = gbank()
                        proj_fm(bk, Bbk, win[s], Bw, 64, 64, tok0)
                        P.op("dve", lambda e, bk=bk, G=G, s=s: e.tensor_tensor(out=kh[s][0:64, :], in0=bk[0:64, :], in1=G[0:64, :], op=ALU.mult), [Bbk, BG], [Bkh[s]])
                        bg, Bbg = gbank()
                        proj_fm(bg, Bbg, win[s], Bw, 640, 128, tok0)
                        silu_to(gsl[s][:], [Bgsl[s]], bg, Bbg)
                        vproj_b(s)
                        if pend[0] is not None:
                            core_b(*pend[0])
                        core_a(s, hd, 64, None)
                        pend[0] = (s, hd, 64, 2 + e_)
                core_b(*pend[0])
                pend[0] = None
                outproj(W["ab_w_out"][e_], tile4)

        def odd_mixer(b, l):
            e_ = l // 2
            win_ap = W["c_w_in"][e_]
            load_wo(W["c_w_out"][e_])
            zero_state()
            for tile4 in range(4):
                tok0 = tile4 * 512
                for hd in range(8):
                    s = hctr[0] % 2
                    hctr[0] += 1
                    Bw = Bwin[s]
                    load_cols(s, 0, win_ap, hd * 128, 128, 0)
                    load_cols(s, 1, win_ap, 1024 + hd * 128, 128, 128)
                    load_cols(s, 6, win_ap, 2048 + hd * 128, 128, 512)
                    load_cols(s, 7, win_ap, 3072 + hd * 128, 128, 640)
                    vproj_a(s, 512, tok0)
                    bf_, Bbf = gbank()
                    proj_fm(bf_, Bbf, win[s], Bw, 128, 128, tok0)
                    sg, Bsg = gwk()
                    P.op("act", lambda e, bf_=bf_, sg=sg: e.activation(out=sg[:], in_=bf_[:], func=AF.Sigmoid), [Bbf], [Bsg])
                    G, BG = gwk()
                    P.op("dve", lambda e, sg=sg, G=G, hd=hd: e.tensor_scalar(out=G[:], in0=sg[:], scalar1=oml[:, l, hd:hd + 1], scalar2=lbt[:, l, hd:hd + 1], op0=ALU.mult, op1=ALU.add), [Bsg, Blbt], [BG])
                    P.op("act", lambda e, G=G: e.activation(out=G[:], in_=G[:], func=AF.Ln), [BG], [BG])
                    G2, BG2 = gwk()
                    P.op("dve", lambda e, G=G, G2=G2: e.tensor_tensor_scan(out=G2[:], data0=cst[:, C_RESET:C_RESET + 512], data1=G[:], initial=0.0, op0=ALU.mult, op1=ALU.add), [BG, Bcst], [BG2])
                    P.op("dve", lambda e, sg=sg, G=G, hd=hd: e.tensor_scalar(out=G[:], in0=sg[:], scalar1=noml[:, l, hd:hd + 1], scalar2=oml[:, l, hd:hd + 1], op0=ALU.mult, op1=ALU.add), [Bsg, Blbt], [BG])
                    P.op("act", lambda e, G2=G2, s=s: e.activation(out=acol[s][:], in_=G2[:].rearrange("p (c i) -> p c i", c=8)[:, :, 63], func=AF.Exp), [BG2], [Bacol[s]])
                    P.op("act", lambda e, G2=G2, sg=sg: e.activation(out=sg[:], in_=G2[:], func=AF.Exp, scale=-1.0), [BG2], [Bsg])
                    P.op("dve", lambda e, sg=sg, G=G, s=s: e.tensor_tensor(out=kh[s][:], in0=G[:], in1=sg[:], op=ALU.mult), [BG, Bsg], [Bkh[s]])
                    P.op("act", lambda e, G2=G2: e.activation(out=G2[:], in_=G2[:], func=AF.Exp), [BG2], [BG2])
                    bq, Bbq = gbank()
                    proj_fm(bq, Bbq, win[s], Bw, 0, 128, tok0)
                    qs, Bqs = gwk()
                    silu_to(qs[:], [Bqs], bq, Bbq)
                    P.op("dve", lambda e, qs=qs, G2=G2, s=s: e.scalar_tensor_tensor(out=qh[s][:], in0=qs[:], scalar=float(128.0 ** -0.5), in1=G2[:], op0=ALU.mult, op1=ALU.mult), [Bqs, BG2], [Bqh[s]])
                    bg, Bbg = gbank()
                    proj_fm(bg, Bbg, win[s], Bw, 640, 128, tok0)
                    silu_to(gsl[s][:], [Bgsl[s]], bg, Bbg)
                    vproj_b(s)
                    if pend[0] is not None:
                        core_b(*pend[0])
                    core_a(s, hd, 128, None)
                    pend[0] = (s, hd, 128, 4 + e_)
                core_b(*pend[0])
                pend[0] = None
                outproj(W["c_w_out"][e_], tile4)

        fctr = [0]
        dctr = [0]

        def swiglu_expert(wg_ap, wu_ap, wd_ap, F, gate_col):
            nchunks = F // 128
            c0 = 0
            while c0 < nchunks:
                ncg = min(4, nchunks - c0)
                s = fctr[0] % 2
                fctr[0] += 1
                f0 = c0 * 128
                nf = ncg * 128
                P.dma("pool", lambda e, s=s, f0=f0, nf=nf: e.dma_start(out=wg[s][:, :, 0:nf], in_=wg_ap.rearrange("(kc p) f -> p kc f", p=128)[:, :, f0:f0 + nf]), [], [Bwg[s]], Bwg[s])
                P.dma("pool", lambda e, s=s, f0=f0, nf=nf: e.dma_start(out=wu[s][:, :, 0:nf], in_=wu_ap.rearrange("(kc p) f -> p kc f", p=128)[:, :, f0:f0 + nf]), [], [Bwu[s]], Bwu[s])
                P.dma("pool", lambda e, s=s, f0=f0, ncg=ncg: e.dma_start(out=wd[s][:, 0:ncg, :], in_=wd_ap[f0:f0 + ncg * 128, :].rearrange("(fc p) d -> p fc d", p=128)), [], [Bwd[s]], Bwd[s])
                for tg in range(4):
                    tok0 = tg * 512
                    hs = (fctr[0] * 4 + tg) % 2
                    for fc in range(ncg):
                        Pg, BPg = pb[0 + (fc % 2)], Bpb[0 + (fc % 2)]
                        Pu, BPu = pb[2 + (fc % 2)], Bpb[2 + (fc % 2)]
                        for kc in range(8):
                            P.op("pe", lambda e, kc=kc, fc=fc, Pg=Pg, s=s, tok0=tok0: e.matmul(Pg[:], lhsT=wg[s][:, kc, fc * 128:(fc + 1) * 128], rhs=hnT[:, kc, tok0:tok0 + 512], start=(kc == 0), stop=(kc == 7)), [Bwg[s]] + BhnT[tg * 4:tg * 4 + 4], [BPg])
                        for kc in range(8):
                            P.op("pe", lambda e, kc=kc, fc=fc, Pu=Pu, s=s, tok0=tok0: e.matmul(Pu[:], lhsT=wu[s][:, kc, fc * 128:(fc + 1) * 128], rhs=hnT[:, kc, tok0:tok0 + 512], start=(kc == 0), stop=(kc == 7)), [Bwu[s]] + BhnT[tg * 4:tg * 4 + 4], [BPu])
                        sg, Bsg = gwk()
                        P.op("act", lambda e, Pg=Pg, sg=sg: e.activation(out=sg[:], in_=Pg[:], func=AF.Silu), [BPg], [Bsg])
                        P.op("dve", lambda e, Pu=Pu, sg=sg, hs=hs, fc=fc: e.tensor_tensor(out=hid[hs][:, fc, :], in0=Pu[:], in1=sg[:], op=ALU.mult), [BPu, Bsg], [Bhid[hs][fc]])
                    for sub in range(4):
                        t = tg * 4 + sub
                        for dh in range(2):
                            k = 4 + (dctr[0] % 4)
                            dctr[0] += 1
                            Pd, BPd = pb[k], Bpb[k]
                            for fc in range(ncg):
                                P.op("pe", lambda e, fc=fc, sub=sub, dh=dh, Pd=Pd, s=s, hs=hs: e.matmul(Pd[:], lhsT=hid[hs][:, fc, sub * 128:(sub + 1) * 128], rhs=wd[s][:, fc, dh * 512:(dh + 1) * 512], start=(fc == 0), stop=(fc == ncg - 1)), [Bhid[hs][fc], Bwd[s]], [BPd])
                            hsl = h[:, t, dh * 512:(dh + 1) * 512]
                            if gate_col is None:
                                P.op("dve", lambda e, Pd=Pd, hsl=hsl: e.tensor_tensor(out=hsl, in0=Pd[:], in1=hsl, op=ALU.add), [BPd, Bh[t]], [Bh[t]])
                            else:
                                P.op("dve", lambda e, Pd=Pd, hsl=hsl, t=t: e.scalar_tensor_tensor(out=hsl, in0=Pd[:], scalar=gt[:, t, gate_col:gate_col + 1], in1=hsl, op0=ALU.mult, op1=ALU.add), [BPd, Bh[t], Bgt], [Bh[t]])
                c0 += ncg

        def router(e_):
            P.dma("pool", lambda e: e.dma_start(out=wr[:], in_=W["moe_router"][e_].rearrange("(kc p) n -> p kc n", p=128)), [], [Bwr], Bwr)
            for t in range(NT):
                bank, Bb = gbank()
                for kc in range(8):
                    P.op("pe", lambda e, kc=kc, t=t, bank=bank: e.matmul(bank[:, 0:8], lhsT=hnT[:, kc, t * 128:(t + 1) * 128], rhs=wr[:, kc, :], start=(kc == 0), stop=(kc == 7)), [BhnT[t], Bwr], [Bb])
                P.op("act", lambda e, t=t, bank=bank: e.copy(out=lg[:, t, :], in_=bank[:, 0:8]), [Bb], [Blg])
            m1 = stat[:, 32:48]
            m2 = stat[:, 48:64]

            def bc(a):
                return a.unsqueeze(2).to_broadcast([128, NT, 8])
            P.op("dve", lambda e: e.tensor_reduce(out=m1, in_=lg[:], axis=AX.X, op=ALU.max), [Blg], [Bstat])
            P.op("dve", lambda e: e.tensor_tensor(out=rt[0][:], in0=lg[:], in1=bc(m1), op=ALU.is_equal), [Blg, Bstat], [Brt[0]])
            P.op("dve", lambda e: e.scalar_tensor_tensor(out=rt[1][:], in0=rt[0][:], scalar=-1e30, in1=lg[:], op0=ALU.mult, op1=ALU.add), [Brt[0], Blg], [Brt[1]])
            P.op("dve", lambda e: e.tensor_reduce(out=m2, in_=rt[1][:], axis=AX.X, op=ALU.max), [Brt[1]], [Bstat])
            P.op("dve", lambda e: e.tensor_tensor(out=rt[0][:], in0=lg[:], in1=bc(m2), op=ALU.is_ge), [Blg, Bstat], [Brt[0]])
            P.op("dve", lambda e: e.tensor_tensor(out=rt[1][:], in0=lg[:], in1=bc(m1), op=ALU.subtract), [Blg, Bstat], [Brt[1]])
            P.op("act", lambda e: e.activation(out=rt[1][:], in_=rt[1][:], func=AF.Exp), [Brt[1]], [Brt[1]])
            P.op("dve", lambda e: e.tensor_tensor(out=rt[1][:], in0=rt[1][:], in1=rt[0][:], op=ALU.mult), [Brt[0], Brt[1]], [Brt[1]])
            P.op("dve", lambda e: e.tensor_reduce(out=m1, in_=rt[1][:], axis=AX.X, op=ALU.add), [Brt[1]], [Bstat])
            P.op("dve", lambda e: e.reciprocal(out=m1, in_=m1), [Bstat], [Bstat])
            P.op("dve", lambda e: e.tensor_tensor(out=gt[:], in0=rt[1][:], in1=bc(m1), op=ALU.mult), [Brt[1], Bstat], [Bgt])

        final_toks = []
        for b in range(nseq):
            for q4 in range(4):
                P.dma("sp", lambda e, b=b, q4=q4: e.dma_start(out=h[:, q4 * 4:(q4 + 1) * 4, :], in_=x_d[b, q4 * 512:(q4 + 1) * 512, :].rearrange("(t p) d -> p t d", p=128)), [], Bh[q4 * 4:q4 * 4 + 4], Bh[q4 * 4])
            sub = 0
            for l in range(4):
                e_ = l // 2
                if sub < nsub and (ONLY is None or sub in ONLY):
                    norm(W["norm_mix_g"][l:l + 1, :])
                    alias_sync(ffn_bufs(), mixer_bufs())
                    if l % 2 == 0:
                        even_mixer(b, l)
                    else:
                        odd_mixer(b, l)
                sub += 1
                if sub < nsub and (ONLY is None or sub in ONLY):
                    norm(W["norm_ffn_g"][l:l + 1, :])
                    alias_sync(mixer_bufs(), ffn_bufs())
                    if l % 2 == 0:
                        swiglu_expert(W["ffn_w_gate"][e_], W["ffn_w_up"][e_], W["ffn_w_down"][e_], 2816, None)
                    else:
                        router(e_)
                        for ex in range(8):
                            swiglu_expert(W["moe_w_gate"][e_, ex], W["moe_w_up"][e_, ex], W["moe_w_down"][e_, ex], 3584, ex)
                sub += 1
            if final:
                final_toks = norm(W["final_norm_g"].rearrange("(o d) -> o d", o=1), to_out=out_d[b])
            else:
                final_toks = []
                for t in range(NT):
                    final_toks.append(P.dma("sp", lambda e, t=t, b=b: e.dma_start(out=out_d[b, t * 128:(t + 1) * 128, :], in_=h[:, t, :]), [Bh[t]], [], Bh[t]))
        P.emit(final_toks + dbg_toks)
        print("instr counts", {k: len(v) for k, v in P.q.items()}, "sems", P.nsem, flush=True)
    return nc


_NC_CACHE = {}


def run(inputs, nseq_per_core, nsub=8, final=True, ncores=8):
    key = (nseq_per_core, nsub, final)
    if key not in _NC_CACHE:
        _NC_CACHE[key] = build(nseq_per_core, nsub, final)
    nc = _NC_CACHE[key]
    consts = make_consts()
    in_maps = []
    for c in range(ncores):
        m = {n: np.ascontiguousarray(inputs[n], dtype=np.float32) for n, _ in WNAMES}
        m["x"] = np.ascontiguousarray(inputs["x"][c * nseq_per_core:(c + 1) * nseq_per_core], dtype=np.float32)
        m["positions"] = np.ascontiguousarray(inputs["positions"][c * nseq_per_core:(c + 1) * nseq_per_core], dtype=np.int32)
        m["consts"] = consts
        in_maps.append(m)
    res = run_bass_kernel_spmd(nc, in_maps, core_ids=list(range(ncores)))
    if DBG:
        np.save("dbg_out.npy", res.results[0]["dbg"])
    return np.concatenate([r["out"] for r in res.results], axis=0)


def kernel(**inputs):
    return run(inputs, 4).astype(np.float32)
```

```python
import numpy as np
from contextlib import ExitStack
import concourse.bass as bass
import concourse.mybir as mybir
from concourse.bass_utils import run_bass_kernel_spmd

F32 = mybir.dt.float32
BF16 = mybir.dt.bfloat16
I32 = mybir.dt.int32
AF = mybir.ActivationFunctionType
ALU = mybir.AluOpType
AX = mybir.AxisListType

D = 1024
DBG_FLIP = False
DBG = False
ONLY = None
T = 2048
NT = 16
EPS = 1e-6


class Tok:
    __slots__ = ("sem", "val", "eng")

    def __init__(self, sem, val, eng):
        self.sem = sem
        self.val = val
        self.eng = eng


class Buf:
    __slots__ = ("name", "w", "r", "dsem", "dcnt")

    def __init__(self, name):
        self.name = name
        self.w = None
        self.r = {}
        self.dsem = None
        self.dcnt = 0


class Prog:
    ENGS = ("pe", "act", "dve", "pool", "sp")

    def __init__(self, nc, stack):
        self.nc = nc
        self.stack = stack
        self.q = {e: [] for e in self.ENGS}
        self.cnt = {e: 0 for e in self.ENGS}
        self.sem = {e: stack.enter_context(nc.semaphore("s_" + e)) for e in ("pe", "act", "dve", "pool")}
        self.waited = {e: {} for e in self.ENGS}
        self.nsem = 4
        self.last_dma = None

    def _deps(self, eng, reads, writes):
        need = {}

        def add(t):
            if t is None:
                return
            if t.eng == "pe" and eng == "pe":
                return
            k = id(t.sem)
            if k not in need or need[k][1] < t.val:
                need[k] = (t.sem, t.val)

        for b in reads:
            add(b.w)
        for b in writes:
            add(b.w)
            for t in b.r.values():
                add(t)
        wd = self.waited[eng]
        for k, (sem, val) in need.items():
            if wd.get(k, -1) >= val:
                continue
            wd[k] = val
            self.q[eng].append(("wait", sem, val))

    def _mark(self, tok, reads, writes):
        for b in reads:
            b.r[id(tok.sem)] = tok
        for b in writes:
            b.w = tok
            b.r = {}

    def op(self, eng, fn, reads=(), writes=()):
        self._deps(eng, reads, writes)
        self.cnt[eng] += 1
        tok = Tok(self.sem[eng], self.cnt[eng], eng)
        self.q[eng].append(("op", fn, self.sem[eng], 1))
        self._mark(tok, reads, writes)
        return tok

    def dma(self, qeng, fn, reads, writes, primary):
        self._deps(qeng, reads, writes)
        if primary.dsem is None:
            primary.dsem = self.stack.enter_context(self.nc.semaphore("d_" + primary.name))
            self.nsem += 1
        primary.dcnt += 16
        tok = Tok(primary.dsem, primary.dcnt, None)
        self.q[qeng].append(("op", fn, primary.dsem, 16))
        self._mark(tok, reads, writes)
        return tok

    def emit(self, final_toks):
        nc = self.nc
        for t in final_toks:
            self.q["sp"].append(("wait", t.sem, t.val))

        def run(e, items):
            for it in items:
                if it[0] == "wait":
                    e.wait_ge(it[1], it[2])
                else:
                    it[1](e).then_inc(it[2], it[3])

        with nc.Block() as block:
            @block.tensor
            def _(e):
                run(e, self.q["pe"])

            @block.scalar
            def _(e):
                run(e, self.q["act"])

            @block.vector
            def _(e):
                run(e, self.q["dve"])

            @block.gpsimd
            def _(e):
                run(e, self.q["pool"])

            @block.sync
            def _(e):
                run(e, self.q["sp"])


C_ID = 0
C_ONES = 128
C_MASK = 256
C_RESET = 320
C_INVF = 832
C_SIGN = 833
C_EPS = 834
C_ONE = 835
C_NEGPI = 836
C_DQ = 840
C_DK = 840 + 256
NCONST = 840 + 512


def make_consts():
    c = np.zeros((128, NCONST), np.float32)
    c[:, C_ID:C_ID + 128] = np.eye(128, dtype=np.float32)
    c[:, C_ONES:C_ONES + 128] = 1.0
    j = np.arange(64)[:, None]
    i = np.arange(64)[None, :]
    c[0:64, C_MASK:C_MASK + 64] = (j <= i).astype(np.float32)
    r = np.ones(512, np.float32)
    r[::64] = 0.0
    c[:, C_RESET:C_RESET + 512] = r[None, :]
    p = np.arange(128)
    c[:, C_INVF] = (10000.0 ** (-(2.0 * (p % 64)) / 128.0)).astype(np.float32)
    c[:, C_SIGN] = np.where(p < 64, -1.0, 1.0)
    c[:, C_EPS] = EPS
    c[:, C_ONE] = 1.0
    c[:, C_NEGPI] = -np.pi
    for hh in range(4):
        lg = np.log1p(-np.exp2(-5.0 - hh))
        t = np.arange(64, dtype=np.float64)
        c[:, C_DQ + hh * 64:C_DQ + (hh + 1) * 64] = np.exp((t + 1.0) * lg)[None, :]
        c[:, C_DK + hh * 64:C_DK + (hh + 1) * 64] = (np.exp(-(t + 1.0) * lg) * 128.0 ** -0.5)[None, :]
    return c


RET_A = [float(np.exp(64.0 * np.log1p(-np.exp2(-5.0 - hh)))) for hh in range(4)]

WNAMES = [("norm_mix_g", [4, 1024]), ("norm_ffn_g", [4, 1024]), ("final_norm_g", [1024]),
          ("ab_w_in", [2, 1024, 3600]), ("gla_w_a2", [2, 16, 256]), ("gla_b_a2", [2, 256]),
          ("ret_norm_g", [2, 128]), ("gla_norm_g", [2, 128]), ("ab_w_out", [2, 1024, 1024]),
          ("ffn_w_gate", [2, 1024, 2816]), ("ffn_w_up", [2, 1024, 2816]), ("ffn_w_down", [2, 2816, 1024]),
          ("hgrn_lb_logits", [4, 1024]), ("c_w_in", [2, 1024, 4096]), ("hgrn_norm_g", [2, 128]),
          ("c_w_out", [2, 1024, 1024]), ("moe_router", [2, 1024, 8]), ("moe_w_gate", [2, 8, 1024, 3584]),
          ("moe_w_up", [2, 8, 1024, 3584]), ("moe_w_down", [2, 8, 3584, 1024])]


def build(nseq, nsub=8, final=True):
    nc = bass.Bass("TRN2", target_bir_lowering=False)
    x_d = nc.dram_tensor("x", [nseq, T, D], F32, kind="ExternalInput").ap()
    pos_d = nc.dram_tensor("positions", [nseq, T], I32, kind="ExternalInput").ap()
    W = {n: nc.dram_tensor(n, s, F32, kind="ExternalInput").ap() for n, s in WNAMES}
    cst_d = nc.dram_tensor("consts", [128, NCONST], F32, kind="ExternalInput").ap()
    out_d = nc.dram_tensor("out", [nseq, T, D], F32, kind="ExternalOutput").ap()
    dbg_d = nc.dram_tensor("dbg", [128, 8, 512], F32, kind="ExternalOutput").ap() if DBG else None

    with ExitStack() as st:
        P = Prog(nc, st)

        def sb(n, s, d):
            return st.enter_context(nc.sbuf_tensor(n, s, d))

        h = sb("h", [128, NT, D], F32)
        Bh = [Buf("h%d" % t) for t in range(NT)]
        hnT = sb("hnT", [128, 8, T], BF16)
        BhnT = [Buf("hnT%d" % t) for t in range(NT)]
        cst = sb("cst", [128, NCONST], F32)
        Bcst = Buf("cst")
        idb = sb("idb", [128, 128], BF16)
        Bidb = Buf("idb")
        gbc = sb("gbc", [128, D], F32)
        Bgbc = Buf("gbc")
        hntok = [sb("hntok%d" % i, [128, D], BF16) for i in range(2)]
        Bhntok = [Buf("hntok%d" % i) for i in range(2)]
        stat = sb("stat", [128, 64], F32)
        Bstat = Buf("stat")
        junk = hntok[1]
        Bjunk = Bhntok[1]
        lbt = sb("lbt", [128, 4, 8], F32)
        Blbt = Buf("lbt")
        oml = sb("oml", [128, 4, 8], F32)
        noml = sb("noml", [128, 4, 8], F32)
        hng = sb("hng", [128, 8], F32)
        Bhng = Buf("hng")
        ba2 = sb("ba2", [64, 2, 4], F32)
        Bba2 = Buf("ba2")
        wa2f = sb("wa2f", [16, 2, 256], F32)
        wa2 = sb("wa2", [16, 2, 256], BF16)
        Bwa2 = Buf("wa2")
        NW = 6
        wk = [sb("wk%d" % i, [128, 512], F32) for i in range(NW)]
        Bwk = [Buf("wk%d" % i) for i in range(NW)]
        arena = sb("arena", [128, 24576], BF16)
        win = [arena[:, i * 6144:(i + 1) * 6144].rearrange("p (k f) -> p k f", k=8) for i in range(2)]
        Bwin = [[Buf("win%d_%d" % (i, j)) for j in range(8)] for i in range(2)]
        qh = [sb("qh%d" % i, [128, 512], BF16) for i in range(2)]
        Bqh = [Buf("qh%d" % i) for i in range(2)]
        kh = [sb("kh%d" % i, [128, 512], BF16) for i in range(2)]
        Bkh = [Buf("kh%d" % i) for i in range(2)]
        vt = [sb("vt%d" % i, [64, 8, 128], BF16) for i in range(2)]
        Bvt = [Buf("vt%d" % i) for i in range(2)]
        gsl = [sb("gsl%d" % i, [128, 512], BF16) for i in range(2)]
        Bgsl = [Buf("gsl%d" % i) for i in range(2)]
        acol = [sb("acol%d" % i, [128, 8], F32) for i in range(2)]
        Bacol = [Buf("acol%d" % i) for i in range(2)]
        bab = sb("bab", [16, 512], BF16)
        Bbab = Buf("bab")
        Sst = sb("Sst", [128, 8, 128], F32)
        arena2 = sb("arena2", [128, 4096], BF16)
        Sbf = arena2[:, 3072:4096].rearrange("p (k f) -> p k f", k=8)
        BS = [Buf("S%d" % i) for i in range(8)]
        BSb = [Buf("Sb%d" % i) for i in range(8)]
        Sa = sb("Sa", [128, 128], F32)
        BSa = Buf("Sa")
        pTall = [sb("pTall%d" % i, [64, 8, 64], BF16) for i in range(2)]
        BpTall = [Buf("pTall%d" % i) for i in range(2)]
        ktkall = [sb("ktkall%d" % i, [64, 8, 128], BF16) for i in range(2)]
        Bktkall = [Buf("ktkall%d" % i) for i in range(2)]
        Sball = [arena2[:, 3072:4096].rearrange("p (k f) -> p k f", k=8), sb("Sball1", [128, 8, 128], BF16)]
        BSball = [Buf("Sball%d" % i) for i in range(2)]
        og = arena[:, 20480:24576].rearrange("p (k f) -> p k f", k=8)
        Bog = [Buf("og%d" % i) for i in range(8)]
        cs = arena2[:, 0:2048].bitcast(F32).rearrange("p (k f) -> p k f", k=2)
        Bcs = Buf("cs")
        posi = arena2[:, 2048:3072].bitcast(I32)
        Bposi = Buf("posi")
        wo = arena[:, 12288:20480].rearrange("p (k f) -> p k f", k=8)
        Bwo = [Buf("wo0"), Buf("wo1")]
        wg = [arena[:, i * 4096:(i + 1) * 4096].rearrange("p (k f) -> p k f", k=8) for i in range(2)]
        wu = [arena[:, 8192 + i * 4096:8192 + (i + 1) * 4096].rearrange("p (k f) -> p k f", k=8) for i in range(2)]
        wd = [arena[:, 16384 + i * 4096:16384 + (i + 1) * 4096].rearrange("p (k f) -> p k f", k=4) for i in range(2)]
        Bwg = [Buf("wg%d" % i) for i in range(2)]
        Bwu = [Buf("wu%d" % i) for i in range(2)]
        Bwd = [Buf("wd%d" % i) for i in range(2)]
        hid = [arena2[:, i * 2048:(i + 1) * 2048].rearrange("p (k f) -> p k f", k=4) for i in range(2)]
        Bhid = [[Buf("hid%d_%d" % (i, j)) for j in range(4)] for i in range(2)]
        wr = sb("wr", [128, 8, 8], BF16)
        Bwr = Buf("wr")
        lg = sb("lg", [128, NT, 8], F32)
        Blg = Buf("lg")
        gt = sb("gt", [128, NT, 8], F32)
        Bgt = Buf("gt")
        rt = [sb("rt%d" % i, [128, NT, 8], F32) for i in range(3)]
        Brt = [Buf("rt%d" % i) for i in range(3)]

        def alias_sync(src, dst):
            toks = {}
            for b_ in src:
                for t_ in ([b_.w] if b_.w is not None else []) + list(b_.r.values()):
                    k_ = id(t_.sem)
                    if k_ not in toks or toks[k_].val < t_.val:
                        toks[k_] = t_
            for b_ in dst:
                for k_, t_ in toks.items():
                    if k_ not in b_.r or b_.r[k_].val < t_.val:
                        b_.r[k_] = t_

        def mixer_bufs():
            return [x_ for l_ in Bwin for x_ in l_] + Bwo + Bog + [Bcs, Bposi, BSball[0]]

        def ffn_bufs():
            return Bwg + Bwu + Bwd + [x_ for l_ in Bhid for x_ in l_]

        pb = [st.enter_context(nc.psum_tensor("pb%d" % i, [128, 512], F32)) for i in range(8)]
        Bpb = [Buf("pb%d" % i) for i in range(8)]
        gctr = [0]

        def gbank():
            i = gctr[0] % 4
            gctr[0] += 1
            return pb[i], Bpb[i]

        wctr = [0]

        def gwk():
            i = wctr[0] % NW
            wctr[0] += 1
            return wk[i], Bwk[i]

        def C(col, n=1, rows=128):
            return cst[0:rows, col:col + n]

        P.dma("sp", lambda e: e.dma_start(out=cst[:], in_=cst_d), [], [Bcst], Bcst)
        P.op("dve", lambda e: e.tensor_copy(out=idb[:], in_=cst[:, C_ID:C_ID + 128]), [Bcst], [Bidb])
        P.dma("sp", lambda e: e.dma_start(out=lbt[:], in_=W["hgrn_lb_logits"].rearrange("l (hh p) -> p l hh", p=128), allow_slow_non_contiguous=True), [], [Blbt], Blbt)
        P.op("act", lambda e: e.activation(out=lbt[:], in_=lbt[:], func=AF.Exp), [Blbt], [Blbt])
        P.op("dve", lambda e: e.tensor_tensor(out=stat[:, 0:8], in0=lbt[:, 0, :], in1=lbt[:, 1, :], op=ALU.add), [Blbt], [Bstat])
        P.op("dve", lambda e: e.tensor_tensor(out=stat[:, 0:8], in0=stat[:, 0:8], in1=lbt[:, 2, :], op=ALU.add), [Blbt, Bstat], [Bstat])
        P.op("dve", lambda e: e.tensor_tensor(out=stat[:, 0:8], in0=stat[:, 0:8], in1=lbt[:, 3, :], op=ALU.add), [Blbt, Bstat], [Bstat])
        P.op("dve", lambda e: e.reciprocal(out=stat[:, 0:8], in_=stat[:, 0:8]), [Bstat], [Bstat])
        for l in range(4):
            P.op("dve", lambda e, l=l: e.tensor_tensor(out=lbt[:, l, :], in0=lbt[:, l, :], in1=stat[:, 0:8], op=ALU.mult), [Blbt, Bstat], [Blbt])
        P.op("dve", lambda e: e.memset(lbt[:, 0, :], 0.0), [Blbt], [Blbt])
        P.op("dve", lambda e: e.tensor_tensor(out=lbt[:, 2, :], in0=lbt[:, 2, :], in1=lbt[:, 1, :], op=ALU.add), [Blbt], [Blbt])
        P.op("dve", lambda e: e.tensor_tensor(out=lbt[:, 3, :], in0=lbt[:, 3, :], in1=lbt[:, 2, :], op=ALU.add), [Blbt], [Blbt])
        P.op("dve", lambda e: e.tensor_scalar(out=oml[:], in0=lbt[:], scalar1=-1.0, scalar2=1.0, op0=ALU.mult, op1=ALU.add), [Blbt], [Blbt])
        P.op("dve", lambda e: e.tensor_scalar(out=noml[:], in0=oml[:], scalar1=-1.0, scalar2=None, op0=ALU.mult), [Blbt], [Blbt])
        P.dma("sp", lambda e: e.dma_start(out=hng[:, 0:2], in_=W["ret_norm_g"].rearrange("l p -> p l"), allow_slow_non_contiguous=True), [], [Bhng], Bhng)
        P.dma("sp", lambda e: e.dma_start(out=hng[:, 2:4], in_=W["gla_norm_g"].rearrange("l p -> p l"), allow_slow_non_contiguous=True), [], [Bhng], Bhng)
        P.dma("sp", lambda e: e.dma_start(out=hng[:, 4:6], in_=W["hgrn_norm_g"].rearrange("l p -> p l"), allow_slow_non_contiguous=True), [], [Bhng], Bhng)
        P.dma("sp", lambda e: e.dma_start(out=ba2[:], in_=W["gla_b_a2"].rearrange("l (hh p) -> p l hh", p=64), allow_slow_non_contiguous=True), [], [Bba2], Bba2)
        P.op("dve", lambda e: e.tensor_scalar(out=ba2[:], in0=ba2[:], scalar1=-1.0, scalar2=None, op0=ALU.mult), [Bba2], [Bba2])
        P.dma("sp", lambda e: e.dma_start(out=wa2f[:], in_=W["gla_w_a2"].rearrange("l r c -> r l c")), [], [Bwa2], Bwa2)
        P.op("dve", lambda e: e.tensor_copy(out=wa2[:], in_=wa2f[:]), [Bwa2], [Bwa2])

        def rstd_from_sumsq(ap, n, bufs):
            P.op("act", lambda e: e.activation(out=ap, in_=ap, func=AF.Ln, scale=1.0 / n, bias=C(C_EPS, rows=ap.shape[0])), bufs + [Bcst], bufs)
            P.op("act", lambda e: e.activation(out=ap, in_=ap, func=AF.Exp, scale=-0.5), bufs, bufs)

        def norm(g_row_ap, to_out=None):
            P.dma("sp", lambda e: e.dma_start(out=gbc[:], in_=g_row_ap.partition_broadcast(128)), [], [Bgbc], Bgbc)
            for t in range(NT):
                P.op("act", lambda e, t=t: e.activation(out=junk[:], in_=h[:, t, :], func=AF.Square, accum_out=stat[:, 16 + t:17 + t]), [Bh[t]], [Bjunk, Bstat])
            rstd_from_sumsq(stat[:, 16:32], float(D), [Bstat])
            toks = []
            for t in range(NT):
                if to_out is not None:
                    for dh in range(2):
                        ot, Bot = gwk()
                        P.op("dve", lambda e, t=t, dh=dh, ot=ot: e.scalar_tensor_tensor(out=ot[:], in0=h[:, t, dh * 512:(dh + 1) * 512], scalar=stat[:, 16 + t:17 + t], in1=gbc[:, dh * 512:(dh + 1) * 512], op0=ALU.mult, op1=ALU.mult), [Bh[t], Bstat, Bgbc], [Bot])
                        toks.append(P.dma("sp", lambda e, t=t, dh=dh, ot=ot: e.dma_start(out=to_out[t * 128:(t + 1) * 128, dh * 512:(dh + 1) * 512], in_=ot[:]), [Bot], [], Bot))
                    continue
                i = t % 2
                P.op("dve", lambda e, t=t, i=i: e.scalar_tensor_tensor(out=hntok[i][:], in0=h[:, t, :], scalar=stat[:, 16 + t:17 + t], in1=gbc[:], op0=ALU.mult, op1=ALU.mult), [Bh[t], Bstat, Bgbc], [Bhntok[i]])
                bank, Bb = gbank()
                bv = bank[:].bitcast(BF16)
                for c in range(8):
                    P.op("pe", lambda e, c=c, i=i, bv=bv: e.transpose(out=bv[:, c * 128:(c + 1) * 128], in_=hntok[i][:, c * 128:(c + 1) * 128], identity=idb[:]), [Bhntok[i], Bidb], [Bb])
                P.op("act", lambda e, t=t, bv=bv: e.copy(out=hnT[:, :, t * 128:(t + 1) * 128], in_=bv.rearrange("p (c k) -> p c k", c=8)), [Bb], [BhnT[t]])
            return toks

        def proj_fm(bank, Bb, wslot, Bw, c0, m, tok0):
            for kc in range(8):
                P.op("pe", lambda e, kc=kc: e.matmul(bank[0:m, :], lhsT=wslot[:, kc, c0:c0 + m], rhs=hnT[:, kc, tok0:tok0 + 512], start=(kc == 0), stop=(kc == 7)),
                     Bw + BhnT[tok0 // 128:tok0 // 128 + 4], [Bb])

        def load_cols(slot, j, w_ap, c0, n, off):
            P.dma("pool", lambda e: e.dma_start(out=win[slot][:, :, off:off + n], in_=w_ap.rearrange("(kc p) f -> p kc f", p=128)[:, :, c0:c0 + n]), [], [Bwin[slot][j]], Bwin[slot][j])

        vstate = {}

        def vproj_a(s, off, tok0):
            bank, Bb = gbank()
            proj_fm(bank, Bb, win[s], Bwin[s], off, 128, tok0)
            vf, Bvf = gwk()
            vfb = vf[:].bitcast(BF16)
            P.op("act", lambda e: e.copy(out=vfb[:, 0:512], in_=bank[:]), [Bb], [Bvf])
            vstate[s] = (vfb, Bvf)

        def vproj_b(s):
            vfb, Bvf = vstate[s]
            bank2, Bb2 = gbank()
            b2v = bank2[:].bitcast(BF16)
            for c in range(8):
                P.op("pe", lambda e, c=c: e.transpose(out=b2v[0:64, c * 128:(c + 1) * 128], in_=vfb[:, c * 64:(c + 1) * 64], identity=idb[:]), [Bvf, Bidb], [Bb2])
            P.op("act", lambda e: e.copy(out=vt[s][:], in_=b2v[0:64, :].rearrange("p (c v) -> p c v", c=8)), [Bb2], [Bvt[s]])

        def core_a(s, hd, dk, a_float):
            Ps, BPs = pb[4], Bpb[4]
            Pk, BPk = pb[5], Bpb[5]
            pkv = Pk[:].bitcast(BF16)
            for c in range(8):
                cols = slice(c * 64, (c + 1) * 64)
                P.op("pe", lambda e, cols=cols: e.matmul(Ps[0:64, cols], lhsT=kh[s][0:dk, cols], rhs=qh[s][0:dk, cols], start=True, stop=True), [Bkh[s], Bqh[s]], [BPs])
            for c in range(8):
                cols = slice(c * 64, (c + 1) * 64)
                P.op("pe", lambda e, cols=cols, c=c: e.transpose(out=pkv[0:64, c * 128:c * 128 + dk], in_=kh[s][0:dk, cols], identity=idb[0:dk, 0:dk]), [Bkh[s], Bidb], [BPk])
            P.op("dve", lambda e: e.tensor_tensor(out=pTall[s][:], in0=Ps[0:64, :].rearrange("p (c i) -> p c i", c=8), in1=cst[0:64, C_MASK:C_MASK + 64].unsqueeze(1).to_broadcast([64, 8, 64]), op=ALU.mult), [BPs, Bcst], [BpTall[s]])
            P.op("act", lambda e: e.copy(out=ktkall[s][:, :, 0:dk], in_=pkv[0:64, :].rearrange("p (c k) -> p c k", c=8)[:, :, 0:dk]), [BPk], [Bktkall[s]])
            for c in range(8):
                bank, Bb_ = (Ps, BPs) if c < 4 else (Pk, BPk)
                cc = c % 4
                P.op("pe", lambda e, c=c, cc=cc, bank=bank: e.matmul(bank[0:dk, cc * 128:(cc + 1) * 128], lhsT=ktkall[s][:, c, 0:dk], rhs=vt[s][:, c, :], start=True, stop=True), [Bktkall[s], Bvt[s]], [Bb_])
            P.op("act", lambda e: e.copy(out=Sball[s][0:dk, 0, :], in_=Sst[0:dk, hd, :]), [BS[hd]], [BSball[s]])
            for c in range(8):
                bank, Bb_ = (Ps, BPs) if c < 4 else (Pk, BPk)
                cc = c % 4
                U = bank[0:dk, cc * 128:(cc + 1) * 128]
                P.op("dve", lambda e, U=U: e.tensor_tensor(out=Sa[0:dk, :], in0=U, in1=Sst[0:dk, hd, :], op=ALU.add), [Bb_, BS[hd]], [BSa])
                if a_float is not None:
                    P.op("dve", lambda e: e.tensor_scalar(out=Sst[0:dk, hd, :], in0=Sa[0:dk, :], scalar1=a_float, scalar2=None, op0=ALU.mult), [BSa], [BS[hd]])
                else:
                    P.op("dve", lambda e, c=c: e.tensor_scalar(out=Sst[0:dk, hd, :], in0=Sa[0:dk, :], scalar1=acol[s][0:dk, c:c + 1], scalar2=None, op0=ALU.mult), [BSa, Bacol[s]], [BS[hd]])
                if c < 7:
                    P.op("act", lambda e, c=c: e.copy(out=Sball[s][0:dk, c + 1, :], in_=Sst[0:dk, hd, :]), [BS[hd]], [BSball[s]])

        def core_b(s, hd, dk, gcol):
            Po, BPo = pb[6 + (hd % 2)], Bpb[6 + (hd % 2)]
            for c in range(8):
                cols = slice(c * 64, (c + 1) * 64)
                P.op("pe", lambda e, cols=cols, c=c: e.matmul(Po[:, cols], lhsT=vt[s][:, c, :], rhs=pTall[s][:, c, :], start=True, stop=False), [Bvt[s], BpTall[s]], [BPo])
                P.op("pe", lambda e, cols=cols, c=c: e.matmul(Po[:, cols], lhsT=Sball[s][0:dk, c, :], rhs=qh[s][0:dk, cols], start=False, stop=True), [BSball[s], Bqh[s]], [BPo])
            post(Po, BPo, s, hd, gcol)

        def post(Po, BPo, s, hd, gcol):
            sq, Bsq = gwk()
            P.op("act", lambda e: e.activation(out=sq[:], in_=Po[:], func=AF.Square), [BPo], [Bsq])
            bank, Bb = gbank()
            P.op("pe", lambda e: e.matmul(bank[:], lhsT=cst[:, C_ONES:C_ONES + 128], rhs=sq[:], start=True, stop=True), [Bcst, Bsq], [Bb])
            r, Br = gwk()
            P.op("act", lambda e: e.activation(out=r[:], in_=bank[:], func=AF.Ln, scale=1.0 / 128.0, bias=C(C_EPS)), [Bb, Bcst], [Br])
            P.op("act", lambda e: e.activation(out=r[:], in_=r[:], func=AF.Exp, scale=-0.5), [Br], [Br])
            P.op("dve", lambda e: e.scalar_tensor_tensor(out=r[:], in0=Po[:], scalar=hng[:, gcol:gcol + 1], in1=r[:], op0=ALU.mult, op1=ALU.mult), [BPo, Bhng, Br], [Br])
            P.op("dve", lambda e: e.tensor_tensor(out=og[:, hd, :], in0=r[:], in1=gsl[s][:], op=ALU.mult), [Br, Bgsl[s]], [Bog[hd]])

        def silu_to(out_ap, Bout, bank, Bb, m=128):
            P.op("act", lambda e: e.activation(out=out_ap, in_=bank[0:m, :], func=AF.Silu), [Bb], Bout)

        def outproj(w_ap, tile4):
            for sub in range(4):
                t = tile4 * 4 + sub
                for dh in range(2):
                    bank, Bb = gbank()
                    for f in range(8):
                        P.op("pe", lambda e, f=f, sub=sub, dh=dh, bank=bank: e.matmul(bank[:], lhsT=og[:, f, sub * 128:(sub + 1) * 128], rhs=wo[:, f, dh * 512:(dh + 1) * 512], start=(f == 0), stop=(f == 7)), [Bog[f], Bwo[dh]], [Bb])
                    P.op("dve", lambda e, t=t, dh=dh, bank=bank: e.tensor_tensor(out=h[:, t, dh * 512:(dh + 1) * 512], in0=bank[:], in1=h[:, t, dh * 512:(dh + 1) * 512], op=ALU.add), [Bb, Bh[t]], [Bh[t]])

        def load_wo(w_ap):
            for dh in range(2):
                P.dma("pool", lambda e, dh=dh: e.dma_start(out=wo[:, :, dh * 512:(dh + 1) * 512], in_=w_ap.rearrange("(kc p) f -> p kc f", p=128)[:, :, dh * 512:(dh + 1) * 512]), [], [Bwo[dh]], Bwo[dh])

        def zero_state():
            for hd in range(8):
                P.op("dve", lambda e, hd=hd: e.memset(Sst[:, hd, :], 0.0), [], [BS[hd]])

        def rotary_tables(b, tok0):
            P.dma("sp", lambda e: e.dma_start(out=posi[:], in_=pos_d[b:b + 1, tok0:tok0 + 512].partition_broadcast(128)), [], [Bposi], Bposi)
            ang, Bang = gwk()
            tf, Btf = gwk()
            ti = tf[:].bitcast(I32)
            P.op("dve", lambda e: e.tensor_copy(out=ang[:], in_=posi[:]), [Bposi], [Bang])
            P.op("dve", lambda e: e.tensor_scalar(out=ang[:], in0=ang[:], scalar1=C(C_INVF), scalar2=None, op0=ALU.mult), [Bang, Bcst], [Bang])
            for k, shift in ((1, 0.0), (0, float(np.pi / 2))):
                R = cs[:, k, :]
                P.op("dve", lambda e, shift=shift: e.tensor_scalar(out=tf[:], in0=ang[:], scalar1=shift, scalar2=float(1 / (2 * np.pi)), op0=ALU.add, op1=ALU.mult), [Bang], [Btf])
                sc, Bsc = gwk()
                P.op("dve", lambda e, sc=sc: e.tensor_copy(out=sc[:].bitcast(I32), in_=tf[:]), [Btf], [Bsc])
                P.op("dve", lambda e, sc=sc: e.tensor_copy(out=tf[:], in_=sc[:].bitcast(I32)), [Bsc], [Btf])
                P.op("dve", lambda e, R=R: e.scalar_tensor_tensor(out=R, in0=tf[:], scalar=float(-2 * np.pi), in1=ang[:], op0=ALU.mult, op1=ALU.add), [Btf, Bang], [Bcs])
                if shift != 0.0:
                    P.op("dve", lambda e, R=R, shift=shift: e.tensor_scalar_add(out=R, in0=R, scalar1=shift), [Bcs], [Bcs])
                P.op("dve", lambda e, R=R: e.tensor_single_scalar(out=tf[:], in_=R, scalar=float(np.pi), op=ALU.is_gt), [Bcs], [Btf])
                P.op("dve", lambda e, R=R: e.scalar_tensor_tensor(out=R, in0=tf[:], scalar=float(-2 * np.pi), in1=R, op0=ALU.mult, op1=ALU.add), [Btf, Bcs], [Bcs])
                P.op("dve", lambda e, R=R: e.tensor_single_scalar(out=tf[:], in_=R, scalar=float(-np.pi), op=ALU.is_lt), [Bcs], [Btf])
                P.op("dve", lambda e, R=R: e.scalar_tensor_tensor(out=R, in0=tf[:], scalar=float(2 * np.pi), in1=R, op0=ALU.mult, op1=ALU.add), [Btf, Bcs], [Bcs])
                P.op("act", lambda e, R=R: e.activation(out=R, in_=R, func=AF.Sin), [Bcs], [Bcs])
            P.op("dve", lambda e: e.tensor_scalar(out=cs[:, 1, :], in0=cs[:, 1, :], scalar1=C(C_SIGN), scalar2=None, op0=ALU.mult), [Bcs, Bcst], [Bcs])

        hctr = [0]
        dbg_toks = []
        pend = [None]

        def even_mixer(b, l):
            e_ = l // 2
            win_ap = W["ab_w_in"][e_]
            load_wo(W["ab_w_out"][e_])
            zero_state()
            for tile4 in range(4):
                tok0 = tile4 * 512
                rotary_tables(b, tok0)
                for hd in range(8):
                    s = hctr[0] % 2
                    hctr[0] += 1
                    if hd >= 4 and DBG_FLIP:
                        s = 1 - s
                    Bw = Bwin[s]
                    if hd < 4:
                        hh = hd
                        cq, ck, cv, cg = hh * 128, 512 + hh * 128, 1024 + hh * 128, 1536 + hh * 128
                        load_cols(s, 0, win_ap, cq, 128, 0)
                        load_cols(s, 1, win_ap, cq + 64, 64, 128)
                        load_cols(s, 2, win_ap, cq, 64, 192)
                        load_cols(s, 3, win_ap, ck, 128, 256)
                        load_cols(s, 4, win_ap, ck + 64, 64, 384)
                        load_cols(s, 5, win_ap, ck, 64, 448)
                        load_cols(s, 6, win_ap, cv, 128, 512)
                        load_cols(s, 7, win_ap, cg, 128, 640)
                        vproj_a(s, 512, tok0)
                        for (o0, dst, Bdst, dtab) in ((0, qh[s], Bqh[s], C_DQ), (256, kh[s], Bkh[s], C_DK)):
                            b1, Bb1 = gbank()
                            proj_fm(b1, Bb1, win[s], Bw, o0, 128, tok0)
                            b2, Bb2 = gbank()
                            proj_fm(b2, Bb2, win[s], Bw, o0 + 128, 128, tok0)
                            t1, Bt1 = gwk()
                            t2, Bt2 = gwk()
                            P.op("dve", lambda e, b1=b1, t1=t1: e.tensor_tensor(out=t1[:], in0=b1[:], in1=cs[:, 0, :], op=ALU.mult), [Bb1, Bcs], [Bt1])
                            P.op("dve", lambda e, b2=b2, t2=t2: e.tensor_tensor(out=t2[:], in0=b2[:], in1=cs[:, 1, :], op=ALU.mult), [Bb2, Bcs], [Bt2])
                            P.op("pool", lambda e, t1=t1, t2=t2: e.tensor_tensor(out=t1[:], in0=t1[:], in1=t2[:], op=ALU.add), [Bt1, Bt2], [Bt1])
                            P.op("dve", lambda e, t1=t1, dst=dst, dtab=dtab, hh=hh: e.tensor_tensor(out=dst[:].rearrange("p (c i) -> p c i", c=8), in0=t1[:].rearrange("p (c i) -> p c i", c=8), in1=cst[:, dtab + hh * 64:dtab + (hh + 1) * 64].unsqueeze(1).to_broadcast([128, 8, 64]), op=ALU.mult), [Bt1, Bcst], [Bdst])
                        bg, Bbg = gbank()
                        proj_fm(bg, Bbg, win[s], Bw, 640, 128, tok0)
                        silu_to(gsl[s][:], [Bgsl[s]], bg, Bbg)
                        vproj_b(s)
                        if pend[0] is not None:
                            core_b(*pend[0])
                        core_a(s, hd, 128, RET_A[hh])
                        pend[0] = (s, hd, 128, 0 + e_)
                    else:
                        hh = hd - 4
                        cq, ck, cv, cg = 2048 + hh * 64, 2304 + hh * 64, 2560 + hh * 128, 3072 + hh * 128
                        load_cols(s, 0, win_ap, cq, 64, 0)
                        load_cols(s, 1, win_ap, ck, 64, 64)
                        if hh == 0:
                            load_cols(s, 2, win_ap, 3584, 16, 128)
                        load_cols(s, 6, win_ap, cv, 128, 512)
                        load_cols(s, 7, win_ap, cg, 128, 640)
                        vproj_a(s, 512, tok0)
                        if hh == 0:
                            bb, Bbb = gbank()
                            proj_fm(bb, Bbb, win[s], Bw, 128, 16, tok0)
                            P.op("act", lambda e, bb=bb: e.copy(out=bab[:], in_=bb[0:16, :]), [Bbb], [Bbab])
                        bx, Bbx = gbank()
                        P.op("pe", lambda e, bx=bx, hh=hh: e.matmul(bx[0:64, :], lhsT=wa2[:, e_, hh * 64:(hh + 1) * 64], rhs=bab[:], start=True, stop=True), [Bwa2, Bbab], [Bbx])
                        bq, Bbq = gbank()
                        proj_fm(bq, Bbq, win[s], Bw, 0, 64, tok0)
                        bk, Bbk = gbank()
                        proj_fm(bk, Bbk, win[s], Bw, 64, 64, tok0)
                        L, BL = gwk()
                        P.op("act", lambda e, bx=bx, L=L, hh=hh: e.activation(out=L[0:64, :], in_=bx[0:64, :], func=AF.Exp, scale=-1.0, bias=ba2[:, e_, hh:hh + 1]), [Bbx, Bba2], [BL])
                        P.op("act", lambda e, L=L: e.activation(out=L[0:64, :], in_=L[0:64, :], func=AF.Ln, scale=1.0, bias=C(C_ONE, rows=64)), [BL, Bcst], [BL])
                        G, BG = gwk()
                        P.op("dve", lambda e, L=L, G=G: e.tensor_tensor_scan(out=G[0:64, :], data0=cst[0:64, C_RESET:C_RESET + 512], data1=L[0:64, :], initial=0.0, op0=ALU.mult, op1=ALU.add), [BL, Bcst], [BG])
                        E1, BE1 = gwk()
                        P.op("act", lambda e, G=G, L=L: e.activation(out=L[0:64, :], in_=G[0:64, :], func=AF.Exp, scale=1.0 / 16.0), [BG], [BL])
                        P.op("dve", lambda e, bk=bk, L=L, s=s: e.tensor_tensor(out=kh[s][0:64, :], in0=bk[0:64, :], in1=L[0:64, :], op=ALU.mult), [Bbk, BL], [Bkh[s]])
                        P.op("act", lambda e, G=G, E1=E1: e.activation(out=E1[0:64, :], in_=G[0:64, :], func=AF.Exp, scale=-1.0 / 16.0), [BG], [BE1])
                        P.op("dve", lambda e, bq=bq, E1=E1, s=s: e.scalar_tensor_tensor(out=qh[s][0:64, :], in0=bq[0:64, :], scalar=0.125, in1=E1[0:64, :], op0=ALU.mult, op1=ALU.mult), [Bbq, BE1], [Bqh[s]])
                        P.op("act", lambda e, G=G, s=s: e.activation(out=acol[s][0:64, :], in_=G[0:64, :].rearrange("p (c i) -> p c i", c=8)[:, :, 63], func=AF.Exp, scale=-1.0 / 16.0), [BG], [Bacol[s]])
                        bg, Bbg = gbank()
                        proj_fm(bg, Bbg, win[s], Bw, 640, 128, tok0)
                        silu_to(gsl[s][:], [Bgsl[s]], bg, Bbg)
                        vproj_b(s)
                        if pend[0] is not None:
                            core_b(*pend[0])
                        core_a(s, hd, 64, None)
                        pend[0] = (s, hd, 64, 2 + e_)
                core_b(*pend[0])
                pend[0] = None
                outproj(W["ab_w_out"][e_], tile4)

        def odd_mixer(b, l):
            e_ = l // 2
            win_ap = W["c_w_in"][e_]
            load_wo(W["c_w_out"][e_])
            zero_state()
            for tile4 in range(4):
                tok0 = tile4 * 512
                for hd in range(8):
                    s = hctr[0] % 2
                    hctr[0] += 1
                    Bw = Bwin[s]
                    load_cols(s, 0, win_ap, hd * 128, 128, 0)
                    load_cols(s, 1, win_ap, 1024 + hd * 128, 128, 128)
                    load_cols(s, 6, win_ap, 2048 + hd * 128, 128, 512)
                    load_cols(s, 7, win_ap, 3072 + hd * 128, 128, 640)
                    vproj_a(s, 512, tok0)
                    bf_, Bbf = gbank()
                    proj_fm(bf_, Bbf, win[s], Bw, 128, 128, tok0)
                    sg, Bsg = gwk()
                    P.op("act", lambda e, bf_=bf_, sg=sg: e.activation(out=sg[:], in_=bf_[:], func=AF.Sigmoid), [Bbf], [Bsg])
                    bq, Bbq = gbank()
                    proj_fm(bq, Bbq, win[s], Bw, 0, 128, tok0)
                    qs, Bqs = gwk()
                    silu_to(qs[:], [Bqs], bq, Bbq)
                    bg, Bbg = gbank()
                    proj_fm(bg, Bbg, win[s], Bw, 640, 128, tok0)
                    silu_to(gsl[s][:], [Bgsl[s]], bg, Bbg)
                    G, BG = gwk()
                    P.op("dve", lambda e, sg=sg, G=G, hd=hd: e.tensor_scalar(out=G[:], in0=sg[:], scalar1=oml[:, l, hd:hd + 1], scalar2=lbt[:, l, hd:hd + 1], op0=ALU.mult, op1=ALU.add), [Bsg, Blbt], [BG])
                    P.op("act", lambda e, G=G: e.activation(out=G[:], in_=G[:], func=AF.Ln), [BG], [BG])
                    P.op("dve", lambda e, sg=sg, hd=hd: e.tensor_scalar(out=sg[:], in0=sg[:], scalar1=noml[:, l, hd:hd + 1], scalar2=oml[:, l, hd:hd + 1], op0=ALU.mult, op1=ALU.add), [Bsg, Blbt], [Bsg])
                    G2, BG2 = gwk()
                    P.op("dve", lambda e, G=G, G2=G2: e.tensor_tensor_scan(out=G2[:], data0=cst[:, C_RESET:C_RESET + 512], data1=G[:], initial=0.0, op0=ALU.mult, op1=ALU.add), [BG, Bcst], [BG2])
                    P.op("act", lambda e, G2=G2, G=G: e.activation(out=G[:], in_=G2[:], func=AF.Exp, scale=-1.0), [BG2], [BG])
                    P.op("dve", lambda e, sg=sg, G=G, s=s: e.tensor_tensor(out=kh[s][:], in0=sg[:], in1=G[:], op=ALU.mult), [BG, Bsg], [Bkh[s]])
                    P.op("act", lambda e, G2=G2, s=s: e.activation(out=acol[s][:], in_=G2[:].rearrange("p (c i) -> p c i", c=8)[:, :, 63], func=AF.Exp), [BG2], [Bacol[s]])
                    P.op("act", lambda e, G2=G2: e.activation(out=G2[:], in_=G2[:], func=AF.Exp), [BG2], [BG2])
                    P.op("dve", lambda e, qs=qs, G2=G2, s=s: e.scalar_tensor_tensor(out=qh[s][:], in0=qs[:], scalar=float(128.0 ** -0.5), in1=G2[:], op0=ALU.mult, op1=ALU.mult), [Bqs, BG2], [Bqh[s]])
                    vproj_b(s)
                    if pend[0] is not None:
                        core_b(*pend[0])
                    core_a(s, hd, 128, None)
                    pend[0] = (s, hd, 128, 4 + e_)
                core_b(*pend[0])
                pend[0] = None
                outproj(W["c_w_out"][e_], tile4)

        fctr = [0]
        dctr = [0]

        def swiglu_expert(wg_ap, wu_ap, wd_ap, F, gate_col):
            nchunks = F // 128
            c0 = 0
            while c0 < nchunks:
                ncg = min(4, nchunks - c0)
                s = fctr[0] % 2
                fctr[0] += 1
                f0 = c0 * 128
                nf = ncg * 128
                P.dma("pool", lambda e, s=s, f0=f0, nf=nf: e.dma_start(out=wg[s][:, :, 0:nf], in_=wg_ap.rearrange("(kc p) f -> p kc f", p=128)[:, :, f0:f0 + nf]), [], [Bwg[s]], Bwg[s])
                P.dma("pool", lambda e, s=s, f0=f0, nf=nf: e.dma_start(out=wu[s][:, :, 0:nf], in_=wu_ap.rearrange("(kc p) f -> p kc f", p=128)[:, :, f0:f0 + nf]), [], [Bwu[s]], Bwu[s])
                P.dma("pool", lambda e, s=s, f0=f0, ncg=ncg: e.dma_start(out=wd[s][:, 0:ncg, :], in_=wd_ap[f0:f0 + ncg * 128, :].rearrange("(fc p) d -> p fc d", p=128)), [], [Bwd[s]], Bwd[s])
                for tg in range(4):
                    tok0 = tg * 512
                    hs = (fctr[0] * 4 + tg) % 2
                    for fc in range(ncg):
                        Pg, BPg = pb[0 + (fc % 2)], Bpb[0 + (fc % 2)]
                        Pu, BPu = pb[2 + (fc % 2)], Bpb[2 + (fc % 2)]
                        for kc in range(8):
                            P.op("pe", lambda e, kc=kc, fc=fc, Pg=Pg, s=s, tok0=tok0: e.matmul(Pg[:], lhsT=wg[s][:, kc, fc * 128:(fc + 1) * 128], rhs=hnT[:, kc, tok0:tok0 + 512], start=(kc == 0), stop=(kc == 7)), [Bwg[s]] + BhnT[tg * 4:tg * 4 + 4], [BPg])
                        for kc in range(8):
                            P.op("pe", lambda e, kc=kc, fc=fc, Pu=Pu, s=s, tok0=tok0: e.matmul(Pu[:], lhsT=wu[s][:, kc, fc * 128:(fc + 1) * 128], rhs=hnT[:, kc, tok0:tok0 + 512], start=(kc == 0), stop=(kc == 7)), [Bwu[s]] + BhnT[tg * 4:tg * 4 + 4], [BPu])
                        sg, Bsg = gwk()
                        P.op("act", lambda e, Pg=Pg, sg=sg: e.activation(out=sg[:], in_=Pg[:], func=AF.Silu), [BPg], [Bsg])
                        P.op("dve", lambda e, Pu=Pu, sg=sg, hs=hs, fc=fc: e.tensor_tensor(out=hid[hs][:, fc, :], in0=Pu[:], in1=sg[:], op=ALU.mult), [BPu, Bsg], [Bhid[hs][fc]])
                    for sub in range(4):
                        t = tg * 4 + sub
                        for dh in range(2):
                            k = 4 + (dctr[0] % 4)
                            dctr[0] += 1
                            Pd, BPd = pb[k], Bpb[k]
                            for fc in range(ncg):
                                P.op("pe", lambda e, fc=fc, sub=sub, dh=dh, Pd=Pd, s=s, hs=hs: e.matmul(Pd[:], lhsT=hid[hs][:, fc, sub * 128:(sub + 1) * 128], rhs=wd[s][:, fc, dh * 512:(dh + 1) * 512], start=(fc == 0), stop=(fc == ncg - 1)), [Bhid[hs][fc], Bwd[s]], [BPd])
                            hsl = h[:, t, dh * 512:(dh + 1) * 512]
                            if gate_col is None:
                                P.op("dve", lambda e, Pd=Pd, hsl=hsl: e.tensor_tensor(out=hsl, in0=Pd[:], in1=hsl, op=ALU.add), [BPd, Bh[t]], [Bh[t]])
                            else:
                                P.op("dve", lambda e, Pd=Pd, hsl=hsl, t=t: e.scalar_tensor_tensor(out=hsl, in0=Pd[:], scalar=gt[:, t, gate_col:gate_col + 1], in1=hsl, op0=ALU.mult, op1=ALU.add), [BPd, Bh[t], Bgt], [Bh[t]])
                c0 += ncg

        def router(e_):
            P.dma("pool", lambda e: e.dma_start(out=wr[:], in_=W["moe_router"][e_].rearrange("(kc p) n -> p kc n", p=128)), [], [Bwr], Bwr)
            for t in range(NT):
                bank, Bb = gbank()
                for kc in range(8):
                    P.op("pe", lambda e, kc=kc, t=t, bank=bank: e.matmul(bank[:, 0:8], lhsT=hnT[:, kc, t * 128:(t + 1) * 128], rhs=wr[:, kc, :], start=(kc == 0), stop=(kc == 7)), [BhnT[t], Bwr], [Bb])
                P.op("act", lambda e, t=t, bank=bank: e.copy(out=lg[:, t, :], in_=bank[:, 0:8]), [Bb], [Blg])
            m1 = stat[:, 32:48]
            m2 = stat[:, 48:64]

            def bc(a):
                return a.unsqueeze(2).to_broadcast([128, NT, 8])
            P.op("dve", lambda e: e.tensor_reduce(out=m1, in_=lg[:], axis=AX.X, op=ALU.max), [Blg], [Bstat])
            P.op("dve", lambda e: e.tensor_tensor(out=rt[0][:], in0=lg[:], in1=bc(m1), op=ALU.is_equal), [Blg, Bstat], [Brt[0]])
            P.op("dve", lambda e: e.scalar_tensor_tensor(out=rt[1][:], in0=rt[0][:], scalar=-1e30, in1=lg[:], op0=ALU.mult, op1=ALU.add), [Brt[0], Blg], [Brt[1]])
            P.op("dve", lambda e: e.tensor_reduce(out=m2, in_=rt[1][:], axis=AX.X, op=ALU.max), [Brt[1]], [Bstat])
            P.op("dve", lambda e: e.tensor_tensor(out=rt[0][:], in0=lg[:], in1=bc(m2), op=ALU.is_ge), [Blg, Bstat], [Brt[0]])
            P.op("dve", lambda e: e.tensor_tensor(out=rt[1][:], in0=lg[:], in1=bc(m1), op=ALU.subtract), [Blg, Bstat], [Brt[1]])
            P.op("act", lambda e: e.activation(out=rt[1][:], in_=rt[1][:], func=AF.Exp), [Brt[1]], [Brt[1]])
            P.op("dve", lambda e: e.tensor_tensor(out=rt[1][:], in0=rt[1][:], in1=rt[0][:], op=ALU.mult), [Brt[0], Brt[1]], [Brt[1]])
            P.op("dve", lambda e: e.tensor_reduce(out=m1, in_=rt[1][:], axis=AX.X, op=ALU.add), [Brt[1]], [Bstat])
            P.op("dve", lambda e: e.reciprocal(out=m1, in_=m1), [Bstat], [Bstat])
            P.op("dve", lambda e: e.tensor_tensor(out=gt[:], in0=rt[1][:], in1=bc(m1), op=ALU.mult), [Brt[1], Bstat], [Bgt])

        final_toks = []
        for b in range(nseq):
            for q4 in range(4):
                P.dma("sp", lambda e, b=b, q4=q4: e.dma_start(out=h[:, q4 * 4:(q4 + 1) * 4, :], in_=x_d[b, q4 * 512:(q4 + 1) * 512, :].rearrange("(t p) d -> p t d", p=128)), [], Bh[q4 * 4:q4 * 4 + 4], Bh[q4 * 4])
            sub = 0
            for l in range(4):
                e_ = l // 2
                if sub < nsub and (ONLY is None or sub in ONLY):
                    norm(W["norm_mix_g"][l:l + 1, :])
                    alias_sync(ffn_bufs(), mixer_bufs())
                    if l % 2 == 0:
                        even_mixer(b, l)
                    else:
                        odd_mixer(b, l)
                sub += 1
                if sub < nsub and (ONLY is None or sub in ONLY):
                    norm(W["norm_ffn_g"][l:l + 1, :])
                    alias_sync(mixer_bufs(), ffn_bufs())
                    if l % 2 == 0:
                        swiglu_expert(W["ffn_w_gate"][e_], W["ffn_w_up"][e_], W["ffn_w_down"][e_], 2816, None)
                    else:
                        router(e_)
                        for ex in range(8):
                            swiglu_expert(W["moe_w_gate"][e_, ex], W["moe_w_up"][e_, ex], W["moe_w_down"][e_, ex], 3584, ex)
                sub += 1
            if final:
                final_toks = norm(W["final_norm_g"].rearrange("(o d) -> o d", o=1), to_out=out_d[b])
            else:
                final_toks = []
                for t in range(NT):
                    final_toks.append(P.dma("sp", lambda e, t=t, b=b: e.dma_start(out=out_d[b, t * 128:(t + 1) * 128, :], in_=h[:, t, :]), [Bh[t]], [], Bh[t]))
        P.emit(final_toks + dbg_toks)
        print("instr counts", {k: len(v) for k, v in P.q.items()}, "sems", P.nsem, flush=True)
    return nc


_NC_CACHE = {}


def run(inputs, nseq_per_core, nsub=8, final=True, ncores=8):
    key = (nseq_per_core, nsub, final)
    if key not in _NC_CACHE:
        _NC_CACHE[key] = build(nseq_per_core, nsub, final)
    nc = _NC_CACHE[key]
    consts = make_consts()
    in_maps = []
    for c in range(ncores):
        m = {n: np.ascontiguousarray(inputs[n], dtype=np.float32) for n, _ in WNAMES}
        m["x"] = np.ascontiguousarray(inputs["x"][c * nseq_per_core:(c + 1) * nseq_per_core], dtype=np.float32)
        m["positions"] = np.ascontiguousarray(inputs["positions"][c * nseq_per_core:(c + 1) * nseq_per_core], dtype=np.int32)
        m["consts"] = consts
        in_maps.append(m)
    res = run_bass_kernel_spmd(nc, in_maps, core_ids=list(range(ncores)))
    if DBG:
        np.save("dbg_out.npy", res.results[0]["dbg"])
    return np.concatenate([r["out"] for r in res.results], axis=0)


def kernel(**inputs):
    return run(inputs, 4).astype(np.float32)
```

```python
import numpy as np
from contextlib import ExitStack
import concourse.bass as bass
import concourse.mybir as mybir
from concourse.bass_utils import run_bass_kernel_spmd

F32 = mybir.dt.float32
BF16 = mybir.dt.bfloat16
I32 = mybir.dt.int32
AF = mybir.ActivationFunctionType
ALU = mybir.AluOpType
AX = mybir.AxisListType

D = 1024
DBG_FLIP = False
DBG = False
ONLY = None
T = 2048
NT = 16
EPS = 1e-6


class Tok:
    __slots__ = ("sem", "val", "eng")

    def __init__(self, sem, val, eng):
        self.sem = sem
        self.val = val
        self.eng = eng


class Buf:
    __slots__ = ("name", "w", "r", "dsem", "dcnt")

    def __init__(self, name):
        self.name = name
        self.w = None
        self.r = {}
        self.dsem = None
        self.dcnt = 0


class Prog:
    ENGS = ("pe", "act", "dve", "pool", "sp")

    def __init__(self, nc, stack):
        self.nc = nc
        self.stack = stack
        self.q = {e: [] for e in self.ENGS}
        self.cnt = {e: 0 for e in self.ENGS}
        self.sem = {e: stack.enter_context(nc.semaphore("s_" + e)) for e in ("pe", "act", "dve", "pool")}
        self.waited = {e: {} for e in self.ENGS}
        self.nsem = 4
        self.last_dma = None

    def _deps(self, eng, reads, writes):
        need = {}

        def add(t):
            if t is None:
                return
            if t.eng == "pe" and eng == "pe":
                return
            k = id(t.sem)
            if k not in need or need[k][1] < t.val:
                need[k] = (t.sem, t.val)

        for b in reads:
            add(b.w)
        for b in writes:
            add(b.w)
            for t in b.r.values():
                add(t)
        wd = self.waited[eng]
        for k, (sem, val) in need.items():
            if wd.get(k, -1) >= val:
                continue
            wd[k] = val
            self.q[eng].append(("wait", sem, val))

    def _mark(self, tok, reads, writes):
        for b in reads:
            b.r[id(tok.sem)] = tok
        for b in writes:
            b.w = tok
            b.r = {}

    def op(self, eng, fn, reads=(), writes=()):
        self._deps(eng, reads, writes)
        self.cnt[eng] += 1
        tok = Tok(self.sem[eng], self.cnt[eng], eng)
        self.q[eng].append(("op", fn, self.sem[eng], 1))
        self._mark(tok, reads, writes)
        return tok

    def dma(self, qeng, fn, reads, writes, primary):
        self._deps(qeng, reads, writes)
        if primary.dsem is None:
            primary.dsem = self.stack.enter_context(self.nc.semaphore("d_" + primary.name))
            self.nsem += 1
        primary.dcnt += 16
        tok = Tok(primary.dsem, primary.dcnt, None)
        self.q[qeng].append(("op", fn, primary.dsem, 16))
        self._mark(tok, reads, writes)
        return tok

    def emit(self, final_toks):
        nc = self.nc
        for t in final_toks:
            self.q["sp"].append(("wait", t.sem, t.val))

        def run(e, items):
            for it in items:
                if it[0] == "wait":
                    e.wait_ge(it[1], it[2])
                else:
                    it[1](e).then_inc(it[2], it[3])

        with nc.Block() as block:
            @block.tensor
            def _(e):
                run(e, self.q["pe"])

            @block.scalar
            def _(e):
                run(e, self.q["act"])

            @block.vector
            def _(e):
                run(e, self.q["dve"])

            @block.gpsimd
            def _(e):
                run(e, self.q["pool"])

            @block.sync
            def _(e):
                run(e, self.q["sp"])


C_ID = 0
C_ONES = 128
C_MASK = 256
C_RESET = 320
C_INVF = 832
C_SIGN = 833
C_EPS = 834
C_ONE = 835
C_NEGPI = 836
C_DQ = 840
C_DK = 840 + 256
NCONST = 840 + 512


def make_consts():
    c = np.zeros((128, NCONST), np.float32)
    c[:, C_ID:C_ID + 128] = np.eye(128, dtype=np.float32)
    c[:, C_ONES:C_ONES + 128] = 1.0
    j = np.arange(64)[:, None]
    i = np.arange(64)[None, :]
    c[0:64, C_MASK:C_MASK + 64] = (j <= i).astype(np.float32)
    r = np.ones(512, np.float32)
    r[::64] = 0.0
    c[:, C_RESET:C_RESET + 512] = r[None, :]
    p = np.arange(128)
    c[:, C_INVF] = (10000.0 ** (-(2.0 * (p % 64)) / 128.0)).astype(np.float32)
    c[:, C_SIGN] = np.where(p < 64, -1.0, 1.0)
    c[:, C_EPS] = EPS
    c[:, C_ONE] = 1.0
    c[:, C_NEGPI] = -np.pi
    for hh in range(4):
        lg = np.log1p(-np.exp2(-5.0 - hh))
        t = np.arange(64, dtype=np.float64)
        c[:, C_DQ + hh * 64:C_DQ + (hh + 1) * 64] = np.exp((t + 1.0) * lg)[None, :]
        c[:, C_DK + hh * 64:C_DK + (hh + 1) * 64] = (np.exp(-(t + 1.0) * lg) * 128.0 ** -0.5)[None, :]
    return c


RET_A = [float(np.exp(64.0 * np.log1p(-np.exp2(-5.0 - hh)))) for hh in range(4)]

WNAMES = [("norm_mix_g", [4, 1024]), ("norm_ffn_g", [4, 1024]), ("final_norm_g", [1024]),
          ("ab_w_in", [2, 1024, 3600]), ("gla_w_a2", [2, 16, 256]), ("gla_b_a2", [2, 256]),
          ("ret_norm_g", [2, 128]), ("gla_norm_g", [2, 128]), ("ab_w_out", [2, 1024, 1024]),
          ("ffn_w_gate", [2, 1024, 2816]), ("ffn_w_up", [2, 1024, 2816]), ("ffn_w_down", [2, 2816, 1024]),
          ("hgrn_lb_logits", [4, 1024]), ("c_w_in", [2, 1024, 4096]), ("hgrn_norm_g", [2, 128]),
          ("c_w_out", [2, 1024, 1024]), ("moe_router", [2, 1024, 8]), ("moe_w_gate", [2, 8, 1024, 3584]),
          ("moe_w_up", [2, 8, 1024, 3584]), ("moe_w_down", [2, 8, 3584, 1024])]


def build(nseq, nsub=8, final=True):
    nc = bass.Bass("TRN2", target_bir_lowering=False)
    x_d = nc.dram_tensor("x", [nseq, T, D], F32, kind="ExternalInput").ap()
    pos_d = nc.dram_tensor("positions", [nseq, T], I32, kind="ExternalInput").ap()
    W = {n: nc.dram_tensor(n, s, F32, kind="ExternalInput").ap() for n, s in WNAMES}
    cst_d = nc.dram_tensor("consts", [128, NCONST], F32, kind="ExternalInput").ap()
    out_d = nc.dram_tensor("out", [nseq, T, D], F32, kind="ExternalOutput").ap()
    dbg_d = nc.dram_tensor("dbg", [128, 8, 512], F32, kind="ExternalOutput").ap() if DBG else None

    with ExitStack() as st:
        P = Prog(nc, st)

        def sb(n, s, d):
            return st.enter_context(nc.sbuf_tensor(n, s, d))

        h = sb("h", [128, NT, D], F32)
        Bh = [Buf("h%d" % t) for t in range(NT)]
        hnT = sb("hnT", [128, 8, T], BF16)
        BhnT = [Buf("hnT%d" % t) for t in range(NT)]
        cst = sb("cst", [128, NCONST], F32)
        Bcst = Buf("cst")
        idb = sb("idb", [128, 128], BF16)
        Bidb = Buf("idb")
        gbc = sb("gbc", [128, D], F32)
        Bgbc = Buf("gbc")
        hntok = [sb("hntok%d" % i, [128, D], BF16) for i in range(2)]
        Bhntok = [Buf("hntok%d" % i) for i in range(2)]
        stat = sb("stat", [128, 64], F32)
        Bstat = Buf("stat")
        junk = hntok[1]
        Bjunk = Bhntok[1]
        lbt = sb("lbt", [128, 4, 8], F32)
        Blbt = Buf("lbt")
        oml = sb("oml", [128, 4, 8], F32)
        noml = sb("noml", [128, 4, 8], F32)
        hng = sb("hng", [128, 8], F32)
        Bhng = Buf("hng")
        ba2 = sb("ba2", [64, 2, 4], F32)
        Bba2 = Buf("ba2")
        wa2f = sb("wa2f", [16, 2, 256], F32)
        wa2 = sb("wa2", [16, 2, 256], BF16)
        Bwa2 = Buf("wa2")
        NW = 6
        wk = [sb("wk%d" % i, [128, 512], F32) for i in range(NW)]
        Bwk = [Buf("wk%d" % i) for i in range(NW)]
        arena = sb("arena", [128, 24576], BF16)
        win = [arena[:, i * 6144:(i + 1) * 6144].rearrange("p (k f) -> p k f", k=8) for i in range(2)]
        Bwin = [[Buf("win%d_%d" % (i, j)) for j in range(8)] for i in range(2)]
        qh = [sb("qh%d" % i, [128, 512], BF16) for i in range(2)]
        Bqh = [Buf("qh%d" % i) for i in range(2)]
        kh = [sb("kh%d" % i, [128, 512], BF16) for i in range(2)]
        Bkh = [Buf("kh%d" % i) for i in range(2)]
        vt = [sb("vt%d" % i, [64, 8, 128], BF16) for i in range(2)]
        Bvt = [Buf("vt%d" % i) for i in range(2)]
        gsl = [sb("gsl%d" % i, [128, 512], BF16) for i in range(2)]
        Bgsl = [Buf("gsl%d" % i) for i in range(2)]
        acol = [sb("acol%d" % i, [128, 8], F32) for i in range(2)]
        Bacol = [Buf("acol%d" % i) for i in range(2)]
        bab = sb("bab", [16, 512], BF16)
        Bbab = Buf("bab")
        Sst = sb("Sst", [128, 8, 128], F32)
        arena2 = sb("arena2", [128, 4096], BF16)
        Sbf = arena2[:, 3072:4096].rearrange("p (k f) -> p k f", k=8)
        BS = [Buf("S%d" % i) for i in range(8)]
        BSb = [Buf("Sb%d" % i) for i in range(8)]
        Sa = sb("Sa", [128, 128], F32)
        BSa = Buf("Sa")
        pTall = [sb("pTall%d" % i, [64, 8, 64], BF16) for i in range(2)]
        BpTall = [Buf("pTall%d" % i) for i in range(2)]
        ktkall = [sb("ktkall%d" % i, [64, 8, 128], BF16) for i in range(2)]
        Bktkall = [Buf("ktkall%d" % i) for i in range(2)]
        Sball = [arena2[:, 3072:4096].rearrange("p (k f) -> p k f", k=8), sb("Sball1", [128, 8, 128], BF16)]
        BSball = [Buf("Sball%d" % i) for i in range(2)]
        og = arena[:, 20480:24576].rearrange("p (k f) -> p k f", k=8)
        Bog = [Buf("og%d" % i) for i in range(8)]
        cs = arena2[:, 0:2048].bitcast(F32).rearrange("p (k f) -> p k f", k=2)
        Bcs = Buf("cs")
        posi = arena2[:, 2048:3072].bitcast(I32)
        Bposi = Buf("posi")
        wo = arena[:, 12288:20480].rearrange("p (k f) -> p k f", k=8)
        Bwo = [Buf("wo0"), Buf("wo1")]
        wg = [arena[:, i * 4096:(i + 1) * 4096].rearrange("p (k f) -> p k f", k=8) for i in range(2)]
        wu = [arena[:, 8192 + i * 4096:8192 + (i + 1) * 4096].rearrange("p (k f) -> p k f", k=8) for i in range(2)]
        wd = [arena[:, 16384 + i * 4096:16384 + (i + 1) * 4096].rearrange("p (k f) -> p k f", k=4) for i in range(2)]
        Bwg = [Buf("wg%d" % i) for i in range(2)]
        Bwu = [Buf("wu%d" % i) for i in range(2)]
        Bwd = [Buf("wd%d" % i) for i in range(2)]
        hid = [arena2[:, i * 2048:(i + 1) * 2048].rearrange("p (k f) -> p k f", k=4) for i in range(2)]
        Bhid = [[Buf("hid%d_%d" % (i, j)) for j in range(4)] for i in range(2)]
        wr = sb("wr", [128, 8, 8], BF16)
        Bwr = Buf("wr")
        lg = sb("lg", [128, NT, 8], F32)
        Blg = Buf("lg")
        gt = sb("gt", [128, NT, 8], F32)
        Bgt = Buf("gt")
        rt = [sb("rt%d" % i, [128, NT, 8], F32) for i in range(3)]
        Brt = [Buf("rt%d" % i) for i in range(3)]

        def alias_sync(src, dst):
            toks = {}
            for b_ in src:
                for t_ in ([b_.w] if b_.w is not None else []) + list(b_.r.values()):
                    k_ = id(t_.sem)
                    if k_ not in toks or toks[k_].val < t_.val:
                        toks[k_] = t_
            for b_ in dst:
                for k_, t_ in toks.items():
                    if k_ not in b_.r or b_.r[k_].val < t_.val:
                        b_.r[k_] = t_

        def mixer_bufs():
            return [x_ for l_ in Bwin for x_ in l_] + Bwo + Bog + [Bcs, Bposi, BSball[0]]

        def ffn_bufs():
            return Bwg + Bwu + Bwd + [x_ for l_ in Bhid for x_ in l_]

        pb = [st.enter_context(nc.psum_tensor("pb%d" % i, [128, 512], F32)) for i in range(8)]
        Bpb = [Buf("pb%d" % i) for i in range(8)]
        gctr = [0]

        def gbank():
            i = gctr[0] % 4
            gctr[0] += 1
            return pb[i], Bpb[i]

        wctr = [0]

        def gwk():
            i = wctr[0] % NW
            wctr[0] += 1
            return wk[i], Bwk[i]

        def C(col, n=1, rows=128):
            return cst[0:rows, col:col + n]

        P.dma("sp", lambda e: e.dma_start(out=cst[:], in_=cst_d), [], [Bcst], Bcst)
        P.op("dve", lambda e: e.tensor_copy(out=idb[:], in_=cst[:, C_ID:C_ID + 128]), [Bcst], [Bidb])
        P.dma("sp", lambda e: e.dma_start(out=lbt[:], in_=W["hgrn_lb_logits"].rearrange("l (hh p) -> p l hh", p=128), allow_slow_non_contiguous=True), [], [Blbt], Blbt)
        P.op("act", lambda e: e.activation(out=lbt[:], in_=lbt[:], func=AF.Exp), [Blbt], [Blbt])
        P.op("dve", lambda e: e.tensor_tensor(out=stat[:, 0:8], in0=lbt[:, 0, :], in1=lbt[:, 1, :], op=ALU.add), [Blbt], [Bstat])
        P.op("dve", lambda e: e.tensor_tensor(out=stat[:, 0:8], in0=stat[:, 0:8], in1=lbt[:, 2, :], op=ALU.add), [Blbt, Bstat], [Bstat])
        P.op("dve", lambda e: e.tensor_tensor(out=stat[:, 0:8], in0=stat[:, 0:8], in1=lbt[:, 3, :], op=ALU.add), [Blbt, Bstat], [Bstat])
        P.op("dve", lambda e: e.reciprocal(out=stat[:, 0:8], in_=stat[:, 0:8]), [Bstat], [Bstat])
        for l in range(4):
            P.op("dve", lambda e, l=l: e.tensor_tensor(out=lbt[:, l, :], in0=lbt[:, l, :], in1=stat[:, 0:8], op=ALU.mult), [Blbt, Bstat], [Blbt])
        P.op("dve", lambda e: e.memset(lbt[:, 0, :], 0.0), [Blbt], [Blbt])
        P.op("dve", lambda e: e.tensor_tensor(out=lbt[:, 2, :], in0=lbt[:, 2, :], in1=lbt[:, 1, :], op=ALU.add), [Blbt], [Blbt])
        P.op("dve", lambda e: e.tensor_tensor(out=lbt[:, 3, :], in0=lbt[:, 3, :], in1=lbt[:, 2, :], op=ALU.add), [Blbt], [Blbt])
        P.op("dve", lambda e: e.tensor_scalar(out=oml[:], in0=lbt[:], scalar1=-1.0, scalar2=1.0, op0=ALU.mult, op1=ALU.add), [Blbt], [Blbt])
        P.op("dve", lambda e: e.tensor_scalar(out=noml[:], in0=oml[:], scalar1=-1.0, scalar2=None, op0=ALU.mult), [Blbt], [Blbt])
        P.dma("sp", lambda e: e.dma_start(out=hng[:, 0:2], in_=W["ret_norm_g"].rearrange("l p -> p l"), allow_slow_non_contiguous=True), [], [Bhng], Bhng)
        P.dma("sp", lambda e: e.dma_start(out=hng[:, 2:4], in_=W["gla_norm_g"].rearrange("l p -> p l"), allow_slow_non_contiguous=True), [], [Bhng], Bhng)
        P.dma("sp", lambda e: e.dma_start(out=hng[:, 4:6], in_=W["hgrn_norm_g"].rearrange("l p -> p l"), allow_slow_non_contiguous=True), [], [Bhng], Bhng)
        P.dma("sp", lambda e: e.dma_start(out=ba2[:], in_=W["gla_b_a2"].rearrange("l (hh p) -> p l hh", p=64), allow_slow_non_contiguous=True), [], [Bba2], Bba2)
        P.op("dve", lambda e: e.tensor_scalar(out=ba2[:], in0=ba2[:], scalar1=-1.0, scalar2=None, op0=ALU.mult), [Bba2], [Bba2])
        P.dma("sp", lambda e: e.dma_start(out=wa2f[:], in_=W["gla_w_a2"].rearrange("l r c -> r l c")), [], [Bwa2], Bwa2)
        P.op("dve", lambda e: e.tensor_copy(out=wa2[:], in_=wa2f[:]), [Bwa2], [Bwa2])

        def rstd_from_sumsq(ap, n, bufs):
            P.op("act", lambda e: e.activation(out=ap, in_=ap, func=AF.Ln, scale=1.0 / n, bias=C(C_EPS, rows=ap.shape[0])), bufs + [Bcst], bufs)
            P.op("act", lambda e: e.activation(out=ap, in_=ap, func=AF.Exp, scale=-0.5), bufs, bufs)

        def norm(g_row_ap, to_out=None):
            P.dma("sp", lambda e: e.dma_start(out=gbc[:], in_=g_row_ap.partition_broadcast(128)), [], [Bgbc], Bgbc)
            for t in range(NT):
                P.op("act", lambda e, t=t: e.activation(out=junk[:], in_=h[:, t, :], func=AF.Square, accum_out=stat[:, 16 + t:17 + t]), [Bh[t]], [Bjunk, Bstat])
            rstd_from_sumsq(stat[:, 16:32], float(D), [Bstat])
            toks = []
            for t in range(NT):
                if to_out is not None:
                    for dh in range(2):
                        ot, Bot = gwk()
                        P.op("dve", lambda e, t=t, dh=dh, ot=ot: e.scalar_tensor_tensor(out=ot[:], in0=h[:, t, dh * 512:(dh + 1) * 512], scalar=stat[:, 16 + t:17 + t], in1=gbc[:, dh * 512:(dh + 1) * 512], op0=ALU.mult, op1=ALU.mult), [Bh[t], Bstat, Bgbc], [Bot])
                        toks.append(P.dma("sp", lambda e, t=t, dh=dh, ot=ot: e.dma_start(out=to_out[t * 128:(t + 1) * 128, dh * 512:(dh + 1) * 512], in_=ot[:]), [Bot], [], Bot))
                    continue
                i = t % 2
                P.op("dve", lambda e, t=t, i=i: e.scalar_tensor_tensor(out=hntok[i][:], in0=h[:, t, :], scalar=stat[:, 16 + t:17 + t], in1=gbc[:], op0=ALU.mult, op1=ALU.mult), [Bh[t], Bstat, Bgbc], [Bhntok[i]])
                bank, Bb = gbank()
                bv = bank[:].bitcast(BF16)
                for c in range(8):
                    P.op("pe", lambda e, c=c, i=i, bv=bv: e.transpose(out=bv[:, c * 128:(c + 1) * 128], in_=hntok[i][:, c * 128:(c + 1) * 128], identity=idb[:]), [Bhntok[i], Bidb], [Bb])
                if t % 2 == 0:
                    P.op("act", lambda e, t=t, bv=bv: e.copy(out=hnT[:, :, t * 128:(t + 1) * 128], in_=bv.rearrange("p (c k) -> p c k", c=8)), [Bb], [BhnT[t]])
                else:
                    P.op("dve", lambda e, t=t, bv=bv: e.tensor_copy(out=hnT[:, :, t * 128:(t + 1) * 128], in_=bv.rearrange("p (c k) -> p c k", c=8)), [Bb], [BhnT[t]])
            return toks

        def proj_fm(bank, Bb, wslot, Bw, c0, m, tok0):
            for kc in range(8):
                P.op("pe", lambda e, kc=kc: e.matmul(bank[0:m, :], lhsT=wslot[:, kc, c0:c0 + m], rhs=hnT[:, kc, tok0:tok0 + 512], start=(kc == 0), stop=(kc == 7)),
                     Bw + BhnT[tok0 // 128:tok0 // 128 + 4], [Bb])

        def load_cols(slot, j, w_ap, c0, n, off):
            P.dma("pool", lambda e: e.dma_start(out=win[slot][:, :, off:off + n], in_=w_ap.rearrange("(kc p) f -> p kc f", p=128)[:, :, c0:c0 + n]), [], [Bwin[slot][j]], Bwin[slot][j])

        vstate = {}

        def vproj_a(s, off, tok0):
            bank, Bb = gbank()
            proj_fm(bank, Bb, win[s], Bwin[s], off, 128, tok0)
            vf, Bvf = gwk()
            vfb = vf[:].bitcast(BF16)
            P.op("act", lambda e: e.copy(out=vfb[:, 0:512], in_=bank[:]), [Bb], [Bvf])
            vstate[s] = (vfb, Bvf)

        def vproj_b(s):
            vfb, Bvf = vstate[s]
            bank2, Bb2 = gbank()
            b2v = bank2[:].bitcast(BF16)
            for c in range(8):
                P.op("pe", lambda e, c=c: e.transpose(out=b2v[0:64, c * 128:(c + 1) * 128], in_=vfb[:, c * 64:(c + 1) * 64], identity=idb[:]), [Bvf, Bidb], [Bb2])
            P.op("act", lambda e: e.copy(out=vt[s][:], in_=b2v[0:64, :].rearrange("p (c v) -> p c v", c=8)), [Bb2], [Bvt[s]])

        def core_a(s, hd, dk, a_float):
            Ps, BPs = pb[4], Bpb[4]
            Pk, BPk = pb[5], Bpb[5]
            pkv = Pk[:].bitcast(BF16)
            for c in range(8):
                cols = slice(c * 64, (c + 1) * 64)
                P.op("pe", lambda e, cols=cols: e.matmul(Ps[0:64, cols], lhsT=kh[s][0:dk, cols], rhs=qh[s][0:dk, cols], start=True, stop=True), [Bkh[s], Bqh[s]], [BPs])
            for c in range(8):
                cols = slice(c * 64, (c + 1) * 64)
                P.op("pe", lambda e, cols=cols, c=c: e.transpose(out=pkv[0:64, c * 128:c * 128 + dk], in_=kh[s][0:dk, cols], identity=idb[0:dk, 0:dk]), [Bkh[s], Bidb], [BPk])
            P.op("dve", lambda e: e.tensor_tensor(out=pTall[s][:], in0=Ps[0:64, :].rearrange("p (c i) -> p c i", c=8), in1=cst[0:64, C_MASK:C_MASK + 64].unsqueeze(1).to_broadcast([64, 8, 64]), op=ALU.mult), [BPs, Bcst], [BpTall[s]])
            P.op("act", lambda e: e.copy(out=ktkall[s][:, :, 0:dk], in_=pkv[0:64, :].rearrange("p (c k) -> p c k", c=8)[:, :, 0:dk]), [BPk], [Bktkall[s]])
            for c in range(8):
                bank, Bb_ = (Ps, BPs) if c < 4 else (Pk, BPk)
                cc = c % 4
                P.op("pe", lambda e, c=c, cc=cc, bank=bank: e.matmul(bank[0:dk, cc * 128:(cc + 1) * 128], lhsT=ktkall[s][:, c, 0:dk], rhs=vt[s][:, c, :], start=True, stop=True), [Bktkall[s], Bvt[s]], [Bb_])
            P.op("act", lambda e: e.copy(out=Sball[s][0:dk, 0, :], in_=Sst[0:dk, hd, :]), [BS[hd]], [BSball[s]])
            for c in range(8):
                bank, Bb_ = (Ps, BPs) if c < 4 else (Pk, BPk)
                cc = c % 4
                U = bank[0:dk, cc * 128:(cc + 1) * 128]
                P.op("dve", lambda e, U=U: e.tensor_tensor(out=Sa[0:dk, :], in0=U, in1=Sst[0:dk, hd, :], op=ALU.add), [Bb_, BS[hd]], [BSa])
                if a_float is not None:
                    P.op("dve", lambda e: e.tensor_scalar(out=Sst[0:dk, hd, :], in0=Sa[0:dk, :], scalar1=a_float, scalar2=None, op0=ALU.mult), [BSa], [BS[hd]])
                else:
                    P.op("dve", lambda e, c=c: e.tensor_scalar(out=Sst[0:dk, hd, :], in0=Sa[0:dk, :], scalar1=acol[s][0:dk, c:c + 1], scalar2=None, op0=ALU.mult), [BSa, Bacol[s]], [BS[hd]])
                if c < 7:
                    P.op("act", lambda e, c=c: e.copy(out=Sball[s][0:dk, c + 1, :], in_=Sst[0:dk, hd, :]), [BS[hd]], [BSball[s]])

        def core_b(s, hd, dk, gcol):
            Po, BPo = pb[6 + (hd % 2)], Bpb[6 + (hd % 2)]
            for c in range(8):
                cols = slice(c * 64, (c + 1) * 64)
                P.op("pe", lambda e, cols=cols, c=c: e.matmul(Po[:, cols], lhsT=vt[s][:, c, :], rhs=pTall[s][:, c, :], start=True, stop=False), [Bvt[s], BpTall[s]], [BPo])
                P.op("pe", lambda e, cols=cols, c=c: e.matmul(Po[:, cols], lhsT=Sball[s][0:dk, c, :], rhs=qh[s][0:dk, cols], start=False, stop=True), [BSball[s], Bqh[s]], [BPo])
            post(Po, BPo, s, hd, gcol)

        def post(Po, BPo, s, hd, gcol):
            sq, Bsq = gwk()
            P.op("act", lambda e: e.activation(out=sq[:], in_=Po[:], func=AF.Square), [BPo], [Bsq])
            bank, Bb = gbank()
            P.op("pe", lambda e: e.matmul(bank[:], lhsT=cst[:, C_ONES:C_ONES + 128], rhs=sq[:], start=True, stop=True), [Bcst, Bsq], [Bb])
            r, Br = gwk()
            P.op("act", lambda e: e.activation(out=r[:], in_=bank[:], func=AF.Ln, scale=1.0 / 128.0, bias=C(C_EPS)), [Bb, Bcst], [Br])
            P.op("act", lambda e: e.activation(out=r[:], in_=r[:], func=AF.Exp, scale=-0.5), [Br], [Br])
            P.op("dve", lambda e: e.scalar_tensor_tensor(out=r[:], in0=Po[:], scalar=hng[:, gcol:gcol + 1], in1=r[:], op0=ALU.mult, op1=ALU.mult), [BPo, Bhng, Br], [Br])
            P.op("dve", lambda e: e.tensor_tensor(out=og[:, hd, :], in0=r[:], in1=gsl[s][:], op=ALU.mult), [Br, Bgsl[s]], [Bog[hd]])

        def silu_to(out_ap, Bout, bank, Bb, m=128):
            P.op("act", lambda e: e.activation(out=out_ap, in_=bank[0:m, :], func=AF.Silu), [Bb], Bout)

        def outproj(w_ap, tile4):
            for sub in range(4):
                t = tile4 * 4 + sub
                for dh in range(2):
                    bank, Bb = gbank()
                    for f in range(8):
                        P.op("pe", lambda e, f=f, sub=sub, dh=dh, bank=bank: e.matmul(bank[:], lhsT=og[:, f, sub * 128:(sub + 1) * 128], rhs=wo[:, f, dh * 512:(dh + 1) * 512], start=(f == 0), stop=(f == 7)), [Bog[f], Bwo[dh]], [Bb])
                    P.op("dve", lambda e, t=t, dh=dh, bank=bank: e.tensor_tensor(out=h[:, t, dh * 512:(dh + 1) * 512], in0=bank[:], in1=h[:, t, dh * 512:(dh + 1) * 512], op=ALU.add), [Bb, Bh[t]], [Bh[t]])

        def load_wo(w_ap):
            for dh in range(2):
                P.dma("pool", lambda e, dh=dh: e.dma_start(out=wo[:, :, dh * 512:(dh + 1) * 512], in_=w_ap.rearrange("(kc p) f -> p kc f", p=128)[:, :, dh * 512:(dh + 1) * 512]), [], [Bwo[dh]], Bwo[dh])

        def zero_state():
            for hd in range(8):
                P.op("dve", lambda e, hd=hd: e.memset(Sst[:, hd, :], 0.0), [], [BS[hd]])

        def rotary_tables(b, tok0):
            P.dma("sp", lambda e: e.dma_start(out=posi[:], in_=pos_d[b:b + 1, tok0:tok0 + 512].partition_broadcast(128)), [], [Bposi], Bposi)
            ang, Bang = gwk()
            tf, Btf = gwk()
            ti = tf[:].bitcast(I32)
            P.op("dve", lambda e: e.tensor_copy(out=ang[:], in_=posi[:]), [Bposi], [Bang])
            P.op("dve", lambda e: e.tensor_scalar(out=ang[:], in0=ang[:], scalar1=C(C_INVF), scalar2=None, op0=ALU.mult), [Bang, Bcst], [Bang])
            for k, shift in ((1, 0.0), (0, float(np.pi / 2))):
                R = cs[:, k, :]
                P.op("dve", lambda e, shift=shift: e.tensor_scalar(out=tf[:], in0=ang[:], scalar1=shift, scalar2=float(1 / (2 * np.pi)), op0=ALU.add, op1=ALU.mult), [Bang], [Btf])
                sc, Bsc = gwk()
                P.op("dve", lambda e, sc=sc: e.tensor_copy(out=sc[:].bitcast(I32), in_=tf[:]), [Btf], [Bsc])
                P.op("dve", lambda e, sc=sc: e.tensor_copy(out=tf[:], in_=sc[:].bitcast(I32)), [Bsc], [Btf])
                P.op("dve", lambda e, R=R: e.scalar_tensor_tensor(out=R, in0=tf[:], scalar=float(-2 * np.pi), in1=ang[:], op0=ALU.mult, op1=ALU.add), [Btf, Bang], [Bcs])
                if shift != 0.0:
                    P.op("dve", lambda e, R=R, shift=shift: e.tensor_scalar_add(out=R, in0=R, scalar1=shift), [Bcs], [Bcs])
                P.op("dve", lambda e, R=R: e.tensor_single_scalar(out=tf[:], in_=R, scalar=float(np.pi), op=ALU.is_gt), [Bcs], [Btf])
                P.op("dve", lambda e, R=R: e.scalar_tensor_tensor(out=R, in0=tf[:], scalar=float(-2 * np.pi), in1=R, op0=ALU.mult, op1=ALU.add), [Btf, Bcs], [Bcs])
                P.op("dve", lambda e, R=R: e.tensor_single_scalar(out=tf[:], in_=R, scalar=float(-np.pi), op=ALU.is_lt), [Bcs], [Btf])
                P.op("dve", lambda e, R=R: e.scalar_tensor_tensor(out=R, in0=tf[:], scalar=float(2 * np.pi), in1=R, op0=ALU.mult, op1=ALU.add), [Btf, Bcs], [Bcs])
                P.op("act", lambda e, R=R: e.activation(out=R, in_=R, func=AF.Sin), [Bcs], [Bcs])
            P.op("dve", lambda e: e.tensor_scalar(out=cs[:, 1, :], in0=cs[:, 1, :], scalar1=C(C_SIGN), scalar2=None, op0=ALU.mult), [Bcs, Bcst], [Bcs])

        hctr = [0]
        dbg_toks = []
        pend = [None]

        def even_mixer(b, l):
            e_ = l // 2
            win_ap = W["ab_w_in"][e_]
            load_wo(W["ab_w_out"][e_])
            zero_state()
            for tile4 in range(4):
                tok0 = tile4 * 512
                rotary_tables(b, tok0)
                for hd in range(8):
                    s = hctr[0] % 2
                    hctr[0] += 1
                    if hd >= 4 and DBG_FLIP:
                        s = 1 - s
                    Bw = Bwin[s]
                    if hd < 4:
                        hh = hd
                        cq, ck, cv, cg = hh * 128, 512 + hh * 128, 1024 + hh * 128, 1536 + hh * 128
                        load_cols(s, 0, win_ap, cq, 128, 0)
                        load_cols(s, 1, win_ap, cq + 64, 64, 128)
                        load_cols(s, 2, win_ap, cq, 64, 192)
                        load_cols(s, 3, win_ap, ck, 128, 256)
                        load_cols(s, 4, win_ap, ck + 64, 64, 384)
                        load_cols(s, 5, win_ap, ck, 64, 448)
                        load_cols(s, 6, win_ap, cv, 128, 512)
                        load_cols(s, 7, win_ap, cg, 128, 640)
                        vproj_a(s, 512, tok0)
                        for (o0, dst, Bdst, dtab) in ((0, qh[s], Bqh[s], C_DQ), (256, kh[s], Bkh[s], C_DK)):
                            b1, Bb1 = gbank()
                            proj_fm(b1, Bb1, win[s], Bw, o0, 128, tok0)
                            b2, Bb2 = gbank()
                            proj_fm(b2, Bb2, win[s], Bw, o0 + 128, 128, tok0)
                            t1, Bt1 = gwk()
                            t2, Bt2 = gwk()
                            P.op("dve", lambda e, b1=b1, t1=t1: e.tensor_tensor(out=t1[:], in0=b1[:], in1=cs[:, 0, :], op=ALU.mult), [Bb1, Bcs], [Bt1])
                            P.op("dve", lambda e, b2=b2, t2=t2: e.tensor_tensor(out=t2[:], in0=b2[:], in1=cs[:, 1, :], op=ALU.mult), [Bb2, Bcs], [Bt2])
                            P.op("dve", lambda e, t1=t1, t2=t2: e.tensor_tensor(out=t1[:], in0=t1[:], in1=t2[:], op=ALU.add), [Bt1, Bt2], [Bt1])
                            P.op("dve", lambda e, t1=t1, dst=dst, dtab=dtab, hh=hh: e.tensor_tensor(out=dst[:].rearrange("p (c i) -> p c i", c=8), in0=t1[:].rearrange("p (c i) -> p c i", c=8), in1=cst[:, dtab + hh * 64:dtab + (hh + 1) * 64].unsqueeze(1).to_broadcast([128, 8, 64]), op=ALU.mult), [Bt1, Bcst], [Bdst])
                        bg, Bbg = gbank()
                        proj_fm(bg, Bbg, win[s], Bw, 640, 128, tok0)
                        silu_to(gsl[s][:], [Bgsl[s]], bg, Bbg)
                        vproj_b(s)
                        if pend[0] is not None:
                            core_b(*pend[0])
                        core_a(s, hd, 128, RET_A[hh])
                        pend[0] = (s, hd, 128, 0 + e_)
                    else:
                        hh = hd - 4
                        cq, ck, cv, cg = 2048 + hh * 64, 2304 + hh * 64, 2560 + hh * 128, 3072 + hh * 128
                        load_cols(s, 0, win_ap, cq, 64, 0)
                        load_cols(s, 1, win_ap, ck, 64, 64)
                        if hh == 0:
                            load_cols(s, 2, win_ap, 3584, 16, 128)
                        load_cols(s, 6, win_ap, cv, 128, 512)
                        load_cols(s, 7, win_ap, cg, 128, 640)
                        vproj_a(s, 512, tok0)
                        if hh == 0:
                            bb, Bbb = gbank()
                            proj_fm(bb, Bbb, win[s], Bw, 128, 16, tok0)
                            P.op("act", lambda e, bb=bb: e.copy(out=bab[:], in_=bb[0:16, :]), [Bbb], [Bbab])
                        bx, Bbx = gbank()
                        P.op("pe", lambda e, bx=bx, hh=hh: e.matmul(bx[0:64, :], lhsT=wa2[:, e_, hh * 64:(hh + 1) * 64], rhs=bab[:], start=True, stop=True), [Bwa2, Bbab], [Bbx])
                        bq, Bbq = gbank()
                        proj_fm(bq, Bbq, win[s], Bw, 0, 64, tok0)
                        bk, Bbk = gbank()
                        proj_fm(bk, Bbk, win[s], Bw, 64, 64, tok0)
                        L, BL = gwk()
                        P.op("act", lambda e, bx=bx, L=L, hh=hh: e.activation(out=L[0:64, :], in_=bx[0:64, :], func=AF.Exp, scale=-1.0, bias=ba2[:, e_, hh:hh + 1]), [Bbx, Bba2], [BL])
                        P.op("act", lambda e, L=L: e.activation(out=L[0:64, :], in_=L[0:64, :], func=AF.Ln, scale=1.0, bias=C(C_ONE, rows=64)), [BL, Bcst], [BL])
                        G, BG = gwk()
                        P.op("dve", lambda e, L=L, G=G: e.tensor_tensor_scan(out=G[0:64, :], data0=cst[0:64, C_RESET:C_RESET + 512], data1=L[0:64, :], initial=0.0, op0=ALU.mult, op1=ALU.add), [BL, Bcst], [BG])
                        E1, BE1 = gwk()
                        P.op("act", lambda e, G=G, L=L: e.activation(out=L[0:64, :], in_=G[0:64, :], func=AF.Exp, scale=1.0 / 16.0), [BG], [BL])
                        P.op("dve", lambda e, bk=bk, L=L, s=s: e.tensor_tensor(out=kh[s][0:64, :], in0=bk[0:64, :], in1=L[0:64, :], op=ALU.mult), [Bbk, BL], [Bkh[s]])
                        P.op("act", lambda e, G=G, E1=E1: e.activation(out=E1[0:64, :], in_=G[0:64, :], func=AF.Exp, scale=-1.0 / 16.0), [BG], [BE1])
                        P.op("dve", lambda e, bq=bq, E1=E1, s=s: e.scalar_tensor_tensor(out=qh[s][0:64, :], in0=bq[0:64, :], scalar=0.125, in1=E1[0:64, :], op0=ALU.mult, op1=ALU.mult), [Bbq, BE1], [Bqh[s]])
                        P.op("act", lambda e, G=G, s=s: e.activation(out=acol[s][0:64, :], in_=G[0:64, :].rearrange("p (c i) -> p c i", c=8)[:, :, 63], func=AF.Exp, scale=-1.0 / 16.0), [BG], [Bacol[s]])
                        bg, Bbg = gbank()
                        proj_fm(bg, Bbg, win[s], Bw, 640, 128, tok0)
                        silu_to(gsl[s][:], [Bgsl[s]], bg, Bbg)
                        vproj_b(s)
                        if pend[0] is not None:
                            core_b(*pend[0])
                        core_a(s, hd, 64, None)
                        pend[0] = (s, hd, 64, 2 + e_)
                core_b(*pend[0])
                pend[0] = None
                outproj(W["ab_w_out"][e_], tile4)

        def odd_mixer(b, l):
            e_ = l // 2
            win_ap = W["c_w_in"][e_]
            load_wo(W["c_w_out"][e_])
            zero_state()
            for tile4 in range(4):
                tok0 = tile4 * 512
                for hd in range(8):
                    s = hctr[0] % 2
                    hctr[0] += 1
                    Bw = Bwin[s]
                    load_cols(s, 0, win_ap, hd * 128, 128, 0)
                    load_cols(s, 1, win_ap, 1024 + hd * 128, 128, 128)
                    load_cols(s, 6, win_ap, 2048 + hd * 128, 128, 512)
                    load_cols(s, 7, win_ap, 3072 + hd * 128, 128, 640)
                    vproj_a(s, 512, tok0)
                    bf_, Bbf = gbank()
                    proj_fm(bf_, Bbf, win[s], Bw, 128, 128, tok0)
                    sg, Bsg = gwk()
                    P.op("act", lambda e, bf_=bf_, sg=sg: e.activation(out=sg[:], in_=bf_[:], func=AF.Sigmoid), [Bbf], [Bsg])
                    bq, Bbq = gbank()
                    proj_fm(bq, Bbq, win[s], Bw, 0, 128, tok0)
                    qs, Bqs = gwk()
                    silu_to(qs[:], [Bqs], bq, Bbq)
                    bg, Bbg = gbank()
                    proj_fm(bg, Bbg, win[s], Bw, 640, 128, tok0)
                    silu_to(gsl[s][:], [Bgsl[s]], bg, Bbg)
                    G, BG = gwk()
                    P.op("dve", lambda e, sg=sg, G=G, hd=hd: e.tensor_scalar(out=G[:], in0=sg[:], scalar1=oml[:, l, hd:hd + 1], scalar2=lbt[:, l, hd:hd + 1], op0=ALU.mult, op1=ALU.add), [Bsg, Blbt], [BG])
                    P.op("act", lambda e, G=G: e.activation(out=G[:], in_=G[:], func=AF.Ln), [BG], [BG])
                    P.op("dve", lambda e, sg=sg, hd=hd: e.tensor_scalar(out=sg[:], in0=sg[:], scalar1=noml[:, l, hd:hd + 1], scalar2=oml[:, l, hd:hd + 1], op0=ALU.mult, op1=ALU.add), [Bsg, Blbt], [Bsg])
                    G2, BG2 = gwk()
                    P.op("dve", lambda e, G=G, G2=G2: e.tensor_tensor_scan(out=G2[:], data0=cst[:, C_RESET:C_RESET + 512], data1=G[:], initial=0.0, op0=ALU.mult, op1=ALU.add), [BG, Bcst], [BG2])
                    P.op("act", lambda e, G2=G2, G=G: e.activation(out=G[:], in_=G2[:], func=AF.Exp, scale=-1.0), [BG2], [BG])
                    P.op("dve", lambda e, sg=sg, G=G, s=s: e.tensor_tensor(out=kh[s][:], in0=sg[:], in1=G[:], op=ALU.mult), [BG, Bsg], [Bkh[s]])
                    P.op("act", lambda e, G2=G2, s=s: e.activation(out=acol[s][:], in_=G2[:].rearrange("p (c i) -> p c i", c=8)[:, :, 63], func=AF.Exp), [BG2], [Bacol[s]])
                    P.op("act", lambda e, G2=G2: e.activation(out=G2[:], in_=G2[:], func=AF.Exp), [BG2], [BG2])
                    P.op("dve", lambda e, qs=qs, G2=G2, s=s: e.scalar_tensor_tensor(out=qh[s][:], in0=qs[:], scalar=float(128.0 ** -0.5), in1=G2[:], op0=ALU.mult, op1=ALU.mult), [Bqs, BG2], [Bqh[s]])
                    vproj_b(s)
                    if pend[0] is not None:
                        core_b(*pend[0])
                    core_a(s, hd, 128, None)
                    pend[0] = (s, hd, 128, 4 + e_)
                core_b(*pend[0])
                pend[0] = None
                outproj(W["c_w_out"][e_], tile4)

        fctr = [0]
        dctr = [0]

        def swiglu_expert(wg_ap, wu_ap, wd_ap, F, gate_col):
            nchunks = F // 128
            c0 = 0
            while c0 < nchunks:
                ncg = min(4, nchunks - c0)
                s = fctr[0] % 2
                fctr[0] += 1
                f0 = c0 * 128
                nf = ncg * 128
                P.dma("pool", lambda e, s=s, f0=f0, nf=nf: e.dma_start(out=wg[s][:, :, 0:nf], in_=wg_ap.rearrange("(kc p) f -> p kc f", p=128)[:, :, f0:f0 + nf]), [], [Bwg[s]], Bwg[s])
                P.dma("pool", lambda e, s=s, f0=f0, nf=nf: e.dma_start(out=wu[s][:, :, 0:nf], in_=wu_ap.rearrange("(kc p) f -> p kc f", p=128)[:, :, f0:f0 + nf]), [], [Bwu[s]], Bwu[s])
                P.dma("pool", lambda e, s=s, f0=f0, ncg=ncg: e.dma_start(out=wd[s][:, 0:ncg, :], in_=wd_ap[f0:f0 + ncg * 128, :].rearrange("(fc p) d -> p fc d", p=128)), [], [Bwd[s]], Bwd[s])
                for tg in range(4):
                    tok0 = tg * 512
                    hs = (fctr[0] * 4 + tg) % 2
                    for fc in range(ncg):
                        Pg, BPg = pb[0 + (fc % 2)], Bpb[0 + (fc % 2)]
                        Pu, BPu = pb[2 + (fc % 2)], Bpb[2 + (fc % 2)]
                        for kc in range(8):
                            P.op("pe", lambda e, kc=kc, fc=fc, Pg=Pg, s=s, tok0=tok0: e.matmul(Pg[:], lhsT=wg[s][:, kc, fc * 128:(fc + 1) * 128], rhs=hnT[:, kc, tok0:tok0 + 512], start=(kc == 0), stop=(kc == 7)), [Bwg[s]] + BhnT[tg * 4:tg * 4 + 4], [BPg])
                        for kc in range(8):
                            P.op("pe", lambda e, kc=kc, fc=fc, Pu=Pu, s=s, tok0=tok0: e.matmul(Pu[:], lhsT=wu[s][:, kc, fc * 128:(fc + 1) * 128], rhs=hnT[:, kc, tok0:tok0 + 512], start=(kc == 0), stop=(kc == 7)), [Bwu[s]] + BhnT[tg * 4:tg * 4 + 4], [BPu])
                        sg, Bsg = gwk()
                        P.op("act", lambda e, Pg=Pg, sg=sg: e.activation(out=sg[:], in_=Pg[:], func=AF.Silu), [BPg], [Bsg])
                        P.op("dve", lambda e, Pu=Pu, sg=sg, hs=hs, fc=fc: e.tensor_tensor(out=hid[hs][:, fc, :], in0=Pu[:], in1=sg[:], op=ALU.mult), [BPu, Bsg], [Bhid[hs][fc]])
                    for sub in range(4):
                        t = tg * 4 + sub
                        for dh in range(2):
                            k = 4 + (dctr[0] % 4)
                            dctr[0] += 1
                            Pd, BPd = pb[k], Bpb[k]
                            for fc in range(ncg):
                                P.op("pe", lambda e, fc=fc, sub=sub, dh=dh, Pd=Pd, s=s, hs=hs: e.matmul(Pd[:], lhsT=hid[hs][:, fc, sub * 128:(sub + 1) * 128], rhs=wd[s][:, fc, dh * 512:(dh + 1) * 512], start=(fc == 0), stop=(fc == ncg - 1)), [Bhid[hs][fc], Bwd[s]], [BPd])
                            hsl = h[:, t, dh * 512:(dh + 1) * 512]
                            if gate_col is None:
                                P.op("dve", lambda e, Pd=Pd, hsl=hsl: e.tensor_tensor(out=hsl, in0=Pd[:], in1=hsl, op=ALU.add), [BPd, Bh[t]], [Bh[t]])
                            else:
                                P.op("dve", lambda e, Pd=Pd, hsl=hsl, t=t: e.scalar_tensor_tensor(out=hsl, in0=Pd[:], scalar=gt[:, t, gate_col:gate_col + 1], in1=hsl, op0=ALU.mult, op1=ALU.add), [BPd, Bh[t], Bgt], [Bh[t]])
                c0 += ncg

        def router(e_):
            P.dma("pool", lambda e: e.dma_start(out=wr[:], in_=W["moe_router"][e_].rearrange("(kc p) n -> p kc n", p=128)), [], [Bwr], Bwr)
            for t in range(NT):
                bank, Bb = gbank()
                for kc in range(8):
                    P.op("pe", lambda e, kc=kc, t=t, bank=bank: e.matmul(bank[:, 0:8], lhsT=hnT[:, kc, t * 128:(t + 1) * 128], rhs=wr[:, kc, :], start=(kc == 0), stop=(kc == 7)), [BhnT[t], Bwr], [Bb])
                P.op("act", lambda e, t=t, bank=bank: e.copy(out=lg[:, t, :], in_=bank[:, 0:8]), [Bb], [Blg])
            m1 = stat[:, 32:48]
            m2 = stat[:, 48:64]

            def bc(a):
                return a.unsqueeze(2).to_broadcast([128, NT, 8])
            P.op("dve", lambda e: e.tensor_reduce(out=m1, in_=lg[:], axis=AX.X, op=ALU.max), [Blg], [Bstat])
            P.op("dve", lambda e: e.tensor_tensor(out=rt[0][:], in0=lg[:], in1=bc(m1), op=ALU.is_equal), [Blg, Bstat], [Brt[0]])
            P.op("dve", lambda e: e.scalar_tensor_tensor(out=rt[1][:], in0=rt[0][:], scalar=-1e30, in1=lg[:], op0=ALU.mult, op1=ALU.add), [Brt[0], Blg], [Brt[1]])
            P.op("dve", lambda e: e.tensor_reduce(out=m2, in_=rt[1][:], axis=AX.X, op=ALU.max), [Brt[1]], [Bstat])
            P.op("dve", lambda e: e.tensor_tensor(out=rt[0][:], in0=lg[:], in1=bc(m2), op=ALU.is_ge), [Blg, Bstat], [Brt[0]])
            P.op("dve", lambda e: e.tensor_tensor(out=rt[1][:], in0=lg[:], in1=bc(m1), op=ALU.subtract), [Blg, Bstat], [Brt[1]])
            P.op("act", lambda e: e.activation(out=rt[1][:], in_=rt[1][:], func=AF.Exp), [Brt[1]], [Brt[1]])
            P.op("dve", lambda e: e.tensor_tensor(out=rt[1][:], in0=rt[1][:], in1=rt[0][:], op=ALU.mult), [Brt[0], Brt[1]], [Brt[1]])
            P.op("dve", lambda e: e.tensor_reduce(out=m1, in_=rt[1][:], axis=AX.X, op=ALU.add), [Brt[1]], [Bstat])
            P.op("dve", lambda e: e.reciprocal(out=m1, in_=m1), [Bstat], [Bstat])
            P.op("dve", lambda e: e.tensor_tensor(out=gt[:], in0=rt[1][:], in1=bc(m1), op=ALU.mult), [Brt[1], Bstat], [Bgt])

        final_toks = []
        for b in range(nseq):
            for q4 in range(4):
                P.dma("sp", lambda e, b=b, q4=q4: e.dma_start(out=h[:, q4 * 4:(q4 + 1) * 4, :], in_=x_d[b, q4 * 512:(q4 + 1) * 512, :].rearrange("(t p) d -> p t d", p=128)), [], Bh[q4 * 4:q4 * 4 + 4], Bh[q4 * 4])
            sub = 0
            for l in range(4):
                e_ = l // 2
                if sub < nsub and (ONLY is None or sub in ONLY):
                    norm(W["norm_mix_g"][l:l + 1, :])
                    alias_sync(ffn_bufs(), mixer_bufs())
                    if l % 2 == 0:
                        even_mixer(b, l)
                    else:
                        odd_mixer(b, l)
                sub += 1
                if sub < nsub and (ONLY is None or sub in ONLY):
                    norm(W["norm_ffn_g"][l:l + 1, :])
                    alias_sync(mixer_bufs(), ffn_bufs())
                    if l % 2 == 0:
                        swiglu_expert(W["ffn_w_gate"][e_], W["ffn_w_up"][e_], W["ffn_w_down"][e_], 2816, None)
                    else:
                        router(e_)
                        for ex in range(8):
                            swiglu_expert(W["moe_w_gate"][e_, ex], W["moe_w_up"][e_, ex], W["moe_w_down"][e_, ex], 3584, ex)
                sub += 1
            if final:
                final_toks = norm(W["final_norm_g"].rearrange("(o d) -> o d", o=1), to_out=out_d[b])
            else:
                final_toks = []
                for t in range(NT):
                    final_toks.append(P.dma("sp", lambda e, t=t, b=b: e.dma_start(out=out_d[b, t * 128:(t + 1) * 128, :], in_=h[:, t, :]), [Bh[t]], [], Bh[t]))
        P.emit(final_toks + dbg_toks)
        print("instr counts", {k: len(v) for k, v in P.q.items()}, "sems", P.nsem, flush=True)
    return nc


_NC_CACHE = {}


def run(inputs, nseq_per_core, nsub=8, final=True, ncores=8):
    key = (nseq_per_core, nsub, final)
    if key not in _NC_CACHE:
        _NC_CACHE[key] = build(nseq_per_core, nsub, final)
    nc = _NC_CACHE[key]
    consts = make_consts()
    in_maps = []
    for c in range(ncores):
        m = {n: np.ascontiguousarray(inputs[n], dtype=np.float32) for n, _ in WNAMES}
        m["x"] = np.ascontiguousarray(inputs["x"][c * nseq_per_core:(c + 1) * nseq_per_core], dtype=np.float32)
        m["positions"] = np.ascontiguousarray(inputs["positions"][c * nseq_per_core:(c + 1) * nseq_per_core], dtype=np.int32)
        m["consts"] = consts
        in_maps.append(m)
    res = run_bass_kernel_spmd(nc, in_maps, core_ids=list(range(ncores)))
    if DBG:
        np.save("dbg_out.npy", res.results[0]["dbg"])
    return np.concatenate([r["out"] for r in res.results], axis=0)


def kernel(**inputs):
    return run(inputs, 4).astype(np.float32)
```

```python
import numpy as np
from contextlib import ExitStack
import concourse.bass as bass
import concourse.mybir as mybir
from concourse.bass_utils import run_bass_kernel_spmd

F32 = mybir.dt.float32
BF16 = mybir.dt.bfloat16
I32 = mybir.dt.int32
AF = mybir.ActivationFunctionType
ALU = mybir.AluOpType
AX = mybir.AxisListType

D = 1024
DBG_FLIP = False
DBG = False
ONLY = None
T = 2048
NT = 16
EPS = 1e-6


class Tok:
    __slots__ = ("sem", "val", "eng")

    def __init__(self, sem, val, eng):
        self.sem = sem
        self.val = val
        self.eng = eng


class Buf:
    __slots__ = ("name", "w", "r", "dsem", "dcnt")

    def __init__(self, name):
        self.name = name
        self.w = None
        self.r = {}
        self.dsem = None
        self.dcnt = 0


class Prog:
    ENGS = ("pe", "act", "dve", "pool", "sp")

    def __init__(self, nc, stack):
        self.nc = nc
        self.stack = stack
        self.q = {e: [] for e in self.ENGS}
        self.cnt = {e: 0 for e in self.ENGS}
        self.sem = {e: stack.enter_context(nc.semaphore("s_" + e)) for e in ("pe", "act", "dve", "pool")}
        self.waited = {e: {} for e in self.ENGS}
        self.nsem = 4
        self.last_dma = None

    def _deps(self, eng, reads, writes):
        need = {}

        def add(t):
            if t is None:
                return
            if t.eng == "pe" and eng == "pe":
                return
            k = id(t.sem)
            if k not in need or need[k][1] < t.val:
                need[k] = (t.sem, t.val)

        for b in reads:
            add(b.w)
        for b in writes:
            add(b.w)
            for t in b.r.values():
                add(t)
        wd = self.waited[eng]
        for k, (sem, val) in need.items():
            if wd.get(k, -1) >= val:
                continue
            wd[k] = val
            self.q[eng].append(("wait", sem, val))

    def _mark(self, tok, reads, writes):
        for b in reads:
            b.r[id(tok.sem)] = tok
        for b in writes:
            b.w = tok
            b.r = {}

    def op(self, eng, fn, reads=(), writes=()):
        self._deps(eng, reads, writes)
        self.cnt[eng] += 1
        tok = Tok(self.sem[eng], self.cnt[eng], eng)
        self.q[eng].append(("op", fn, self.sem[eng], 1))
        self._mark(tok, reads, writes)
        return tok

    def dma(self, qeng, fn, reads, writes, primary):
        self._deps(qeng, reads, writes)
        if primary.dsem is None:
            primary.dsem = self.stack.enter_context(self.nc.semaphore("d_" + primary.name))
            self.nsem += 1
        primary.dcnt += 16
        tok = Tok(primary.dsem, primary.dcnt, None)
        self.q[qeng].append(("op", fn, primary.dsem, 16))
        self._mark(tok, reads, writes)
        return tok

    def emit(self, final_toks):
        nc = self.nc
        for t in final_toks:
            self.q["sp"].append(("wait", t.sem, t.val))

        def run(e, items):
            for it in items:
                if it[0] == "wait":
                    e.wait_ge(it[1], it[2])
                else:
                    it[1](e).then_inc(it[2], it[3])

        with nc.Block() as block:
            @block.tensor
            def _(e):
                run(e, self.q["pe"])

            @block.scalar
            def _(e):
                run(e, self.q["act"])

            @block.vector
            def _(e):
                run(e, self.q["dve"])

            @block.gpsimd
            def _(e):
                run(e, self.q["pool"])

            @block.sync
            def _(e):
                run(e, self.q["sp"])


C_ID = 0
C_ONES = 128
C_MASK = 256
C_RESET = 320
C_INVF = 832
C_SIGN = 833
C_EPS = 834
C_ONE = 835
C_NEGPI = 836
C_DQ = 840
C_DK = 840 + 256
NCONST = 840 + 512


def make_consts():
    c = np.zeros((128, NCONST), np.float32)
    c[:, C_ID:C_ID + 128] = np.eye(128, dtype=np.float32)
    c[:, C_ONES:C_ONES + 128] = 1.0
    j = np.arange(64)[:, None]
    i = np.arange(64)[None, :]
    c[0:64, C_MASK:C_MASK + 64] = (j <= i).astype(np.float32)
    r = np.ones(512, np.float32)
    r[::64] = 0.0
    c[:, C_RESET:C_RESET + 512] = r[None, :]
    p = np.arange(128)
    c[:, C_INVF] = (10000.0 ** (-(2.0 * (p % 64)) / 128.0)).astype(np.float32)
    c[:, C_SIGN] = np.where(p < 64, -1.0, 1.0)
    c[:, C_EPS] = EPS
    c[:, C_ONE] = 1.0
    c[:, C_NEGPI] = -np.pi
    for hh in range(4):
        lg = np.log1p(-np.exp2(-5.0 - hh))
        t = np.arange(64, dtype=np.float64)
        c[:, C_DQ + hh * 64:C_DQ + (hh + 1) * 64] = np.exp((t + 1.0) * lg)[None, :]
        c[:, C_DK + hh * 64:C_DK + (hh + 1) * 64] = (np.exp(-(t + 1.0) * lg) * 128.0 ** -0.5)[None, :]
    return c


RET_A = [float(np.exp(64.0 * np.log1p(-np.exp2(-5.0 - hh)))) for hh in range(4)]

WNAMES = [("norm_mix_g", [4, 1024]), ("norm_ffn_g", [4, 1024]), ("final_norm_g", [1024]),
          ("ab_w_in", [2, 1024, 3600]), ("gla_w_a2", [2, 16, 256]), ("gla_b_a2", [2, 256]),
          ("ret_norm_g", [2, 128]), ("gla_norm_g", [2, 128]), ("ab_w_out", [2, 1024, 1024]),
          ("ffn_w_gate", [2, 1024, 2816]), ("ffn_w_up", [2, 1024, 2816]), ("ffn_w_down", [2, 2816, 1024]),
          ("hgrn_lb_logits", [4, 1024]), ("c_w_in", [2, 1024, 4096]), ("hgrn_norm_g", [2, 128]),
          ("c_w_out", [2, 1024, 1024]), ("moe_router", [2, 1024, 8]), ("moe_w_gate", [2, 8, 1024, 3584]),
          ("moe_w_up", [2, 8, 1024, 3584]), ("moe_w_down", [2, 8, 3584, 1024])]


def build(nseq, nsub=8, final=True):
    nc = bass.Bass("TRN2", target_bir_lowering=False)
    x_d = nc.dram_tensor("x", [nseq, T, D], F32, kind="ExternalInput").ap()
    pos_d = nc.dram_tensor("positions", [nseq, T], I32, kind="ExternalInput").ap()
    W = {n: nc.dram_tensor(n, s, F32, kind="ExternalInput").ap() for n, s in WNAMES}
    cst_d = nc.dram_tensor("consts", [128, NCONST], F32, kind="ExternalInput").ap()
    out_d = nc.dram_tensor("out", [nseq, T, D], F32, kind="ExternalOutput").ap()
    dbg_d = nc.dram_tensor("dbg", [128, 8, 512], F32, kind="ExternalOutput").ap() if DBG else None

    with ExitStack() as st:
        P = Prog(nc, st)

        def sb(n, s, d):
            return st.enter_context(nc.sbuf_tensor(n, s, d))

        h = sb("h", [128, NT, D], F32)
        Bh = [Buf("h%d" % t) for t in range(NT)]
        hnT = sb("hnT", [128, 8, T], BF16)
        BhnT = [Buf("hnT%d" % t) for t in range(NT)]
        cst = sb("cst", [128, NCONST], F32)
        Bcst = Buf("cst")
        idb = sb("idb", [128, 128], BF16)
        Bidb = Buf("idb")
        gbc = sb("gbc", [128, D], F32)
        Bgbc = Buf("gbc")
        hntok = [sb("hntok%d" % i, [128, D], BF16) for i in range(2)]
        Bhntok = [Buf("hntok%d" % i) for i in range(2)]
        stat = sb("stat", [128, 64], F32)
        Bstat = Buf("stat")
        junk = hntok[1]
        Bjunk = Bhntok[1]
        lbt = sb("lbt", [128, 4, 8], F32)
        Blbt = Buf("lbt")
        oml = sb("oml", [128, 4, 8], F32)
        noml = sb("noml", [128, 4, 8], F32)
        hng = sb("hng", [128, 8], F32)
        Bhng = Buf("hng")
        ba2 = sb("ba2", [64, 2, 4], F32)
        Bba2 = Buf("ba2")
        wa2f = sb("wa2f", [16, 2, 256], F32)
        wa2 = sb("wa2", [16, 2, 256], BF16)
        Bwa2 = Buf("wa2")
        NW = 6
        wk = [sb("wk%d" % i, [128, 512], F32) for i in range(NW)]
        Bwk = [Buf("wk%d" % i) for i in range(NW)]
        arena = sb("arena", [128, 24576], BF16)
        win = [arena[:, i * 6144:(i + 1) * 6144].rearrange("p (k f) -> p k f", k=8) for i in range(2)]
        Bwin = [[Buf("win%d_%d" % (i, j)) for j in range(8)] for i in range(2)]
        qh = [sb("qh%d" % i, [128, 512], BF16) for i in range(2)]
        Bqh = [Buf("qh%d" % i) for i in range(2)]
        kh = [sb("kh%d" % i, [128, 512], BF16) for i in range(2)]
        Bkh = [Buf("kh%d" % i) for i in range(2)]
        vt = [sb("vt%d" % i, [64, 8, 128], BF16) for i in range(2)]
        Bvt = [Buf("vt%d" % i) for i in range(2)]
        gsl = [sb("gsl%d" % i, [128, 512], BF16) for i in range(2)]
        Bgsl = [Buf("gsl%d" % i) for i in range(2)]
        acol = [sb("acol%d" % i, [128, 8], F32) for i in range(2)]
        Bacol = [Buf("acol%d" % i) for i in range(2)]
        bab = sb("bab", [16, 512], BF16)
        Bbab = Buf("bab")
        Sst = sb("Sst", [128, 8, 128], F32)
        arena2 = sb("arena2", [128, 4096], BF16)
        Sbf = arena2[:, 3072:4096].rearrange("p (k f) -> p k f", k=8)
        BS = [Buf("S%d" % i) for i in range(8)]
        BSb = [Buf("Sb%d" % i) for i in range(8)]
        Sa = sb("Sa", [128, 128], F32)
        BSa = Buf("Sa")
        pTall = [sb("pTall%d" % i, [64, 8, 64], BF16) for i in range(2)]
        BpTall = [Buf("pTall%d" % i) for i in range(2)]
        ktkall = [sb("ktkall%d" % i, [64, 8, 128], BF16) for i in range(2)]
        Bktkall = [Buf("ktkall%d" % i) for i in range(2)]
        Sball = [arena2[:, 3072:4096].rearrange("p (k f) -> p k f", k=8), sb("Sball1", [128, 8, 128], BF16)]
        BSball = [Buf("Sball%d" % i) for i in range(2)]
        og = arena[:, 20480:24576].rearrange("p (k f) -> p k f", k=8)
        Bog = [Buf("og%d" % i) for i in range(8)]
        cs = arena2[:, 0:2048].bitcast(F32).rearrange("p (k f) -> p k f", k=2)
        Bcs = Buf("cs")
        posi = arena2[:, 2048:3072].bitcast(I32)
        Bposi = Buf("posi")
        wo = arena[:, 12288:20480].rearrange("p (k f) -> p k f", k=8)
        Bwo = [Buf("wo0"), Buf("wo1")]
        wg = [arena[:, i * 4096:(i + 1) * 4096].rearrange("p (k f) -> p k f", k=8) for i in range(2)]
        wu = [arena[:, 8192 + i * 4096:8192 + (i + 1) * 4096].rearrange("p (k f) -> p k f", k=8) for i in range(2)]
        wd = [arena[:, 16384 + i * 4096:16384 + (i + 1) * 4096].rearrange("p (k f) -> p k f", k=4) for i in range(2)]
        Bwg = [Buf("wg%d" % i) for i in range(2)]
        Bwu = [Buf("wu%d" % i) for i in range(2)]
        Bwd = [Buf("wd%d" % i) for i in range(2)]
        hid = [arena2[:, i * 2048:(i + 1) * 2048].rearrange("p (k f) -> p k f", k=4) for i in range(2)]
        Bhid = [[Buf("hid%d_%d" % (i, j)) for j in range(4)] for i in range(2)]
        wr = sb("wr", [128, 8, 8], BF16)
        Bwr = Buf("wr")
        lg = sb("lg", [128, NT, 8], F32)
        Blg = Buf("lg")
        gt = sb("gt", [128, NT, 8], F32)
        Bgt = Buf("gt")
        rt = [sb("rt%d" % i, [128, NT, 8], F32) for i in range(3)]
        Brt = [Buf("rt%d" % i) for i in range(3)]

        def alias_sync(src, dst):
            toks = {}
            for b_ in src:
                for t_ in ([b_.w] if b_.w is not None else []) + list(b_.r.values()):
                    k_ = id(t_.sem)
                    if k_ not in toks or toks[k_].val < t_.val:
                        toks[k_] = t_
            for b_ in dst:
                for k_, t_ in toks.items():
                    if k_ not in b_.r or b_.r[k_].val < t_.val:
                        b_.r[k_] = t_

        def mixer_bufs():
            return [x_ for l_ in Bwin for x_ in l_] + Bwo + Bog + [Bcs, Bposi, BSball[0]]

        def ffn_bufs():
            return Bwg + Bwu + Bwd + [x_ for l_ in Bhid for x_ in l_]

        pb = [st.enter_context(nc.psum_tensor("pb%d" % i, [128, 512], F32)) for i in range(8)]
        Bpb = [Buf("pb%d" % i) for i in range(8)]
        gctr = [0]

        def gbank():
            i = gctr[0] % 4
            gctr[0] += 1
            return pb[i], Bpb[i]

        wctr = [0]

        def gwk():
            i = wctr[0] % NW
            wctr[0] += 1
            return wk[i], Bwk[i]

        def C(col, n=1, rows=128):
            return cst[0:rows, col:col + n]

        P.dma("sp", lambda e: e.dma_start(out=cst[:], in_=cst_d), [], [Bcst], Bcst)
        P.op("dve", lambda e: e.tensor_copy(out=idb[:], in_=cst[:, C_ID:C_ID + 128]), [Bcst], [Bidb])
        P.dma("sp", lambda e: e.dma_start(out=lbt[:], in_=W["hgrn_lb_logits"].rearrange("l (hh p) -> p l hh", p=128), allow_slow_non_contiguous=True), [], [Blbt], Blbt)
        P.op("act", lambda e: e.activation(out=lbt[:], in_=lbt[:], func=AF.Exp), [Blbt], [Blbt])
        P.op("dve", lambda e: e.tensor_tensor(out=stat[:, 0:8], in0=lbt[:, 0, :], in1=lbt[:, 1, :], op=ALU.add), [Blbt], [Bstat])
        P.op("dve", lambda e: e.tensor_tensor(out=stat[:, 0:8], in0=stat[:, 0:8], in1=lbt[:, 2, :], op=ALU.add), [Blbt, Bstat], [Bstat])
        P.op("dve", lambda e: e.tensor_tensor(out=stat[:, 0:8], in0=stat[:, 0:8], in1=lbt[:, 3, :], op=ALU.add), [Blbt, Bstat], [Bstat])
        P.op("dve", lambda e: e.reciprocal(out=stat[:, 0:8], in_=stat[:, 0:8]), [Bstat], [Bstat])
        for l in range(4):
            P.op("dve", lambda e, l=l: e.tensor_tensor(out=lbt[:, l, :], in0=lbt[:, l, :], in1=stat[:, 0:8], op=ALU.mult), [Blbt, Bstat], [Blbt])
        P.op("dve", lambda e: e.memset(lbt[:, 0, :], 0.0), [Blbt], [Blbt])
        P.op("dve", lambda e: e.tensor_tensor(out=lbt[:, 2, :], in0=lbt[:, 2, :], in1=lbt[:, 1, :], op=ALU.add), [Blbt], [Blbt])
        P.op("dve", lambda e: e.tensor_tensor(out=lbt[:, 3, :], in0=lbt[:, 3, :], in1=lbt[:, 2, :], op=ALU.add), [Blbt], [Blbt])
        P.op("dve", lambda e: e.tensor_scalar(out=oml[:], in0=lbt[:], scalar1=-1.0, scalar2=1.0, op0=ALU.mult, op1=ALU.add), [Blbt], [Blbt])
        P.op("dve", lambda e: e.tensor_scalar(out=noml[:], in0=oml[:], scalar1=-1.0, scalar2=None, op0=ALU.mult), [Blbt], [Blbt])
        P.dma("sp", lambda e: e.dma_start(out=hng[:, 0:2], in_=W["ret_norm_g"].rearrange("l p -> p l"), allow_slow_non_contiguous=True), [], [Bhng], Bhng)
        P.dma("sp", lambda e: e.dma_start(out=hng[:, 2:4], in_=W["gla_norm_g"].rearrange("l p -> p l"), allow_slow_non_contiguous=True), [], [Bhng], Bhng)
        P.dma("sp", lambda e: e.dma_start(out=hng[:, 4:6], in_=W["hgrn_norm_g"].rearrange("l p -> p l"), allow_slow_non_contiguous=True), [], [Bhng], Bhng)
        P.dma("sp", lambda e: e.dma_start(out=ba2[:], in_=W["gla_b_a2"].rearrange("l (hh p) -> p l hh", p=64), allow_slow_non_contiguous=True), [], [Bba2], Bba2)
        P.op("dve", lambda e: e.tensor_scalar(out=ba2[:], in0=ba2[:], scalar1=-1.0, scalar2=None, op0=ALU.mult), [Bba2], [Bba2])
        P.dma("sp", lambda e: e.dma_start(out=wa2f[:], in_=W["gla_w_a2"].rearrange("l r c -> r l c")), [], [Bwa2], Bwa2)
        P.op("dve", lambda e: e.tensor_copy(out=wa2[:], in_=wa2f[:]), [Bwa2], [Bwa2])

        def rstd_from_sumsq(ap, n, bufs):
            P.op("act", lambda e: e.activation(out=ap, in_=ap, func=AF.Ln, scale=1.0 / n, bias=C(C_EPS, rows=ap.shape[0])), bufs + [Bcst], bufs)
            P.op("act", lambda e: e.activation(out=ap, in_=ap, func=AF.Exp, scale=-0.5), bufs, bufs)

        def norm(g_row_ap, to_out=None):
            P.dma("sp", lambda e: e.dma_start(out=gbc[:], in_=g_row_ap.partition_broadcast(128)), [], [Bgbc], Bgbc)
            for t in range(NT):
                P.op("act", lambda e, t=t: e.activation(out=junk[:], in_=h[:, t, :], func=AF.Square, accum_out=stat[:, 16 + t:17 + t]), [Bh[t]], [Bjunk, Bstat])
            rstd_from_sumsq(stat[:, 16:32], float(D), [Bstat])
            toks = []
            for t in range(NT):
                if to_out is not None:
                    for dh in range(2):
                        ot, Bot = gwk()
                        P.op("dve", lambda e, t=t, dh=dh, ot=ot: e.scalar_tensor_tensor(out=ot[:], in0=h[:, t, dh * 512:(dh + 1) * 512], scalar=stat[:, 16 + t:17 + t], in1=gbc[:, dh * 512:(dh + 1) * 512], op0=ALU.mult, op1=ALU.mult), [Bh[t], Bstat, Bgbc], [Bot])
                        toks.append(P.dma("sp", lambda e, t=t, dh=dh, ot=ot: e.dma_start(out=to_out[t * 128:(t + 1) * 128, dh * 512:(dh + 1) * 512], in_=ot[:]), [Bot], [], Bot))
                    continue
                i = t % 2
                P.op("dve", lambda e, t=t, i=i: e.scalar_tensor_tensor(out=hntok[i][:], in0=h[:, t, :], scalar=stat[:, 16 + t:17 + t], in1=gbc[:], op0=ALU.mult, op1=ALU.mult), [Bh[t], Bstat, Bgbc], [Bhntok[i]])
                bank, Bb = gbank()
                bv = bank[:].bitcast(BF16)
                for c in range(8):
                    P.op("pe", lambda e, c=c, i=i, bv=bv: e.transpose(out=bv[:, c * 128:(c + 1) * 128], in_=hntok[i][:, c * 128:(c + 1) * 128], identity=idb[:]), [Bhntok[i], Bidb], [Bb])
                if t % 2 == 0:
                    P.op("act", lambda e, t=t, bv=bv: e.copy(out=hnT[:, :, t * 128:(t + 1) * 128], in_=bv.rearrange("p (c k) -> p c k", c=8)), [Bb], [BhnT[t]])
                else:
                    P.op("dve", lambda e, t=t, bv=bv: e.tensor_copy(out=hnT[:, :, t * 128:(t + 1) * 128], in_=bv.rearrange("p (c k) -> p c k", c=8)), [Bb], [BhnT[t]])
            return toks

        def proj_fm(bank, Bb, wslot, Bw, c0, m, tok0):
            for kc in range(8):
                P.op("pe", lambda e, kc=kc: e.matmul(bank[0:m, :], lhsT=wslot[:, kc, c0:c0 + m], rhs=hnT[:, kc, tok0:tok0 + 512], start=(kc == 0), stop=(kc == 7)),
                     Bw + BhnT[tok0 // 128:tok0 // 128 + 4], [Bb])

        def load_cols(slot, j, w_ap, c0, n, off):
            P.dma("pool", lambda e: e.dma_start(out=win[slot][:, :, off:off + n], in_=w_ap.rearrange("(kc p) f -> p kc f", p=128)[:, :, c0:c0 + n]), [], [Bwin[slot][j]], Bwin[slot][j])

        vstate = {}

        def vproj_a(s, off, tok0):
            bank, Bb = gbank()
            proj_fm(bank, Bb, win[s], Bwin[s], off, 128, tok0)
            vf, Bvf = gwk()
            vfb = vf[:].bitcast(BF16)
            P.op("act", lambda e: e.copy(out=vfb[:, 0:512], in_=bank[:]), [Bb], [Bvf])
            vstate[s] = (vfb, Bvf)

        def vproj_b(s):
            vfb, Bvf = vstate[s]
            bank2, Bb2 = gbank()
            b2v = bank2[:].bitcast(BF16)
            for c in range(8):
                P.op("pe", lambda e, c=c: e.transpose(out=b2v[0:64, c * 128:(c + 1) * 128], in_=vfb[:, c * 64:(c + 1) * 64], identity=idb[:]), [Bvf, Bidb], [Bb2])
            P.op("dve", lambda e: e.tensor_copy(out=vt[s][:], in_=b2v[0:64, :].rearrange("p (c v) -> p c v", c=8)), [Bb2], [Bvt[s]])

        def core_a(s, hd, dk, a_float):
            Ps, BPs = pb[4], Bpb[4]
            Pk, BPk = pb[5], Bpb[5]
            pkv = Pk[:].bitcast(BF16)
            for c in range(8):
                cols = slice(c * 64, (c + 1) * 64)
                P.op("pe", lambda e, cols=cols: e.matmul(Ps[0:64, cols], lhsT=kh[s][0:dk, cols], rhs=qh[s][0:dk, cols], start=True, stop=True), [Bkh[s], Bqh[s]], [BPs])
            for c in range(8):
                cols = slice(c * 64, (c + 1) * 64)
                P.op("pe", lambda e, cols=cols, c=c: e.transpose(out=pkv[0:64, c * 128:c * 128 + dk], in_=kh[s][0:dk, cols], identity=idb[0:dk, 0:dk]), [Bkh[s], Bidb], [BPk])
            P.op("dve", lambda e: e.tensor_tensor(out=pTall[s][:], in0=Ps[0:64, :].rearrange("p (c i) -> p c i", c=8), in1=cst[0:64, C_MASK:C_MASK + 64].unsqueeze(1).to_broadcast([64, 8, 64]), op=ALU.mult), [BPs, Bcst], [BpTall[s]])
            P.op("act", lambda e: e.copy(out=ktkall[s][:, :, 0:dk], in_=pkv[0:64, :].rearrange("p (c k) -> p c k", c=8)[:, :, 0:dk]), [BPk], [Bktkall[s]])
            for c in range(8):
                bank, Bb_ = (Ps, BPs) if c < 4 else (Pk, BPk)
                cc = c % 4
                P.op("pe", lambda e, c=c, cc=cc, bank=bank: e.matmul(bank[0:dk, cc * 128:(cc + 1) * 128], lhsT=ktkall[s][:, c, 0:dk], rhs=vt[s][:, c, :], start=True, stop=True), [Bktkall[s], Bvt[s]], [Bb_])
            P.op("act", lambda e: e.copy(out=Sball[s][0:dk, 0, :], in_=Sst[0:dk, hd, :]), [BS[hd]], [BSball[s]])
            for c in range(8):
                bank, Bb_ = (Ps, BPs) if c < 4 else (Pk, BPk)
                cc = c % 4
                U = bank[0:dk, cc * 128:(cc + 1) * 128]
                P.op("dve", lambda e, U=U: e.tensor_tensor(out=Sa[0:dk, :], in0=U, in1=Sst[0:dk, hd, :], op=ALU.add), [Bb_, BS[hd]], [BSa])
                if a_float is not None:
                    P.op("dve", lambda e: e.tensor_scalar(out=Sst[0:dk, hd, :], in0=Sa[0:dk, :], scalar1=a_float, scalar2=None, op0=ALU.mult), [BSa], [BS[hd]])
                else:
                    P.op("dve", lambda e, c=c: e.tensor_scalar(out=Sst[0:dk, hd, :], in0=Sa[0:dk, :], scalar1=acol[s][0:dk, c:c + 1], scalar2=None, op0=ALU.mult), [BSa, Bacol[s]], [BS[hd]])
                if c < 7:
                    P.op("act", lambda e, c=c: e.copy(out=Sball[s][0:dk, c + 1, :], in_=Sst[0:dk, hd, :]), [BS[hd]], [BSball[s]])

        def core_b(s, hd, dk, gcol):
            Po, BPo = pb[6 + (hd % 2)], Bpb[6 + (hd % 2)]
            for c in range(8):
                cols = slice(c * 64, (c + 1) * 64)
                P.op("pe", lambda e, cols=cols, c=c: e.matmul(Po[:, cols], lhsT=vt[s][:, c, :], rhs=pTall[s][:, c, :], start=True, stop=False), [Bvt[s], BpTall[s]], [BPo])
                P.op("pe", lambda e, cols=cols, c=c: e.matmul(Po[:, cols], lhsT=Sball[s][0:dk, c, :], rhs=qh[s][0:dk, cols], start=False, stop=True), [BSball[s], Bqh[s]], [BPo])
            post(Po, BPo, s, hd, gcol)

        def post(Po, BPo, s, hd, gcol):
            sq, Bsq = gwk()
            P.op("act", lambda e: e.activation(out=sq[:], in_=Po[:], func=AF.Square), [BPo], [Bsq])
            bank, Bb = gbank()
            P.op("pe", lambda e: e.matmul(bank[:], lhsT=cst[:, C_ONES:C_ONES + 128], rhs=sq[:], start=True, stop=True), [Bcst, Bsq], [Bb])
            r, Br = gwk()
            P.op("act", lambda e: e.activation(out=r[:], in_=bank[:], func=AF.Ln, scale=1.0 / 128.0, bias=C(C_EPS)), [Bb, Bcst], [Br])
            P.op("act", lambda e: e.activation(out=r[:], in_=r[:], func=AF.Exp, scale=-0.5), [Br], [Br])
            P.op("dve", lambda e: e.scalar_tensor_tensor(out=r[:], in0=Po[:], scalar=hng[:, gcol:gcol + 1], in1=r[:], op0=ALU.mult, op1=ALU.mult), [BPo, Bhng, Br], [Br])
            P.op("dve", lambda e: e.tensor_tensor(out=og[:, hd, :], in0=r[:], in1=gsl[s][:], op=ALU.mult), [Br, Bgsl[s]], [Bog[hd]])

        def silu_to(out_ap, Bout, bank, Bb, m=128):
            P.op("act", lambda e: e.activation(out=out_ap, in_=bank[0:m, :], func=AF.Silu), [Bb], Bout)

        def outproj(w_ap, tile4):
            for sub in range(4):
                t = tile4 * 4 + sub
                for dh in range(2):
                    bank, Bb = gbank()
                    for f in range(8):
                        P.op("pe", lambda e, f=f, sub=sub, dh=dh, bank=bank: e.matmul(bank[:], lhsT=og[:, f, sub * 128:(sub + 1) * 128], rhs=wo[:, f, dh * 512:(dh + 1) * 512], start=(f == 0), stop=(f == 7)), [Bog[f], Bwo[dh]], [Bb])
                    P.op("dve", lambda e, t=t, dh=dh, bank=bank: e.tensor_tensor(out=h[:, t, dh * 512:(dh + 1) * 512], in0=bank[:], in1=h[:, t, dh * 512:(dh + 1) * 512], op=ALU.add), [Bb, Bh[t]], [Bh[t]])

        def load_wo(w_ap):
            for dh in range(2):
                P.dma("pool", lambda e, dh=dh: e.dma_start(out=wo[:, :, dh * 512:(dh + 1) * 512], in_=w_ap.rearrange("(kc p) f -> p kc f", p=128)[:, :, dh * 512:(dh + 1) * 512]), [], [Bwo[dh]], Bwo[dh])

        def zero_state():
            for hd in range(8):
                P.op("dve", lambda e, hd=hd: e.memset(Sst[:, hd, :], 0.0), [], [BS[hd]])

        def rotary_tables(b, tok0):
            P.dma("sp", lambda e: e.dma_start(out=posi[:], in_=pos_d[b:b + 1, tok0:tok0 + 512].partition_broadcast(128)), [], [Bposi], Bposi)
            ang, Bang = gwk()
            tf, Btf = gwk()
            ti = tf[:].bitcast(I32)
            P.op("dve", lambda e: e.tensor_copy(out=ang[:], in_=posi[:]), [Bposi], [Bang])
            P.op("dve", lambda e: e.tensor_scalar(out=ang[:], in0=ang[:], scalar1=C(C_INVF), scalar2=None, op0=ALU.mult), [Bang, Bcst], [Bang])
            for k, shift in ((1, 0.0), (0, float(np.pi / 2))):
                R = cs[:, k, :]
                P.op("dve", lambda e, shift=shift: e.tensor_scalar(out=tf[:], in0=ang[:], scalar1=shift, scalar2=float(1 / (2 * np.pi)), op0=ALU.add, op1=ALU.mult), [Bang], [Btf])
                sc, Bsc = gwk()
                P.op("dve", lambda e, sc=sc: e.tensor_copy(out=sc[:].bitcast(I32), in_=tf[:]), [Btf], [Bsc])
                P.op("dve", lambda e, sc=sc: e.tensor_copy(out=tf[:], in_=sc[:].bitcast(I32)), [Bsc], [Btf])
                P.op("dve", lambda e, R=R: e.scalar_tensor_tensor(out=R, in0=tf[:], scalar=float(-2 * np.pi), in1=ang[:], op0=ALU.mult, op1=ALU.add), [Btf, Bang], [Bcs])
                if shift != 0.0:
                    P.op("dve", lambda e, R=R, shift=shift: e.tensor_scalar_add(out=R, in0=R, scalar1=shift), [Bcs], [Bcs])
                P.op("dve", lambda e, R=R: e.tensor_single_scalar(out=tf[:], in_=R, scalar=float(np.pi), op=ALU.is_gt), [Bcs], [Btf])
                P.op("dve", lambda e, R=R: e.scalar_tensor_tensor(out=R, in0=tf[:], scalar=float(-2 * np.pi), in1=R, op0=ALU.mult, op1=ALU.add), [Btf, Bcs], [Bcs])
                P.op("dve", lambda e, R=R: e.tensor_single_scalar(out=tf[:], in_=R, scalar=float(-np.pi), op=ALU.is_lt), [Bcs], [Btf])
                P.op("dve", lambda e, R=R: e.scalar_tensor_tensor(out=R, in0=tf[:], scalar=float(2 * np.pi), in1=R, op0=ALU.mult, op1=ALU.add), [Btf, Bcs], [Bcs])
                P.op("act", lambda e, R=R: e.activation(out=R, in_=R, func=AF.Sin), [Bcs], [Bcs])
            P.op("dve", lambda e: e.tensor_scalar(out=cs[:, 1, :], in0=cs[:, 1, :], scalar1=C(C_SIGN), scalar2=None, op0=ALU.mult), [Bcs, Bcst], [Bcs])

        hctr = [0]
        dbg_toks = []
        pend = [None]

        def even_mixer(b, l):
            e_ = l // 2
            win_ap = W["ab_w_in"][e_]
            load_wo(W["ab_w_out"][e_])
            zero_state()
            for tile4 in range(4):
                tok0 = tile4 * 512
                rotary_tables(b, tok0)
                for hd in range(8):
                    s = hctr[0] % 2
                    hctr[0] += 1
                    if hd >= 4 and DBG_FLIP:
                        s = 1 - s
                    Bw = Bwin[s]
                    if hd < 4:
                        hh = hd
                        cq, ck, cv, cg = hh * 128, 512 + hh * 128, 1024 + hh * 128, 1536 + hh * 128
                        load_cols(s, 0, win_ap, cq, 128, 0)
                        load_cols(s, 1, win_ap, cq + 64, 64, 128)
                        load_cols(s, 2, win_ap, cq, 64, 192)
                        load_cols(s, 3, win_ap, ck, 128, 256)
                        load_cols(s, 4, win_ap, ck + 64, 64, 384)
                        load_cols(s, 5, win_ap, ck, 64, 448)
                        load_cols(s, 6, win_ap, cv, 128, 512)
                        load_cols(s, 7, win_ap, cg, 128, 640)
                        vproj_a(s, 512, tok0)
                        for (o0, dst, Bdst, dtab) in ((0, qh[s], Bqh[s], C_DQ), (256, kh[s], Bkh[s], C_DK)):
                            b1, Bb1 = gbank()
                            proj_fm(b1, Bb1, win[s], Bw, o0, 128, tok0)
                            b2, Bb2 = gbank()
                            proj_fm(b2, Bb2, win[s], Bw, o0 + 128, 128, tok0)
                            t1, Bt1 = gwk()
                            t2, Bt2 = gwk()
                            P.op("dve", lambda e, b1=b1, t1=t1: e.tensor_tensor(out=t1[:], in0=b1[:], in1=cs[:, 0, :], op=ALU.mult), [Bb1, Bcs], [Bt1])
                            P.op("dve", lambda e, b2=b2, t2=t2: e.tensor_tensor(out=t2[:], in0=b2[:], in1=cs[:, 1, :], op=ALU.mult), [Bb2, Bcs], [Bt2])
                            P.op("dve", lambda e, t1=t1, t2=t2: e.tensor_tensor(out=t1[:], in0=t1[:], in1=t2[:], op=ALU.add), [Bt1, Bt2], [Bt1])
                            P.op("dve", lambda e, t1=t1, dst=dst, dtab=dtab, hh=hh: e.tensor_tensor(out=dst[:].rearrange("p (c i) -> p c i", c=8), in0=t1[:].rearrange("p (c i) -> p c i", c=8), in1=cst[:, dtab + hh * 64:dtab + (hh + 1) * 64].unsqueeze(1).to_broadcast([128, 8, 64]), op=ALU.mult), [Bt1, Bcst], [Bdst])
                        bg, Bbg = gbank()
                        proj_fm(bg, Bbg, win[s], Bw, 640, 128, tok0)
                        silu_to(gsl[s][:], [Bgsl[s]], bg, Bbg)
                        vproj_b(s)
                        if pend[0] is not None:
                            core_b(*pend[0])
                        core_a(s, hd, 128, RET_A[hh])
                        pend[0] = (s, hd, 128, 0 + e_)
                    else:
                        hh = hd - 4
                        cq, ck, cv, cg = 2048 + hh * 64, 2304 + hh * 64, 2560 + hh * 128, 3072 + hh * 128
                        load_cols(s, 0, win_ap, cq, 64, 0)
                        load_cols(s, 1, win_ap, ck, 64, 64)
                        if hh == 0:
                            load_cols(s, 2, win_ap, 3584, 16, 128)
                        load_cols(s, 6, win_ap, cv, 128, 512)
                        load_cols(s, 7, win_ap, cg, 128, 640)
                        vproj_a(s, 512, tok0)
                        if hh == 0:
                            bb, Bbb = gbank()
                            proj_fm(bb, Bbb, win[s], Bw, 128, 16, tok0)
                            P.op("act", lambda e, bb=bb: e.copy(out=bab[:], in_=bb[0:16, :]), [Bbb], [Bbab])
                        bx, Bbx = gbank()
                        P.op("pe", lambda e, bx=bx, hh=hh: e.matmul(bx[0:64, :], lhsT=wa2[:, e_, hh * 64:(hh + 1) * 64], rhs=bab[:], start=True, stop=True), [Bwa2, Bbab], [Bbx])
                        bq, Bbq = gbank()
                        proj_fm(bq, Bbq, win[s], Bw, 0, 64, tok0)
                        bk, Bbk = gbank()
                        proj_fm(bk, Bbk, win[s], Bw, 64, 64, tok0)
                        L, BL = gwk()
                        P.op("act", lambda e, bx=bx, L=L, hh=hh: e.activation(out=L[0:64, :], in_=bx[0:64, :], func=AF.Exp, scale=-1.0, bias=ba2[:, e_, hh:hh + 1]), [Bbx, Bba2], [BL])
                        P.op("act", lambda e, L=L: e.activation(out=L[0:64, :], in_=L[0:64, :], func=AF.Ln, scale=1.0, bias=C(C_ONE, rows=64)), [BL, Bcst], [BL])
                        G, BG = gwk()
                        P.op("dve", lambda e, L=L, G=G: e.tensor_tensor_scan(out=G[0:64, :], data0=cst[0:64, C_RESET:C_RESET + 512], data1=L[0:64, :], initial=0.0, op0=ALU.mult, op1=ALU.add), [BL, Bcst], [BG])
                        E1, BE1 = gwk()
                        P.op("act", lambda e, G=G, L=L: e.activation(out=L[0:64, :], in_=G[0:64, :], func=AF.Exp, scale=1.0 / 16.0), [BG], [BL])
                        P.op("dve", lambda e, bk=bk, L=L, s=s: e.tensor_tensor(out=kh[s][0:64, :], in0=bk[0:64, :], in1=L[0:64, :], op=ALU.mult), [Bbk, BL], [Bkh[s]])
                        P.op("act", lambda e, G=G, E1=E1: e.activation(out=E1[0:64, :], in_=G[0:64, :], func=AF.Exp, scale=-1.0 / 16.0), [BG], [BE1])
                        P.op("dve", lambda e, bq=bq, E1=E1, s=s: e.scalar_tensor_tensor(out=qh[s][0:64, :], in0=bq[0:64, :], scalar=0.125, in1=E1[0:64, :], op0=ALU.mult, op1=ALU.mult), [Bbq, BE1], [Bqh[s]])
                        P.op("act", lambda e, G=G, s=s: e.activation(out=acol[s][0:64, :], in_=G[0:64, :].rearrange("p (c i) -> p c i", c=8)[:, :, 63], func=AF.Exp, scale=-1.0 / 16.0), [BG], [Bacol[s]])
                        bg, Bbg = gbank()
                        proj_fm(bg, Bbg, win[s], Bw, 640, 128, tok0)
                        silu_to(gsl[s][:], [Bgsl[s]], bg, Bbg)
                        vproj_b(s)
                        if pend[0] is not None:
                            core_b(*pend[0])
                        core_a(s, hd, 64, None)
                        pend[0] = (s, hd, 64, 2 + e_)
                core_b(*pend[0])
                pend[0] = None
                outproj(W["ab_w_out"][e_], tile4)

        def odd_mixer(b, l):
            e_ = l // 2
            win_ap = W["c_w_in"][e_]
            load_wo(W["c_w_out"][e_])
            zero_state()
            for tile4 in range(4):
                tok0 = tile4 * 512
                for hd in range(8):
                    s = hctr[0] % 2
                    hctr[0] += 1
                    Bw = Bwin[s]
                    load_cols(s, 0, win_ap, hd * 128, 128, 0)
                    load_cols(s, 1, win_ap, 1024 + hd * 128, 128, 128)
                    load_cols(s, 6, win_ap, 2048 + hd * 128, 128, 512)
                    load_cols(s, 7, win_ap, 3072 + hd * 128, 128, 640)
                    vproj_a(s, 512, tok0)
                    bf_, Bbf = gbank()
                    proj_fm(bf_, Bbf, win[s], Bw, 128, 128, tok0)
                    sg, Bsg = gwk()
                    P.op("act", lambda e, bf_=bf_, sg=sg: e.activation(out=sg[:], in_=bf_[:], func=AF.Sigmoid), [Bbf], [Bsg])
                    bq, Bbq = gbank()
                    proj_fm(bq, Bbq, win[s], Bw, 0, 128, tok0)
                    qs, Bqs = gwk()
                    silu_to(qs[:], [Bqs], bq, Bbq)
                    bg, Bbg = gbank()
                    proj_fm(bg, Bbg, win[s], Bw, 640, 128, tok0)
                    silu_to(gsl[s][:], [Bgsl[s]], bg, Bbg)
                    G, BG = gwk()
                    P.op("dve", lambda e, sg=sg, G=G, hd=hd: e.tensor_scalar(out=G[:], in0=sg[:], scalar1=oml[:, l, hd:hd + 1], scalar2=lbt[:, l, hd:hd + 1], op0=ALU.mult, op1=ALU.add), [Bsg, Blbt], [BG])
                    P.op("act", lambda e, G=G: e.activation(out=G[:], in_=G[:], func=AF.Ln), [BG], [BG])
                    P.op("dve", lambda e, sg=sg, hd=hd: e.tensor_scalar(out=sg[:], in0=sg[:], scalar1=noml[:, l, hd:hd + 1], scalar2=oml[:, l, hd:hd + 1], op0=ALU.mult, op1=ALU.add), [Bsg, Blbt], [Bsg])
                    G2, BG2 = gwk()
                    P.op("dve", lambda e, G=G, G2=G2: e.tensor_tensor_scan(out=G2[:], data0=cst[:, C_RESET:C_RESET + 512], data1=G[:], initial=0.0, op0=ALU.mult, op1=ALU.add), [BG, Bcst], [BG2])
                    P.op("act", lambda e, G2=G2, G=G: e.activation(out=G[:], in_=G2[:], func=AF.Exp, scale=-1.0), [BG2], [BG])
                    P.op("dve", lambda e, sg=sg, G=G, s=s: e.tensor_tensor(out=kh[s][:], in0=sg[:], in1=G[:], op=ALU.mult), [BG, Bsg], [Bkh[s]])
                    P.op("act", lambda e, G2=G2, s=s: e.activation(out=acol[s][:], in_=G2[:].rearrange("p (c i) -> p c i", c=8)[:, :, 63], func=AF.Exp), [BG2], [Bacol[s]])
                    P.op("act", lambda e, G2=G2: e.activation(out=G2[:], in_=G2[:], func=AF.Exp), [BG2], [BG2])
                    P.op("dve", lambda e, qs=qs, G2=G2, s=s: e.scalar_tensor_tensor(out=qh[s][:], in0=qs[:], scalar=float(128.0 ** -0.5), in1=G2[:], op0=ALU.mult, op1=ALU.mult), [Bqs, BG2], [Bqh[s]])
                    vproj_b(s)
                    if pend[0] is not None:
                        core_b(*pend[0])
                    core_a(s, hd, 128, None)
                    pend[0] = (s, hd, 128, 4 + e_)
                core_b(*pend[0])
                pend[0] = None
                outproj(W["c_w_out"][e_], tile4)

        fctr = [0]
        dctr = [0]

        def swiglu_expert(wg_ap, wu_ap, wd_ap, F, gate_col):
            nchunks = F // 128
            c0 = 0
            while c0 < nchunks:
                ncg = min(4, nchunks - c0)
                s = fctr[0] % 2
                fctr[0] += 1
                f0 = c0 * 128
                nf = ncg * 128
                P.dma("pool", lambda e, s=s, f0=f0, nf=nf: e.dma_start(out=wg[s][:, :, 0:nf], in_=wg_ap.rearrange("(kc p) f -> p kc f", p=128)[:, :, f0:f0 + nf]), [], [Bwg[s]], Bwg[s])
                P.dma("pool", lambda e, s=s, f0=f0, nf=nf: e.dma_start(out=wu[s][:, :, 0:nf], in_=wu_ap.rearrange("(kc p) f -> p kc f", p=128)[:, :, f0:f0 + nf]), [], [Bwu[s]], Bwu[s])
                P.dma("pool", lambda e, s=s, f0=f0, ncg=ncg: e.dma_start(out=wd[s][:, 0:ncg, :], in_=wd_ap[f0:f0 + ncg * 128, :].rearrange("(fc p) d -> p fc d", p=128)), [], [Bwd[s]], Bwd[s])
                for tg in range(4):
                    tok0 = tg * 512
                    hs = (fctr[0] * 4 + tg) % 2
                    for fc in range(ncg):
                        Pg, BPg = pb[0 + (fc % 2)], Bpb[0 + (fc % 2)]
                        Pu, BPu = pb[2 + (fc % 2)], Bpb[2 + (fc % 2)]
                        for kc in range(8):
                            P.op("pe", lambda e, kc=kc, fc=fc, Pg=Pg, s=s, tok0=tok0: e.matmul(Pg[:], lhsT=wg[s][:, kc, fc * 128:(fc + 1) * 128], rhs=hnT[:, kc, tok0:tok0 + 512], start=(kc == 0), stop=(kc == 7)), [Bwg[s]] + BhnT[tg * 4:tg * 4 + 4], [BPg])
                        for kc in range(8):
                            P.op("pe", lambda e, kc=kc, fc=fc, Pu=Pu, s=s, tok0=tok0: e.matmul(Pu[:], lhsT=wu[s][:, kc, fc * 128:(fc + 1) * 128], rhs=hnT[:, kc, tok0:tok0 + 512], start=(kc == 0), stop=(kc == 7)), [Bwu[s]] + BhnT[tg * 4:tg * 4 + 4], [BPu])
                        sg, Bsg = gwk()
                        P.op("act", lambda e, Pg=Pg, sg=sg: e.activation(out=sg[:], in_=Pg[:], func=AF.Silu), [BPg], [Bsg])
                        P.op("dve", lambda e, Pu=Pu, sg=sg, hs=hs, fc=fc: e.tensor_tensor(out=hid[hs][:, fc, :], in0=Pu[:], in1=sg[:], op=ALU.mult), [BPu, Bsg], [Bhid[hs][fc]])
                    for sub in range(4):
                        t = tg * 4 + sub
                        for dh in range(2):
                            k = 4 + (dctr[0] % 4)
                            dctr[0] += 1
                            Pd, BPd = pb[k], Bpb[k]
                            for fc in range(ncg):
                                P.op("pe", lambda e, fc=fc, sub=sub, dh=dh, Pd=Pd, s=s, hs=hs: e.matmul(Pd[:], lhsT=hid[hs][:, fc, sub * 128:(sub + 1) * 128], rhs=wd[s][:, fc, dh * 512:(dh + 1) * 512], start=(fc == 0), stop=(fc == ncg - 1)), [Bhid[hs][fc], Bwd[s]], [BPd])
                            hsl = h[:, t, dh * 512:(dh + 1) * 512]
                            if gate_col is None:
                                P.op("dve", lambda e, Pd=Pd, hsl=hsl: e.tensor_tensor(out=hsl, in0=Pd[:], in1=hsl, op=ALU.add), [BPd, Bh[t]], [Bh[t]])
                            else:
                                P.op("dve", lambda e, Pd=Pd, hsl=hsl, t=t: e.scalar_tensor_tensor(out=hsl, in0=Pd[:], scalar=gt[:, t, gate_col:gate_col + 1], in1=hsl, op0=ALU.mult, op1=ALU.add), [BPd, Bh[t], Bgt], [Bh[t]])
                c0 += ncg

        def router(e_):
            P.dma("pool", lambda e: e.dma_start(out=wr[:], in_=W["moe_router"][e_].rearrange("(kc p) n -> p kc n", p=128)), [], [Bwr], Bwr)
            for t in range(NT):
                bank, Bb = gbank()
                for kc in range(8):
                    P.op("pe", lambda e, kc=kc, t=t, bank=bank: e.matmul(bank[:, 0:8], lhsT=hnT[:, kc, t * 128:(t + 1) * 128], rhs=wr[:, kc, :], start=(kc == 0), stop=(kc == 7)), [BhnT[t], Bwr], [Bb])
                P.op("act", lambda e, t=t, bank=bank: e.copy(out=lg[:, t, :], in_=bank[:, 0:8]), [Bb], [Blg])
            m1 = stat[:, 32:48]
            m2 = stat[:, 48:64]

            def bc(a):
                return a.unsqueeze(2).to_broadcast([128, NT, 8])
            P.op("dve", lambda e: e.tensor_reduce(out=m1, in_=lg[:], axis=AX.X, op=ALU.max), [Blg], [Bstat])
            P.op("dve", lambda e: e.tensor_tensor(out=rt[0][:], in0=lg[:], in1=bc(m1), op=ALU.is_equal), [Blg, Bstat], [Brt[0]])
            P.op("dve", lambda e: e.scalar_tensor_tensor(out=rt[1][:], in0=rt[0][:], scalar=-1e30, in1=lg[:], op0=ALU.mult, op1=ALU.add), [Brt[0], Blg], [Brt[1]])
            P.op("dve", lambda e: e.tensor_reduce(out=m2, in_=rt[1][:], axis=AX.X, op=ALU.max), [Brt[1]], [Bstat])
            P.op("dve", lambda e: e.tensor_tensor(out=rt[0][:], in0=lg[:], in1=bc(m2), op=ALU.is_ge), [Blg, Bstat], [Brt[0]])
            P.op("dve", lambda e: e.tensor_tensor(out=rt[1][:], in0=lg[:], in1=bc(m1), op=ALU.subtract), [Blg, Bstat], [Brt[1]])
            P.op("act", lambda e: e.activation(out=rt[1][:], in_=rt[1][:], func=AF.Exp), [Brt[1]], [Brt[1]])
            P.op("dve", lambda e: e.tensor_tensor(out=rt[1][:], in0=rt[1][:], in1=rt[0][:], op=ALU.mult), [Brt[0], Brt[1]], [Brt[1]])
            P.op("dve", lambda e: e.tensor_reduce(out=m1, in_=rt[1][:], axis=AX.X, op=ALU.add), [Brt[1]], [Bstat])
            P.op("dve", lambda e: e.reciprocal(out=m1, in_=m1), [Bstat], [Bstat])
            P.op("dve", lambda e: e.tensor_tensor(out=gt[:], in0=rt[1][:], in1=bc(m1), op=ALU.mult), [Brt[1], Bstat], [Bgt])

        final_toks = []
        for b in range(nseq):
            for q4 in range(4):
                P.dma("sp", lambda e, b=b, q4=q4: e.dma_start(out=h[:, q4 * 4:(q4 + 1) * 4, :], in_=x_d[b, q4 * 512:(q4 + 1) * 512, :].rearrange("(t p) d -> p t d", p=128)), [], Bh[q4 * 4:q4 * 4 + 4], Bh[q4 * 4])
            sub = 0
            for l in range(4):
                e_ = l // 2
                if sub < nsub and (ONLY is None or sub in ONLY):
                    norm(W["norm_mix_g"][l:l + 1, :])
                    alias_sync(ffn_bufs(), mixer_bufs())
                    if l % 2 == 0:
                        even_mixer(b, l)
                    else:
                        odd_mixer(b, l)
                sub += 1
                if sub < nsub and (ONLY is None or sub in ONLY):
                    norm(W["norm_ffn_g"][l:l + 1, :])
                    alias_sync(mixer_bufs(), ffn_bufs())
                    if l % 2 == 0:
                        swiglu_expert(W["ffn_w_gate"][e_], W["ffn_w_up"][e_], W["ffn_w_down"][e_], 2816, None)
                    else:
                        router(e_)
                        for ex in range(8):
                            swiglu_expert(W["moe_w_gate"][e_, ex], W["moe_w_up"][e_, ex], W["moe_w_down"][e_, ex], 3584, ex)
                sub += 1
            if final:
                final_toks = norm(W["final_norm_g"].rearrange("(o d) -> o d", o=1), to_out=out_d[b])
            else:
                final_toks = []
                for t in range(NT):
                    final_toks.append(P.dma("sp", lambda e, t=t, b=b: e.dma_start(out=out_d[b, t * 128:(t + 1) * 128, :], in_=h[:, t, :]), [Bh[t]], [], Bh[t]))
        P.emit(final_toks + dbg_toks)
        print("instr counts", {k: len(v) for k, v in P.q.items()}, "sems", P.nsem, flush=True)
    return nc


_NC_CACHE = {}


def run(inputs, nseq_per_core, nsub=8, final=True, ncores=8):
    key = (nseq_per_core, nsub, final)
    if key not in _NC_CACHE:
        _NC_CACHE[key] = build(nseq_per_core, nsub, final)
    nc = _NC_CACHE[key]
    consts = make_consts()
    in_maps = []
    for c in range(ncores):
        m = {n: np.ascontiguousarray(inputs[n], dtype=np.float32) for n, _ in WNAMES}
        m["x"] = np.ascontiguousarray(inputs["x"][c * nseq_per_core:(c + 1) * nseq_per_core], dtype=np.float32)
        m["positions"] = np.ascontiguousarray(inputs["positions"][c * nseq_per_core:(c + 1) * nseq_per_core], dtype=np.int32)
        m["consts"] = consts
        in_maps.append(m)
    res = run_bass_kernel_spmd(nc, in_maps, core_ids=list(range(ncores)))
    if DBG:
        np.save("dbg_out.npy", res.results[0]["dbg"])
    return np.concatenate([r["out"] for r in res.results], axis=0)


def kernel(**inputs):
    return run(inputs, 4).astype(np.float32)
```
